# Optimizing a Trainium2 kernel written in Bass

```python
import jax
import jax.numpy as jnp
from jax import lax
import numpy as np

D_MODEL = 2048
BATCH = 1
SEQ = 8192
DEPTH = 2

HEAD_DIM = 128
ROPE_THETA = 10000.0
ATTN_SCALE = HEAD_DIM ** -0.5
QB = 128
NEG = -1e30
TINY = 1e-30

DIL_PATTERNS = ((128, 1), (512, 4), (2048, 16))
A_HEADS_PER_GROUP = 4
A_HEADS = A_HEADS_PER_GROUP * len(DIL_PATTERNS)
B_HEADS = 4
C_HEADS = 4
IDX_HEADS = 16
IDX_DIM = 64
DSA_TOPK = 256
D_HEADS = 4
CMP_LEN = 32
CMP_STRIDE = 16
CMP_HIDDEN = 256
SLC_BLOCK = 64
SLC_COUNT = 16
WIN = 512
FORCE = 1e9
N_BRANCH = 4
BRANCH_WIDTH = 4 * HEAD_DIM
FFN_CEIL = -(-8 * D_MODEL // 3)
FFN_HIDDEN = -(-FFN_CEIL // 256) * 256

IN_SPLITS = (
    A_HEADS * HEAD_DIM, A_HEADS * HEAD_DIM, A_HEADS * HEAD_DIM,
    B_HEADS * HEAD_DIM, B_HEADS * HEAD_DIM, B_HEADS * HEAD_DIM, B_HEADS,
    C_HEADS * HEAD_DIM, C_HEADS * HEAD_DIM, C_HEADS * HEAD_DIM,
    IDX_HEADS * IDX_DIM, IDX_DIM, IDX_HEADS,
    D_HEADS * HEAD_DIM,
    HEAD_DIM, HEAD_DIM, HEAD_DIM, HEAD_DIM, HEAD_DIM, HEAD_DIM,
    3 * D_HEADS,
)
IN_COLS = sum(IN_SPLITS)

kernel_name = 'hybrid_gated_dilated_fox_dsa_nsa_block'


def rms_norm(x, g, eps=1e-6):
    xf = x.astype(jnp.float32)
    y = xf * lax.rsqrt(jnp.mean(xf * xf, axis=-1, keepdims=True) + eps)
    return (y * g.astype(jnp.float32)).astype(x.dtype)


def rope(x, pos):
    half = x.shape[-1] // 2
    inv_freq = ROPE_THETA ** (-jnp.arange(half, dtype=jnp.float32) / half)
    ang = pos.astype(jnp.float32)[:, None] * inv_freq[None, :]
    cos = jnp.cos(ang)[None, :, None, :]
    sin = jnp.sin(ang)[None, :, None, :]
    x1 = x[..., :half].astype(jnp.float32)
    x2 = x[..., half:].astype(jnp.float32)
    return jnp.concatenate([x1 * cos - x2 * sin, x2 * cos + x1 * sin], axis=-1).astype(x.dtype)


def masked_softmax(logits, mask):
    logits = jnp.where(mask, logits.astype(jnp.float32), NEG)
    m = jnp.max(logits, axis=-1, keepdims=True)
    p = jnp.where(mask, jnp.exp(logits - m), 0.0)
    s = jnp.maximum(jnp.sum(p, axis=-1, keepdims=True), TINY)
    return p / s, (m + jnp.log(s))[..., 0]


def sweep_query_blocks(fn, seq_len):
    out = lax.map(fn, jnp.arange(seq_len // QB))
    out = jnp.moveaxis(out, 0, 1)
    return out.reshape((out.shape[0], seq_len) + out.shape[3:])


def dilated_group(q, k, v, window, dilation):
    B, S, H, Dh = q.shape
    span = window // dilation
    L = S // dilation
    nb = -(-L // QB)
    Lp = nb * QB

    def to_blocks(a):
        a = a.reshape(B, L, dilation, H, Dh).transpose(0, 2, 1, 3, 4)
        a = jnp.pad(a, ((0, 0), (0, 0), (0, Lp - L), (0, 0), (0, 0)))
        return a.reshape(B, dilation, nb, QB, H, Dh)

    def with_prev(a):
        prev = jnp.pad(a, ((0, 0), (0, 0), (1, 0), (0, 0), (0, 0), (0, 0)))[:, :, :-1]
        return jnp.concatenate([prev, a], axis=3)

    qb = to_blocks(q)
    kb = with_prev(to_blocks(k))
    vb = with_prev(to_blocks(v))
    logits = jnp.einsum('brnqhd,brnkhd->brnhqk', qb, kb).astype(jnp.float32) * ATTN_SCALE
    qi = jnp.arange(QB)[:, None]
    ki = jnp.arange(2 * QB)[None, :]
    dist = QB + qi - ki
    band = (dist >= 0) & (dist <= span)
    has_prev = (jnp.arange(nb) > 0)[:, None, None] | (ki >= QB)[None]
    mask = (band[None] & has_prev)[None, None, :, None]
    probs, lse = masked_softmax(logits, mask)
    out = jnp.einsum('brnhqk,brnkhd->brnqhd', probs.astype(v.dtype), vb)
    out = out.reshape(B, dilation, Lp, H, Dh)[:, :, :L].transpose(0, 2, 1, 3, 4).reshape(B, S, H, Dh)
    lse = lse.transpose(0, 1, 2, 4, 3).reshape(B, dilation, Lp, H)[:, :, :L]
    lse = lse.transpose(0, 2, 1, 3).reshape(B, S, H)
    return out, lse


def mixer_a(q, k, v):
    outs, lses = [], []
    for g, (window, dilation) in enumerate(DIL_PATTERNS):
        sl = slice(g * A_HEADS_PER_GROUP, (g + 1) * A_HEADS_PER_GROUP)
        o, l = dilated_group(q[:, :, sl], k[:, :, sl], v[:, :, sl], window, dilation)
        outs.append(o)
        lses.append(l)
    wts = jax.nn.softmax(jnp.stack(lses, axis=0), axis=0)
    return jnp.einsum('gbsh,gbshd->bshd', wts.astype(v.dtype), jnp.stack(outs, axis=0))


def mixer_b(q, k, v, f_logit):
    B, S, H, Dh = q.shape
    c = lax.cumsum(jax.nn.log_sigmoid(f_logit.astype(jnp.float32)), axis=1)
    c_k = c.transpose(0, 2, 1)[:, :, None, :]
    kpos = jnp.arange(S)

    def block(b):
        q0 = b * QB
        qb = lax.dynamic_slice_in_dim(q, q0, QB, axis=1)
        cb = lax.dynamic_slice_in_dim(c, q0, QB, axis=1).transpose(0, 2, 1)[..., None]
        logits = jnp.einsum('bqhd,bkhd->bhqk', qb, k).astype(jnp.float32) * ATTN_SCALE + cb - c_k
        mask = (q0 + jnp.arange(QB))[:, None] >= kpos[None, :]
        probs, _ = masked_softmax(logits, mask)
        return jnp.einsum('bhqk,bkhd->bqhd', probs.astype(v.dtype), v)

    return sweep_query_blocks(block, S)


def mixer_c(q, k, v, q_idx, k_idx, w_idx):
    B, S, H, Dh = q.shape
    topk = min(DSA_TOPK, S // 4)
    kpos = jnp.arange(S)
    bi = jnp.arange(B)[:, None, None]

    def block(b):
        q0 = b * QB
        qpos = q0 + jnp.arange(QB)
        qib = lax.dynamic_slice_in_dim(q_idx, q0, QB, axis=1)
        wib = lax.dynamic_slice_in_dim(w_idx, q0, QB, axis=1).astype(jnp.float32) * IDX_HEADS ** -0.5
        rel = jax.nn.relu(jnp.einsum('bqhe,bke->bqhk', qib, k_idx).astype(jnp.float32) * IDX_DIM ** -0.5)
        score = jnp.einsum('bqh,bqhk->bqk', wib, rel)
        score = jnp.where((kpos[None, :] <= qpos[:, None])[None], score, NEG)
        _, idx = lax.top_k(score, topk)
        ks = k[bi, idx]
        vs = v[bi, idx]
        qb = lax.dynamic_slice_in_dim(q, q0, QB, axis=1)
        logits = jnp.einsum('bqhd,bqkhd->bqhk', qb, ks).astype(jnp.float32) * ATTN_SCALE
        valid = (idx <= qpos[None, :, None])[:, :, None, :]
        probs, _ = masked_softmax(logits, valid)
        return jnp.einsum('bqhk,bqkhd->bqhd', probs.astype(v.dtype), vs)

    return sweep_query_blocks(block, S)


def compress(x, pe, w1, w2):
    B, S, Dh = x.shape
    nc = (S - CMP_LEN) // CMP_STRIDE + 1
    idx = jnp.arange(nc)[:, None] * CMP_STRIDE + jnp.arange(CMP_LEN)[None, :]
    blocks = (x[:, idx] + pe).reshape(B, nc, CMP_LEN * Dh)
    return jax.nn.gelu(blocks @ w1) @ w2


def mixer_d(q, kc, vc, ks, vs, kw, vw, gate_logit, pe, w1, w2):
    B, S, H, Dh = q.shape
    k_cmp = compress(kc, pe[0], w1[0], w2[0])
    v_cmp = compress(vc, pe[1], w1[1], w2[1])
    nc = k_cmp.shape[1]
    cmp_end = jnp.arange(nc) * CMP_STRIDE + CMP_LEN - 1
    n_slc = S // SLC_BLOCK
    n_sel = min(SLC_COUNT, n_slc)
    ci = jnp.arange(nc)[:, None]
    sj = jnp.arange(n_slc)[None, :]
    overlap = ((ci * CMP_STRIDE < (sj + 1) * SLC_BLOCK) &
               (ci * CMP_STRIDE + CMP_LEN > sj * SLC_BLOCK)).astype(jnp.float32)
    ks_blk = ks.reshape(B, n_slc, SLC_BLOCK, Dh)
    vs_blk = vs.reshape(B, n_slc, SLC_BLOCK, Dh)
    kw_pad = jnp.pad(kw, ((0, 0), (WIN, 0), (0, 0)))
    vw_pad = jnp.pad(vw, ((0, 0), (WIN, 0), (0, 0)))
    gates = jax.nn.sigmoid(gate_logit.astype(jnp.float32))
    bi = jnp.arange(B)[:, None, None]
    jpos = jnp.arange(n_slc)[None, :]

    def block(b):
        q0 = b * QB
        qpos = q0 + jnp.arange(QB)
        qb = lax.dynamic_slice_in_dim(q, q0, QB, axis=1)
        lc = jnp.einsum('bqhd,bnd->bqhn', qb, k_cmp).astype(jnp.float32) * ATTN_SCALE
        mc = (cmp_end[None, :] <= qpos[:, None])[None, :, None, :]
        pc, _ = masked_softmax(lc, mc)
        o_cmp = jnp.einsum('bqhn,bnd->bqhd', pc.astype(qb.dtype), v_cmp)
        imp = jnp.einsum('bqhn,nj->bqj', pc, overlap)
        cur = (qpos // SLC_BLOCK)[:, None]
        admissible = jpos * SLC_BLOCK <= qpos[:, None]
        forced = (jpos == 0) | (jpos == cur) | (jpos == cur - 1)
        imp = jnp.where(forced[None], FORCE, jnp.where(admissible[None], imp, NEG))
        _, sel = lax.top_k(imp, n_sel)
        kg = ks_blk[bi, sel]
        vg = vs_blk[bi, sel].reshape(B, QB, n_sel * SLC_BLOCK, Dh)
        ls = jnp.einsum('bqhd,bqnld->bqhnl', qb, kg).astype(jnp.float32) * ATTN_SCALE
        ls = ls.reshape(B, QB, H, n_sel * SLC_BLOCK)
        tokpos = sel[..., None] * SLC_BLOCK + jnp.arange(SLC_BLOCK)
        ms = (tokpos <= qpos[None, :, None, None]).reshape(B, QB, 1, n_sel * SLC_BLOCK)
        ps, _ = masked_softmax(ls, ms)
        o_slc = jnp.einsum('bqhk,bqkd->bqhd', ps.astype(qb.dtype), vg)
        kwb = lax.dynamic_slice_in_dim(kw_pad, q0, QB + WIN, axis=1)
        vwb = lax.dynamic_slice_in_dim(vw_pad, q0, QB + WIN, axis=1)
        lw = jnp.einsum('bqhd,bkd->bqhk', qb, kwb).astype(jnp.float32) * ATTN_SCALE
        kpos = q0 - WIN + jnp.arange(QB + WIN)
        dist = qpos[:, None] - kpos[None, :]
        mw = ((dist >= 0) & (dist < WIN) & (kpos[None, :] >= 0))[None, :, None, :]
        pw, _ = masked_softmax(lw, mw)
        o_win = jnp.einsum('bqhk,bkd->bqhd', pw.astype(qb.dtype), vwb)
        gb = lax.dynamic_slice_in_dim(gates, q0, QB, axis=1)
        out = gb[..., 0:1] * o_cmp + gb[..., 1:2] * o_slc + gb[..., 2:3] * o_win
        return out.astype(qb.dtype)

    return sweep_query_blocks(block, S)


def hybrid_layer(x, norm_mix, w_in, w_gate, b_gate, b_f, cmp_pe, cmp_w1, cmp_w2,
                 w_branch, w_out, norm_ffn, w_ffn_in, w_ffn_out):
    B, S, _ = x.shape
    pos = jnp.arange(S)
    h = rms_norm(x, norm_mix)
    (aq, ak, av, bq, bk, bv, bfl, cq, ck, cv, cqi, cki, cwi,
     dq, dkc, dvc, dks, dvs, dkw, dvw, dg) = jnp.split(
        h @ w_in, np.cumsum(IN_SPLITS)[:-1].tolist(), axis=-1)

    def heads(t):
        return t.reshape(B, S, -1, HEAD_DIM)

    def rope1(t):
        return rope(t[:, :, None, :], pos)[:, :, 0]

    o_a = mixer_a(rope(heads(aq), pos), rope(heads(ak), pos), heads(av))
    o_b = mixer_b(heads(bq), heads(bk), heads(bv), bfl + b_f)
    o_c = mixer_c(rope(heads(cq), pos), rope(heads(ck), pos), heads(cv),
                  rope(cqi.reshape(B, S, IDX_HEADS, IDX_DIM), pos), rope1(cki), cwi)
    o_d = mixer_d(rope(heads(dq), pos), rope1(dkc), dvc, rope1(dks), dvs, rope1(dkw), dvw,
                  dg.reshape(B, S, D_HEADS, 3), cmp_pe, cmp_w1, cmp_w2)

    branches = jnp.stack([o_a, o_b, o_c, o_d], axis=2).reshape(B, S, N_BRANCH, BRANCH_WIDTH)
    y = jnp.einsum('bsnc,ncd->bsnd', branches, w_branch)
    g = jax.nn.sigmoid(jnp.einsum('bsd,dne->bsne', h, w_gate) + b_gate)
    x = x + jnp.einsum('bsnd,bsnd->bsd', g, y) @ w_out

    h2 = rms_norm(x, norm_ffn)
    gate, up = jnp.split(h2 @ w_ffn_in, 2, axis=-1)
    return x + (jax.nn.silu(gate) * up) @ w_ffn_out


def setup_inputs(seed: int = 0) -> dict:
    key = jax.random.key(seed)
    ks = jax.random.split(key, 16)

    def dense(k, shape, fan_in):
        return jax.random.normal(k, shape, jnp.float32) * fan_in ** -0.5

    def gain(k, shape):
        return 1.0 + 0.05 * jax.random.normal(k, shape, jnp.float32)

    return {
        'x': jax.random.normal(ks[0], (BATCH, SEQ, D_MODEL), jnp.float32),
        'norm_mix': gain(ks[1], (DEPTH, D_MODEL)),
        'w_in': dense(ks[2], (DEPTH, D_MODEL, IN_COLS), D_MODEL),
        'w_gate': dense(ks[3], (DEPTH, D_MODEL, N_BRANCH, D_MODEL), D_MODEL),
        'b_gate': 0.02 * jax.random.normal(ks[4], (DEPTH, N_BRANCH, D_MODEL), jnp.float32),
        'b_f': 3.0 + 0.1 * jax.random.normal(ks[5], (DEPTH, B_HEADS), jnp.float32),
        'cmp_pe': 0.1 * jax.random.normal(ks[6], (DEPTH, 2, CMP_LEN, HEAD_DIM), jnp.float32),
        'cmp_w1': dense(ks[7], (DEPTH, 2, CMP_LEN * HEAD_DIM, CMP_HIDDEN), CMP_LEN * HEAD_DIM),
        'cmp_w2': dense(ks[8], (DEPTH, 2, CMP_HIDDEN, HEAD_DIM), CMP_HIDDEN),
        'w_branch': dense(ks[9], (DEPTH, N_BRANCH, BRANCH_WIDTH, D_MODEL), BRANCH_WIDTH),
        'w_out': dense(ks[10], (DEPTH, D_MODEL, D_MODEL), D_MODEL),
        'norm_ffn': gain(ks[11], (DEPTH, D_MODEL)),
        'w_ffn_in': dense(ks[12], (DEPTH, D_MODEL, 2 * FFN_HIDDEN), D_MODEL),
        'w_ffn_out': dense(ks[13], (DEPTH, FFN_HIDDEN, D_MODEL), FFN_HIDDEN),
        'norm_final': gain(ks[14], (D_MODEL,)),
    }


def reference(x, norm_mix, w_in, w_gate, b_gate, b_f, cmp_pe, cmp_w1, cmp_w2,
              w_branch, w_out, norm_ffn, w_ffn_in, w_ffn_out, norm_final):
    for l in range(DEPTH):
        x = hybrid_layer(x, norm_mix[l], w_in[l], w_gate[l], b_gate[l], b_f[l],
                         cmp_pe[l], cmp_w1[l], cmp_w2[l], w_branch[l], w_out[l],
                         norm_ffn[l], w_ffn_in[l], w_ffn_out[l])
    return rms_norm(x, norm_final)
```

```python
from contextlib import ExitStack
import numpy as np
import ml_dtypes
import concourse.bass as bass
import concourse.mybir as mybir
from concourse.bass_utils import run_bass_kernel_spmd


F32 = mybir.dt.float32
BF16 = mybir.dt.bfloat16
AF = mybir.ActivationFunctionType
ALU = mybir.AluOpType
AX = mybir.AxisListType

SEM_MAXV = 16000
SAME_ENGINE_SYNC = True


class Sched:
    ENGS = ("pe", "act", "dve", "pool", "sp")

    def __init__(self, nc, stack):
        self.nc = nc
        self.stack = stack
        self.ops = {e: [] for e in self.ENGS}
        self.semh = {}
        self.cnt = {}
        self.epoch = {}
        self.waited = {e: {} for e in self.ENGS}
        self.res = {}
        self.nsem = 0

    def _semkey(self, base, inc):
        ep = self.epoch.get(base, 0)
        key = (base, ep)
        if self.cnt.get(key, 0) + inc > (32000 if inc == 16 else SEM_MAXV):
            ep += 1
            self.epoch[base] = ep
            key = (base, ep)
        if key not in self.semh:
            self.semh[key] = self.stack.enter_context(self.nc.semaphore("s%d" % self.nsem))
            self.nsem += 1
            self.cnt[key] = 0
        return key

    def op(self, eng, fn, reads=(), writes=(), dsem=None):
        deps = []
        def _isps(k):
            n = k if isinstance(k, str) else k[0]
            return isinstance(n, str) and n.startswith("ps")
        writes = list(writes) + [r for r in reads if _isps(r) and r not in writes]
        reads = [r for r in reads if not _isps(r)]
        for r in reads:
            st = self.res.get(r)
            if st is not None and st[0] is not None:
                deps.append(st[0])
        for w in writes:
            st = self.res.get(w)
            if st is not None:
                if st[0] is not None:
                    deps.append(st[0])
                deps.extend(st[1])
        is_dma = dsem is not None
        inc = 16 if is_dma else 1
        key = self._semkey(("d", dsem) if is_dma else ("e", eng), inc)
        self.cnt[key] += inc
        ev = (key, self.cnt[key], eng if not is_dma else None)
        waits = []
        wd = self.waited[eng]
        best = {}
        for (k, v, src) in deps:
            if src == eng and (eng == "pe" or not SAME_ENGINE_SYNC):
                continue
            if wd.get(k, 0) >= v:
                continue
            if best.get(k, 0) < v:
                best[k] = v
        for k, v in best.items():
            wd[k] = v
            waits.append((k, v))
        self.ops[eng].append((waits, fn, key, inc))
        for r in reads:
            st = self.res.setdefault(r, [None, []])
            st[1].append(ev)
        for w in writes:
            self.res[w] = [ev, []]
        return ev

    def wait_all(self, eng, evs):
        waits = []
        wd = self.waited[eng]
        best = {}
        for (k, v, src) in evs:
            if wd.get(k, 0) < v and best.get(k, 0) < v:
                best[k] = v
        for k, v in best.items():
            wd[k] = v
            waits.append((k, v))
        self.ops[eng].append((waits, None, None, 0))

    def emit(self):
        nc = self.nc
        eng_obj = {"pe": "tensor", "act": "scalar", "dve": "vector", "pool": "gpsimd", "sp": "sync"}
        with nc.Block() as block:
            for e in self.ENGS:
                if not self.ops[e]:
                    continue
                ops = self.ops[e]
                semh = self.semh

                def body(engine, ops=ops):
                    for (waits, fn, key, inc) in ops:
                        for (k, v) in waits:
                            engine.wait_ge(semh[k], v)
                        if fn is not None:
                            ins = fn(engine)
                            ins.then_inc(semh[key], inc)

                getattr(block, eng_obj[e])(body)
        self.ops = {e: [] for e in self.ENGS}

    def barrier(self):
        evs = [(k, v, None) for k, v in self.cnt.items() if v > 0]
        for e in self.ENGS:
            self.wait_all(e, evs)

    def n_ops(self):
        return {e: len(self.ops[e]) for e in self.ENGS}


T = 1024
TG = 512
NTG = T // TG
D = 2048
KC = D // 128
EPS = 1e-6


def new_nc():
    return bass.Bass("TRN2", target_bir_lowering=False)


class Ctx:
    def __init__(self, nc, st):
        self.nc = nc
        self.st = st
        self.S = Sched(nc, st)

    def sb(self, name, shape, dt):
        return self.st.enter_context(self.nc.sbuf_tensor(name, shape, dt))

    def ps(self, name, shape, dt):
        return self.st.enter_context(self.nc.psum_tensor(name, shape, dt))

    def din(self, name, shape, dt):
        return self.nc.dram_tensor(name, shape, dt, kind="ExternalInput").ap()

    def dout(self, name, shape, dt):
        return self.nc.dram_tensor(name, shape, dt, kind="ExternalOutput").ap()


class WStream:
    def __init__(self, cx, nslots=4, kcmax=16):
        self.cx = cx
        self.n = nslots
        self.i = 0
        self.stg = [cx.sb("wstg%d" % i, [128, kcmax, 128], F32) for i in range(nslots)]
        self.bf = [cx.sb("wbf%d" % i, [128, kcmax, 128], BF16) for i in range(nslots)]

    def load(self, w_ap, k0, kc, c0, M, cast_eng="pool"):
        S = self.cx.S
        s = self.i % self.n
        self.i += 1
        src = w_ap[k0:k0 + kc * 128, c0:c0 + M].rearrange("(kc p) m -> p kc m", p=128)
        stg = self.stg[s]
        bf = self.bf[s]
        for k4 in range(0, kc, 4):
            k5 = min(kc, k4 + 4)
            S.op("sp", (lambda e, k4=k4, k5=k5: e.dma_start(out=stg[:, k4:k5, 0:M], in_=src[:, k4:k5, :])), writes=[("wstg", s)], dsem=("wstg", s))
        S.op(cast_eng, lambda e: e.tensor_copy(bf[:, 0:kc, 0:M], stg[:, 0:kc, 0:M]), reads=[("wstg", s)], writes=[("wbf", s)])
        return bf, ("wbf", s)


def pipeline(units, depth=2):
    handles = {}
    n = len(units)
    for i in range(n + depth):
        if i < n:
            handles[i] = units[i][0]()
        j = i - depth
        if j >= 0:
            units[j][1](handles.pop(j))


def rmsnorm_fm(cx, x_sb, xkey, gcol_sb, gkey, out_sb, okey, ones32, ps_ss, sq, rstd, out_scaled_by_g=True):
    S = cx.S

    def do_tg(tg):
        tsl = slice(tg * TG, (tg + 1) * TG)
        for kc in range(KC):
            b = kc % 2
            S.op("act", (lambda e, kc=kc, b=b: e.activation(sq[b][:], x_sb[:, kc, tsl], AF.Square)),
                 reads=[(xkey, kc)], writes=[("sq", b)])
            S.op("pe", (lambda e, kc=kc, b=b: e.matmul(ps_ss[:], ones32[:], sq[b][:], start=(kc == 0), stop=(kc == KC - 1))),
                 reads=[("sq", b), "ones32"], writes=["ps_ss"])
        S.op("dve", lambda e: e.tensor_scalar(rstd[:], ps_ss[:], 1.0 / D, EPS, op0=ALU.mult, op1=ALU.add),
             reads=["ps_ss"], writes=["rstd"])
        S.op("act", lambda e: e.activation(rstd[:], rstd[:], AF.Sqrt),
             reads=["rstd"], writes=["rstd"])
        S.op("dve", lambda e: e.reciprocal(rstd[:], rstd[:]),
             reads=["rstd"], writes=["rstd"])
        for kc in range(KC):
            S.op("dve", (lambda e, kc=kc: e.scalar_tensor_tensor(out=out_sb[:, kc, tsl], in0=x_sb[:, kc, tsl], scalar=gcol_sb[:, kc:kc + 1],
                                                               in1=rstd[:], op0=ALU.mult, op1=ALU.mult)),
                 reads=[(xkey, kc), gkey, "rstd"], writes=[(okey, kc)])

    for tg in range(NTG):
        do_tg(tg)


IN_GROUPS = [
    ("aq", 0, 1536, "r128"), ("ak", 1536, 1536, "r128"), ("av", 3072, 1536, None),
    ("bq", 4608, 512, None), ("bk", 5120, 512, None), ("bv", 5632, 512, None),
    ("cq", 6148, 512, "r128"), ("ck", 6660, 512, "r128"), ("cv", 7172, 512, None),
    ("cqi", 7684, 1024, "r64"),
    ("dq", 8788, 512, "r128"), ("dkc", 9300, 128, "r128"), ("dvc", 9428, 128, None),
    ("dks", 9556, 128, "r128"), ("dvs", 9684, 128, None), ("dkw", 9812, 128, "r128"), ("dvw", 9940, 128, None),
]
LAST_COLS = list(range(8708, 8772)) + list(range(6144, 6148)) + list(range(8772, 8788)) + list(range(10068, 10080))


def in_col_perm():
    cols = []
    for (_, c0, n, _) in IN_GROUPS:
        cols += list(range(c0, c0 + n))
    cols += LAST_COLS
    assert len(cols) == 10080 and len(set(cols)) == 10080
    return np.array(cols)


def fm_row_offsets():
    off = {}
    r = 0
    for (name, c0, n, _) in IN_GROUPS:
        off[name] = (r, n)
        r += n
    return off, r


def p1_chunks():
    ch = []
    c = 0
    for (name, c0, n, rk) in IN_GROUPS:
        for i in range(n // 128):
            ch.append((c, 128, rk, "fm"))
            c += 128
    ch.append((c, 96, "rL", "sm"))
    return ch


def build_p1(nchunks=None, stage=9):
    nc = new_nc()
    with ExitStack() as st:
        cx = Ctx(nc, st)
        S = cx.S
        xT = cx.din("xT", [D, T], F32)
        gcol = cx.din("gcol", [128, KC], F32)
        w = cx.din("w", [D, 10080], F32)
        cs = cx.din("cs", [6, 128, T], F32)
        rmat = cx.din("rmat", [3, 128, 128], F32)
        fm = cx.dout("fm", [9984, T], BF16)
        sm = cx.dout("sm", [96, T], F32)
        hT = cx.dout("hT", [D, T], BF16)

        x_sb = cx.sb("x_sb", [128, KC, T], F32)
        h_sb = cx.sb("h_sb", [128, KC, T], BF16)
        g_sb = cx.sb("g_sb", [128, KC], F32)
        cs_sb = cx.sb("cs_sb", [128, 6, T], F32)
        r32 = cx.sb("r32", [128, 3, 128], F32)
        rbf = cx.sb("rbf", [128, 3, 128], BF16)
        ones32 = cx.sb("ones32", [128, 128], F32)
        sq = [cx.sb("sq%d" % i, [128, TG], F32) for i in range(2)]
        rstd = cx.sb("rstd", [128, TG], F32)
        ysb = [cx.sb("ysb%d" % i, [128, TG], BF16) for i in range(2)]
        t1 = [cx.sb("t1_%d" % i, [128, TG], F32) for i in range(2)]
        t2 = [cx.sb("t2_%d" % i, [128, TG], F32) for i in range(2)]
        obf = [cx.sb("obf%d" % i, [128, TG], BF16) for i in range(4)]
        o32 = [cx.sb("o32_%d" % i, [128, TG], F32) for i in range(2)]
        ps_y = [cx.ps("ps_y%d" % i, [128, TG], F32) for i in range(2)]
        ps_r = [cx.ps("ps_r%d" % i, [128, TG], F32) for i in range(2)]
        ps_ss = cx.ps("ps_ss", [128, TG], F32)
        ws = WStream(cx, nslots=4)

        for kc in range(KC):
            S.op("sp", (lambda e, kc=kc: e.dma_start(out=x_sb[:, kc, :], in_=xT[kc * 128:(kc + 1) * 128, :])),
                 writes=[("x", kc)], dsem=("x", kc))
        S.op("sp", lambda e: e.dma_start(out=g_sb[:], in_=gcol), writes=["g"], dsem="g")
        S.op("sp", lambda e: e.dma_start(out=cs_sb[:], in_=cs.rearrange("s p t -> p s t")), writes=["cs"], dsem="cs")
        S.op("sp", lambda e: e.dma_start(out=r32[:], in_=rmat.rearrange("s p t -> p s t")), writes=["r32"], dsem="r32")
        S.op("dve", lambda e: e.tensor_copy(rbf[:], r32[:]), reads=["r32"], writes=["rbf"])
        S.op("dve", lambda e: e.memset(ones32[:], 1.0), writes=["ones32"])

        if stage >= 2:
            rmsnorm_fm(cx, x_sb, "x", g_sb, "g", h_sb, "h", ones32, ps_ss, sq, rstd)
        else:
            for kc in range(KC):
                S.op("dve", (lambda e, kc=kc: e.tensor_copy(h_sb[:, kc, :], x_sb[:, kc, :])), reads=[("x", kc)], writes=[("h", kc)])
        hreads = [("h", kc) for kc in range(KC)]
        for kc in range(KC):
            S.op("sp", (lambda e, kc=kc: e.dma_start(out=hT[kc * 128:(kc + 1) * 128, :], in_=h_sb[:, kc, :])),
                 reads=[("h", kc)], writes=[("hT", kc)], dsem=("hTo", kc % 2))

        chunks = p1_chunks()
        if nchunks is not None:
            chunks = chunks[:nchunks] + chunks[-1:]
        if stage < 3:
            chunks = []
        if stage == 3:
            chunks = chunks[:-1]
        cnt = [0]

        def mk_unit(ci, c0, M, rk, okind):
            def load():
                return ws.load(w, 0, KC, c0, M)

            def compute(hd):
                bf, bkey = hd
                for tg in range(NTG):
                    do_tg(bf, bkey, tg)

            def do_tg(bf, bkey, tg):
                if True:
                    tsl = slice(tg * TG, (tg + 1) * TG)
                    k = cnt[0]
                    cnt[0] += 1
                    b = k % 2
                    py = ps_y[b]
                    for kc in range(KC):
                        S.op("pe", (lambda e, kc=kc, py=py: e.matmul(py[0:M, :], bf[:, kc, 0:M], h_sb[:, kc, tsl], start=(kc == 0), stop=(kc == KC - 1))),
                             reads=[bkey, ("h", kc)], writes=[("ps_y", b)])
                    if okind == "fm":
                        ob = obf[k % 4]
                        okey = ("obf", k % 4)
                    else:
                        ob = o32[b]
                        okey = ("o32", b)
                    if rk is None:
                        S.op("act", (lambda e, py=py, ob=ob: e.copy(ob[0:M, :], py[0:M, :])), reads=[("ps_y", b)], writes=[okey])
                    else:
                        ri = {"r128": 0, "r64": 1, "rL": 1}[rk]
                        ci_ = {"r128": 0, "r64": 2, "rL": 4}[rk]
                        pr = ps_r[b]
                        S.op("act", (lambda e, py=py: e.copy(ysb[b][0:M, :], py[0:M, :])), reads=[("ps_y", b)], writes=[("ysb", b)])
                        S.op("pe", (lambda e, pr=pr: e.matmul(pr[0:M, :], rbf[0:M, ri, 0:M], ysb[b][0:M, :], start=True, stop=True)),
                             reads=[("ysb", b), "rbf"], writes=[("ps_r", b)])
                        S.op("dve", (lambda e, py=py: e.tensor_tensor(t1[b][0:M, :], py[0:M, :], cs_sb[0:M, ci_, tsl], op=ALU.mult)),
                             reads=[("ps_y", b), "cs"], writes=[("t1", b)])
                        S.op("dve", (lambda e, pr=pr: e.tensor_tensor(t2[b][0:M, :], pr[0:M, :], cs_sb[0:M, ci_ + 1, tsl], op=ALU.mult)),
                             reads=[("ps_r", b), "cs"], writes=[("t2", b)])
                        S.op("pool", (lambda e, ob=ob: e.tensor_tensor(ob[0:M, :], t1[b][0:M, :], t2[b][0:M, :], op=ALU.add)),
                             reads=[("t1", b), ("t2", b)], writes=[okey])
                    if okind == "fm":
                        S.op("sp", (lambda e, ob=ob: e.dma_start(out=fm[c0:c0 + M, tsl], in_=ob[0:M, :])),
                             reads=[okey], writes=[("fm", c0, tg)], dsem=("fmo", k % 4))
                    else:
                        S.op("sp", (lambda e, ob=ob: e.dma_start(out=sm[0:M, tsl], in_=ob[0:M, :])),
                             reads=[okey], writes=[("sm", tg)], dsem=("smo", tg))
            return (load, compute)

        units = [mk_unit(i, *ch) for i, ch in enumerate(chunks)]
        pipeline(units, depth=2)
        evs = [v[0] for k, v in S.res.items() if isinstance(k, tuple) and k[0] in ("fm", "sm", "hT") and v[0] is not None]
        S.wait_all("sp", evs)
        S.emit()
    return nc


FF = 5632
FC = FF // 128
FCH = FC // 2


def rmsnorm_gen(cx, xv, xkey, gcol_sb, gkey, ones32, ps_ss, sq, sqkey, rstd, write_out):
    S = cx.S

    def do_tg(tg):
        tsl = slice(tg * TG, (tg + 1) * TG)
        for kc in range(KC):
            b = kc % 2
            S.op("act", (lambda e, kc=kc, b=b: e.activation(sq[b][:], xv(kc, tsl), AF.Square)),
                 reads=[(xkey, kc)], writes=[(sqkey, b)])
            S.op("pe", (lambda e, kc=kc, b=b: e.matmul(ps_ss[:], ones32[:], sq[b][:], start=(kc == 0), stop=(kc == KC - 1))),
                 reads=[(sqkey, b), "ones32"], writes=["ps_ss"])
        S.op("dve", lambda e: e.tensor_scalar(rstd[:], ps_ss[:], 1.0 / D, EPS, op0=ALU.mult, op1=ALU.add),
             reads=["ps_ss"], writes=["rstd"])
        S.op("act", lambda e: e.activation(rstd[:], rstd[:], AF.Sqrt), reads=["rstd"], writes=["rstd"])
        S.op("dve", lambda e: e.reciprocal(rstd[:], rstd[:]), reads=["rstd"], writes=["rstd"])
        for kc in range(KC):
            def emit(out_ap, okey, kc=kc):
                S.op("dve", (lambda e: e.scalar_tensor_tensor(out=out_ap, in0=xv(kc, tsl), scalar=gcol_sb[:, kc:kc + 1],
                                                              in1=rstd[:], op0=ALU.mult, op1=ALU.mult)),
                     reads=[(xkey, kc), gkey, "rstd"], writes=[okey])
            write_out(kc, tg, tsl, emit)

    for tg in range(NTG):
        do_tg(tg)


def build_p3():
    nc = new_nc()
    with ExitStack() as st:
        cx = Ctx(nc, st)
        S = cx.S
        xT = cx.din("xT", [D, T], F32)
        hT = cx.din("hT", [D, T], BF16)
        brT = cx.din("brT", [2048, T], BF16)
        wg = cx.din("wg", [D, 8192], F32)
        bg = cx.din("bg", [128, 64], F32)
        wb = cx.din("wb", [2048, D], F32)
        wo = cx.din("wo", [D, D], F32)
        gffn = cx.din("gffn", [128, KC], F32)
        wfi = cx.din("wfi", [D, 2 * FF], F32)
        wfo = cx.din("wfo", [FF, D], F32)
        gfin = cx.din("gfin", [128, KC], F32)
        x2T = cx.dout("x2T", [D, T], F32)
        onT = cx.dout("onT", [D, T], F32)

        R = cx.sb("R", [128, 24576], F32)
        Rb = R.bitcast(BF16)
        h1 = Rb[:, 0:16384].rearrange("p (k t) -> p k t", k=KC)
        br = Rb[:, 16384:32768].rearrange("p (k t) -> p k t", k=KC)
        mg = Rb[:, 32768:49152].rearrange("p (k t) -> p k t", k=KC)
        xs = R[:, 0:16384].rearrange("p (k t) -> p k t", k=KC)
        h2 = mg
        act = cx.sb("act", [128, FCH, T], BF16)
        tA = [[cx.sb("tA%d%d" % (i, j), [128, TG], F32) for j in range(2)] for i in range(2)]
        acc = [cx.sb("acc%d" % i, [128, TG], F32) for i in range(2)]
        tmp = [cx.sb("tmp%d" % i, [128, TG], F32) for i in range(2)]
        rstd = cx.sb("rstd", [128, TG], F32)
        ones32 = cx.sb("ones32", [128, 128], F32)
        bg_sb = cx.sb("bg_sb", [128, 64], F32)
        gf_sb = cx.sb("gf_sb", [128, KC], F32)
        gl_sb = cx.sb("gl_sb", [128, KC], F32)
        ps_a = [cx.ps("ps_a%d" % i, [128, TG], F32) for i in range(2)]
        ps_b = [cx.ps("ps_b%d" % i, [128, TG], F32) for i in range(2)]
        ps_ss = cx.ps("ps_ss", [128, TG], F32)
        ws = WStream(cx, nslots=3)

        S.op("dve", lambda e: e.memset(ones32[:], 1.0), writes=["ones32"])
        S.op("sp", lambda e: e.dma_start(out=bg_sb[:], in_=bg), writes=["bg"], dsem="bg")
        S.op("sp", lambda e: e.dma_start(out=gf_sb[:], in_=gffn), writes=["gf"], dsem="gf")
        S.op("sp", lambda e: e.dma_start(out=gl_sb[:], in_=gfin), writes=["gl"], dsem="gl")
        for kc in range(KC):
            S.op("sp", (lambda e, kc=kc: e.dma_start(out=h1[:, kc, :], in_=hT[kc * 128:(kc + 1) * 128, :])),
                 writes=[("h", kc)], dsem=("h", kc))
        for kc in range(KC):
            S.op("sp", (lambda e, kc=kc: e.dma_start(out=br[:, kc, :], in_=brT[kc * 128:(kc + 1) * 128, :])),
                 writes=[("br", kc)], dsem=("br", kc))

        def tsl_of(tg):
            return slice(tg * TG, (tg + 1) * TG)

        def mm_group(ps, pkey, bf, bkey, nk, M, rhs_fn, rkey_fn, first=True, last=True):
            for kc in range(nk):
                S.op("pe", (lambda e, kc=kc: e.matmul(ps[0:M, :], bf[:, kc, 0:M], rhs_fn(kc), start=(first and kc == 0), stop=(last and kc == nk - 1))),
                     reads=[bkey, rkey_fn(kc)], writes=[pkey])

        units = []

        def u_gate(dc, n):
            def load():
                return ws.load(wg, 0, KC, n * 2048 + dc * 128, 128)

            def compute(hd):
                bf, bkey = hd
                for tg in range(NTG):
                    tsl = tsl_of(tg)
                    mm_group(ps_a[tg], ("ps_a", tg), bf, bkey, KC, 128, (lambda kc, tsl=tsl: h1[:, kc, tsl]), (lambda kc: ("h", kc)))
                    gs = tA[tg][n % 2]
                    S.op("act", (lambda e, tg=tg, gs=gs: e.activation(gs[:], ps_a[tg][:], AF.Sigmoid, bias=bg_sb[:, n * 16 + dc:n * 16 + dc + 1], scale=1.0)),
                         reads=[("ps_a", tg), "bg"], writes=[("tA", tg, n % 2)])
            return (load, compute)

        def u_branch(dc, n):
            def load():
                return ws.load(wb, n * 512, 4, dc * 128, 128)

            def compute(hd):
                bf, bkey = hd
                for tg in range(NTG):
                    tsl = tsl_of(tg)
                    mm_group(ps_b[tg], ("ps_b", tg), bf, bkey, 4, 128, (lambda kc, tsl=tsl: br[:, n * 4 + kc, tsl]), (lambda kc: ("br", n * 4 + kc)))
                    gs = tA[tg][n % 2]
                    if n == 0:
                        S.op("dve", (lambda e, tg=tg, gs=gs: e.tensor_tensor(acc[tg][:], gs[:], ps_b[tg][:], op=ALU.mult)),
                             reads=[("tA", tg, n % 2), ("ps_b", tg)], writes=[("acc", tg)])
                    else:
                        S.op("dve", (lambda e, tg=tg, gs=gs: e.tensor_tensor(tmp[tg][:], gs[:], ps_b[tg][:], op=ALU.mult)),
                             reads=[("tA", tg, n % 2), ("ps_b", tg)], writes=[("tmp", tg)])
                        if n < 3:
                            S.op("pool", (lambda e, tg=tg: e.tensor_tensor(acc[tg][:], acc[tg][:], tmp[tg][:], op=ALU.add)),
                                 reads=[("tmp", tg)], writes=[("acc", tg)])
                        else:
                            S.op("pool", (lambda e, tg=tg, tsl=tsl: e.tensor_tensor(mg[:, dc, tsl], acc[tg][:], tmp[tg][:], op=ALU.add)),
                                 reads=[("tmp", tg), ("acc", tg)], writes=[("mg", dc)])
            return (load, compute)

        for dc in range(KC):
            for n in range(4):
                units.append(u_gate(dc, n))
                units.append(u_branch(dc, n))
        pipeline(units, depth=2)
        S.barrier()
        for kc in range(KC):
            S.op("sp", (lambda e, kc=kc: e.dma_start(out=xs[:, kc, :], in_=xT[kc * 128:(kc + 1) * 128, :])),
                 writes=[("x", kc)], dsem=("x", kc))

        def u_out(dc):
            def load():
                return ws.load(wo, 0, KC, dc * 128, 128)

            def compute(hd):
                bf, bkey = hd
                for tg in range(NTG):
                    tsl = tsl_of(tg)
                    mm_group(ps_a[tg], ("ps_a", tg), bf, bkey, KC, 128, (lambda kc, tsl=tsl: mg[:, kc, tsl]), (lambda kc: ("mg", kc)))
                    S.op("dve", (lambda e, tg=tg, tsl=tsl: e.tensor_tensor(xs[:, dc, tsl], xs[:, dc, tsl], ps_a[tg][:], op=ALU.add)),
                         reads=[("ps_a", tg)], writes=[("x", dc)])
            return (load, compute)

        pipeline([u_out(dc) for dc in range(KC)], depth=2)
        S.barrier()
        def wr_h2(kc, tg, tsl, emit):
            emit(h2[:, kc, tsl], ("h2", kc))

        rmsnorm_gen(cx, (lambda kc, tsl: xs[:, kc, tsl]), "x", gf_sb, "gf", ones32, ps_ss, tmp, "tmp", rstd, wr_h2)

        for hh in range(2):
            units = []

            def u_ffn_in(fcl, which, hh=hh):
                col0 = which * FF + (hh * FCH + fcl) * 128

                def load():
                    return ws.load(wfi, 0, KC, col0, 128)

                def compute(hd):
                    bf, bkey = hd
                    for tg in range(NTG):
                        tsl = tsl_of(tg)
                        if which == 0:
                            mm_group(ps_a[tg], ("ps_a", tg), bf, bkey, KC, 128, (lambda kc, tsl=tsl: h2[:, kc, tsl]), (lambda kc: ("h2", kc)))
                            sg = tA[tg][fcl % 2]
                            S.op("act", (lambda e, tg=tg, sg=sg: e.activation(sg[:], ps_a[tg][:], AF.Silu)),
                                 reads=[("ps_a", tg)], writes=[("tA", tg, fcl % 2)])
                        else:
                            mm_group(ps_b[tg], ("ps_b", tg), bf, bkey, KC, 128, (lambda kc, tsl=tsl: h2[:, kc, tsl]), (lambda kc: ("h2", kc)))
                            sg = tA[tg][fcl % 2]
                            S.op("dve", (lambda e, tg=tg, sg=sg, tsl=tsl: e.tensor_tensor(act[:, fcl, tsl], sg[:], ps_b[tg][:], op=ALU.mult)),
                                 reads=[("tA", tg, fcl % 2), ("ps_b", tg)], writes=[("act", fcl)])
                return (load, compute)

            def u_ffn_out(dc, part, hh=hh):
                k0 = 0 if part == 0 else 16
                nk = 16 if part == 0 else FCH - 16

                def load():
                    return ws.load(wfo, (hh * FCH + k0) * 128, nk, dc * 128, 128)

                def compute(hd):
                    bf, bkey = hd
                    for tg in range(NTG):
                        tsl = tsl_of(tg)
                        mm_group(ps_a[tg], ("ps_a", tg), bf, bkey, nk, 128, (lambda kc, tsl=tsl: act[:, k0 + kc, tsl]), (lambda kc: ("act", k0 + kc)),
                                 first=(part == 0), last=(part == 1))
                        if part == 1:
                            S.op("dve", (lambda e, tg=tg, tsl=tsl: e.tensor_tensor(xs[:, dc, tsl], xs[:, dc, tsl], ps_a[tg][:], op=ALU.add)),
                                 reads=[("ps_a", tg)], writes=[("x", dc)])
                return (load, compute)

            for fcl in range(FCH):
                units.append(u_ffn_in(fcl, 0))
                units.append(u_ffn_in(fcl, 1))
            for dc in range(KC):
                units.append(u_ffn_out(dc, 0))
                units.append(u_ffn_out(dc, 1))
            pipeline(units, depth=2)

        for kc in range(KC):
            S.op("sp", (lambda e, kc=kc: e.dma_start(out=x2T[kc * 128:(kc + 1) * 128, :], in_=xs[:, kc, :])),
                 reads=[("x", kc)], writes=[("x2T", kc)], dsem=("x2o", kc % 2))
        ocnt = [0]

        def wr_fin(kc, tg, tsl, emit):
            k = ocnt[0]
            ocnt[0] += 1
            ob = tA[k % 2][(k // 2) % 2]
            okey = ("tA", k % 2, (k // 2) % 2)
            emit(ob[:], okey)
            S.op("sp", (lambda e: e.dma_start(out=onT[kc * 128:(kc + 1) * 128, tsl], in_=ob[:])),
                 reads=[okey], writes=[("onT", kc, tg)], dsem=("ono", k % 4))

        rmsnorm_gen(cx, (lambda kc, tsl: xs[:, kc, tsl]), "x", gl_sb, "gl", ones32, ps_ss, tmp, "tmp", rstd, wr_fin)
        evs = [v[0] for k, v in S.res.items() if isinstance(k, tuple) and k[0] in ("x2T", "onT") and v[0] is not None]
        S.wait_all("sp", evs)
        S.emit()
    return nc


NSLOT = 8
ATTN_SCALE = 128 ** -0.5
NEG = -1e30


def slot_tile(c, j):
    return 16 * (j // 2) + (c if j % 2 == 0 else 15 - c)


class Attn:
    def __init__(self, cx):
        self.cx = cx
        S = cx.S
        self.ps_s = [cx.ps("ps_s%d" % i, [128, 512], F32) for i in range(2)]
        self.acc = [cx.ps("ps_acc%d" % i, [128, 512], F32) for i in range(2)]
        self.pe_buf = [cx.sb("pe_buf%d" % i, [128, 512], BF16) for i in range(3)]
        self.pt_buf = [cx.sb("pt_buf%d" % i, [128, 512], BF16) for i in range(3)]
        self.rs = cx.sb("rs_t", [128, 4], F32)
        self.osb = [cx.sb("osb%d" % i, [128, 512], BF16) for i in range(2)]
        self.n = 0
        self.no = 0
        self.acc_started = [False, False]

    def begin_acc(self):
        self.acc_started = [False, False]

    def scores(self, mms):
        S = self.cx.S
        b = self.n % 2
        ps = self.ps_s[b]
        for i, (c0, ncol, lhsT, rhs, reads) in enumerate(mms):
            S.op("pe", (lambda e, i=i, c0=c0, ncol=ncol, lhsT=lhsT, rhs=rhs: e.matmul(ps[:, c0:c0 + ncol], lhsT, rhs, start=(i == 0), stop=(i == len(mms) - 1), skip_group_check=True)),
                 reads=reads, writes=[("ps_s", b)])
        return b

    def exp(self, b, biases=None, breads=()):
        S = self.cx.S
        k = self.n % 3
        ps = self.ps_s[b]
        buf = self.pe_buf[k]
        if biases is None:
            S.op("act", (lambda e: e.activation(buf[:], ps[:], AF.Exp, scale=ATTN_SCALE)), reads=[("ps_s", b)], writes=[("pe_buf", k)])
        else:
            for h in range(4):
                S.op("act", (lambda e, h=h: e.activation(buf[:, h * 128:(h + 1) * 128], ps[:, h * 128:(h + 1) * 128], AF.Exp, bias=biases[h], scale=ATTN_SCALE)),
                     reads=[("ps_s", b)] + list(breads), writes=[("pe_buf", k)])
        return k

    def mask_stt(self, k, in0, scalar, op0, reads, inplace_pt=False):
        S = self.cx.S
        src = self.pt_buf[k] if inplace_pt else self.pe_buf[k]
        skey = ("pt_buf", k) if inplace_pt else ("pe_buf", k)
        dst = self.pt_buf[k]
        S.op("dve", (lambda e: e.scalar_tensor_tensor(out=dst[:].rearrange("p (h q) -> p h q", h=4), in0=in0.unsqueeze(1).to_broadcast([128, 4, 128]),
                                                      scalar=scalar, in1=src[:].rearrange("p (h q) -> p h q", h=4), op0=op0, op1=ALU.mult)),
             reads=[skey] + list(reads), writes=[("pt_buf", k)])

    def mask_tt(self, k, mask_ap, reads, eng="dve"):
        S = self.cx.S
        src = self.pe_buf[k]
        dst = self.pt_buf[k]
        S.op(eng, (lambda e: e.tensor_tensor(dst[:].rearrange("p (h q) -> p h q", h=4), src[:].rearrange("p (h q) -> p h q", h=4),
                                             mask_ap.unsqueeze(1).to_broadcast([128, 4, 128]), op=ALU.mult)),
             reads=[("pe_buf", k)] + list(reads), writes=[("pt_buf", k)])

    def pv(self, k, masked, v_aps, vreads, extra=None):
        S = self.cx.S
        buf = self.pt_buf[k] if masked else self.pe_buf[k]
        bkey = ("pt_buf", k) if masked else ("pe_buf", k)
        for h in range(4):
            a = h // 2
            acc = self.acc[a]
            st_ = not self.acc_started[a]
            self.acc_started[a] = True
            c0 = (h % 2) * 129
            S.op("pe", (lambda e, h=h, acc=acc, st_=st_, c0=c0: e.matmul(acc[:, c0:c0 + 129], buf[:, h * 128:(h + 1) * 128], v_aps[h], start=st_, stop=False, skip_group_check=True)),
                 reads=[bkey] + list(vreads), writes=[("ps_acc", a)])
            if extra is not None:
                pst, pkey, rhs, rreads, started = extra
                S.op("pe", (lambda e, h=h, fs=(not started[0]): e.matmul(pst[:, h * 128:(h + 1) * 128], buf[:, h * 128:(h + 1) * 128], rhs, start=fs, stop=False, skip_group_check=True)),
                     reads=[bkey] + list(rreads), writes=[pkey])
                started[0] = True
        self.n += 1

    def recip(self):
        S = self.cx.S
        for h in range(4):
            a = h // 2
            c0 = (h % 2) * 129 + 128
            S.op("dve", (lambda e, h=h, a=a, c0=c0: e.tensor_scalar(self.rs[:, h:h + 1], self.acc[a][:, c0:c0 + 1], 1e-30, None, op0=ALU.max)),
                 reads=[("ps_acc", a)], writes=["rs_t"])
        S.op("dve", (lambda e: e.reciprocal(self.rs[:], self.rs[:])), reads=["rs_t"], writes=["rs_t"])

    def finish_simple(self, out_ap, okey):
        S = self.cx.S
        self.recip()
        o = self.no % 2
        self.no += 1
        ob = self.osb[o]
        for h in range(4):
            a = h // 2
            c0 = (h % 2) * 129
            S.op("dve", (lambda e, h=h, a=a, c0=c0: e.tensor_scalar(ob[:, h * 128:(h + 1) * 128], self.acc[a][:, c0:c0 + 128], self.rs[:, h:h + 1], None, op0=ALU.mult)),
                 reads=[("ps_acc", a), "rs_t"], writes=[("osb", o)])
        S.op("sp", (lambda e: e.dma_start(out=out_ap, in_=ob[:])), reads=[("osb", o)], writes=[okey], dsem=("oo", o))


def load_consts(cx, names):
    S = cx.S
    out = {}
    for (name, shape, dt) in names:
        d = cx.din(name, shape, dt)
        t = cx.sb(name + "_sb", shape, dt)
        S.op("sp", (lambda e, d=d, t=t: e.dma_start(out=t[:], in_=d)), writes=[name], dsem=name)
        out[name] = t
    return out


def finish_prog(cx, prefixes):
    S = cx.S
    evs = [v[0] for k, v in S.res.items() if isinstance(k, tuple) and k[0] in prefixes and v[0] is not None]
    S.wait_all("sp", evs)
    S.emit()


def build_p2b(nslot=NSLOT):
    nc = new_nc()
    with ExitStack() as st:
        cx = Ctx(nc, st)
        S = cx.S
        QT = cx.din("QT", [128, NSLOT, 4, 128], BF16)
        KT = cx.din("KT", [128, 4, 8192], BF16)
        V = cx.din("V", [128, 64, 4, 129], BF16)
        oB = cx.dout("oB", [T, 512], BF16)
        C = load_consts(cx, [("flog", [128, 4, 64], F32), ("bf_rep", [128, 4], F32), ("oh", [128, NSLOT, 64], F32),
                             ("qrel_rep", [128, T], F32), ("kcol_rel", [128, 8], F32), ("tri32", [128, 128], F32)])
        q_sb = cx.sb("q_sb", [128, NSLOT, 4, 128], BF16)
        k_sb = cx.sb("k_sb", [128, 4, 8192], BF16)
        v_sb = cx.sb("v_sb", [128, 64, 4, 129], BF16)
        S.op("sp", lambda e: e.dma_start(out=q_sb[:], in_=QT), writes=["q"], dsem="q")
        for h in range(4):
            S.op("sp", (lambda e, h=h: e.dma_start(out=k_sb[:, h, :], in_=KT[:, h, :])), writes=[("k", h)], dsem=("k", h))
        for t8 in range(8):
            S.op("sp", (lambda e, t8=t8: e.dma_start(out=v_sb[:, t8 * 8:(t8 + 1) * 8], in_=V[:, t8 * 8:(t8 + 1) * 8])), writes=[("v", t8)], dsem=("v", t8))
        at = Attn(cx)
        ones32 = cx.sb("ones32", [128, 128], F32)
        S.op("dve", lambda e: e.memset(ones32[:], 1.0), writes=["ones32"])
        xl = cx.sb("xl", [128, 4, 64], F32)
        tot = cx.sb("tot", [128, 4, 64], F32)
        inc = cx.sb("inc", [128, 4, 64], F32)
        off = cx.sb("off", [128, 4, 64], F32)
        Lk = cx.sb("Lk", [128, 4, 64], F32)
        zer = cx.sb("zer", [128, 64], F32)
        t64 = cx.sb("t64", [128, 64], F32)
        lref = cx.sb("lref", [128, NSLOT * 4], F32)
        bb = cx.sb("bb", [128, NSLOT, 4, 64], F32)
        S.op("dve", lambda e: e.memset(zer[:], 0.0), writes=["zer"])
        for h in range(4):
            S.op("dve", (lambda e, h=h: e.tensor_scalar(xl[:, h, :], C["flog"][:, h, :], C["bf_rep"][:, h:h + 1], None, op0=ALU.add)),
                 reads=["flog", "bf_rep"], writes=["xl"])
        S.op("act", lambda e: e.activation(xl[:], xl[:], AF.Exp, scale=-1.0), reads=["xl"], writes=["xl"])
        S.op("act", lambda e: e.activation(xl[:], xl[:], AF.Ln, bias=1.0, scale=1.0), reads=["xl"], writes=["xl"])
        psw = at.ps_s[0]
        pst = at.ps_s[1]
        xl2 = xl[:].rearrange("p h t -> p (h t)")
        S.op("pe", lambda e: e.matmul(psw[:, 0:256], C["tri32"][:], xl2, start=True, stop=True), reads=["xl", "tri32"], writes=[("ps_s", 0)])
        S.op("pe", lambda e: e.matmul(pst[:, 0:256], ones32[:], xl2, start=True, stop=True), reads=["xl", "ones32"], writes=[("ps_s", 1)])
        S.op("dve", lambda e: e.tensor_copy(tot[:].rearrange("p h t -> p (h t)"), pst[:, 0:256]), reads=[("ps_s", 1)], writes=["tot"])
        for h in range(4):
            S.op("dve", (lambda e, h=h: e.tensor_tensor_scan(inc[:, h, :], tot[:, h, :], zer[:], 0.0, op0=ALU.add, op1=ALU.add)),
                 reads=["tot", "zer"], writes=["inc"])
        S.op("dve", lambda e: e.tensor_tensor(off[:], inc[:], tot[:], op=ALU.subtract), reads=["inc", "tot"], writes=["off"])
        S.op("dve", lambda e: e.tensor_tensor(Lk[:].rearrange("p h t -> p (h t)"), psw[:, 0:256], off[:].rearrange("p h t -> p (h t)"), op=ALU.add),
             reads=[("ps_s", 0), "off"], writes=["Lk"])
        for j in range(NSLOT):
            for h in range(4):
                S.op("dve", (lambda e, j=j, h=h: e.tensor_tensor(t64[:], C["oh"][:, j, :], off[:, h, :], op=ALU.mult)), reads=["oh", "off"], writes=["t64"])
                S.op("dve", (lambda e, j=j, h=h: e.reduce_sum(lref[:, j * 4 + h:j * 4 + h + 1], t64[:], axis=AX.X)), reads=["t64"], writes=["lref"])
        for j in range(NSLOT):
            for h in range(4):
                S.op("dve", (lambda e, j=j, h=h: e.tensor_scalar(bb[:, j, h, :], Lk[:, h, :], lref[:, j * 4 + h:j * 4 + h + 1], 30.0, op0=ALU.subtract, op1=ALU.min)),
                     reads=["Lk", "lref"], writes=["bb"])
        for j in range(nslot):
            at.begin_acc()
            nkt = 8 * (j + 1)
            for kt in range(nkt):
                mms = [(h * 128, 128, k_sb[:, h, kt * 128:(kt + 1) * 128], q_sb[:, j, h, :], [("k", h), "q"]) for h in range(4)]
                b = at.scores(mms)
                k = at.exp(b, biases=[bb[:, j, h, kt:kt + 1] for h in range(4)], breads=["bb"])
                masked = kt >= 8 * j
                if masked:
                    at.mask_stt(k, C["qrel_rep"][:, j * 128:(j + 1) * 128], C["kcol_rel"][:, kt - 8 * j:kt - 8 * j + 1], ALU.is_ge, ["qrel_rep", "kcol_rel"])
                at.pv(k, masked, [v_sb[:, kt, h, :] for h in range(4)], [("v", kt // 8)])
            at.finish_simple(oB[j * 128:(j + 1) * 128, :], ("oB", j))
        finish_prog(cx, ("oB",))
    return nc


class TileStream:
    def __init__(self, cx, name, shape, dt, nslots):
        self.cx = cx
        self.name = name
        self.n = nslots
        self.i = 0
        self.bufs = [cx.sb("%s%d" % (name, i), shape, dt) for i in range(nslots)]

    def load(self, src_ap):
        S = self.cx.S
        s = self.i % self.n
        self.i += 1
        buf = self.bufs[s]
        key = (self.name, s)
        S.op("sp", (lambda e: e.dma_start(out=buf[:], in_=src_ap)), writes=[key], dsem=key)
        return buf, key


A_TILES = [(0, w) for w in range(2)] + [(1, w) for w in range(5)] + [(2, w) for w in range(17)]


def build_p2a(nslot=NSLOT):
    nc = new_nc()
    with ExitStack() as st:
        cx = Ctx(nc, st)
        S = cx.S
        QT = cx.din("QT", [128, NSLOT, 12, 128], BF16)
        KT = cx.din("KT", [NSLOT, 24, 128, 4, 128], BF16)
        V = cx.din("V", [NSLOT, 24, 128, 4, 129], BF16)
        oA = cx.dout("oA", [T, 512], BF16)
        C = load_consts(cx, [("amask", [128, 24, 128], BF16)])
        q_sb = cx.sb("q_sb", [128, NSLOT, 12, 128], BF16)
        S.op("sp", lambda e: e.dma_start(out=q_sb[:], in_=QT), writes=["q"], dsem="q")
        at = Attn(cx)
        ks = TileStream(cx, "kst", [128, 4, 128], BF16, 6)
        vs = TileStream(cx, "vst", [128, 4, 129], BF16, 6)
        units = []

        def mk(j, ti):
            g, w = A_TILES[ti]

            def load():
                return (ks.load(KT[j, ti]), vs.load(V[j, ti]))

            def compute(hd):
                (kb, kkey), (vb, vkey) = hd
                if ti == 0:
                    at.begin_acc()
                mms = [(h * 128, 128, kb[:, h, :], q_sb[:, j, g * 4 + h, :], [kkey, "q"]) for h in range(4)]
                b = at.scores(mms)
                k = at.exp(b)
                at.mask_tt(k, C["amask"][:, ti, :], ["amask"])
                at.pv(k, True, [vb[:, h, :] for h in range(4)], [vkey])
                if ti == 23:
                    at.finish_simple(oA[j * 128:(j + 1) * 128, :], ("oA", j))
            return (load, compute)

        for j in range(nslot):
            for ti in range(24):
                units.append(mk(j, ti))
        pipeline(units, depth=4)
        finish_prog(cx, ("oA",))
    return nc


DSA_TOPK = 256


def build_p2c(nslot=NSLOT, topk_rounds=DSA_TOPK // 8):
    nc = new_nc()
    with ExitStack() as st:
        cx = Ctx(nc, st)
        S = cx.S
        QT = cx.din("QT", [128, NSLOT, 4, 128], BF16)
        KT = cx.din("KT", [64, 128, 4, 128], BF16)
        V = cx.din("V", [64, 128, 4, 129], BF16)
        QiT = cx.din("QiT", [128, NSLOT, 8, 128], BF16)
        KiT2 = cx.din("KiT2", [128, 8192], F32)
        oC = cx.dout("oC", [T, 512], BF16)
        C = load_consts(cx, [("wi", [128, NSLOT, 16], F32), ("qrel_col", [128, NSLOT], F32), ("iota_row", [128, 1024], F32),
                             ("ident_bf", [128, 128], BF16)])
        q_sb = cx.sb("q_sb", [128, NSLOT, 4, 128], BF16)
        qi_sb = cx.sb("qi_sb", [128, NSLOT, 8, 128], BF16)
        ki_sb = cx.sb("ki_sb", [128, 8192], BF16)
        S.op("sp", lambda e: e.dma_start(out=q_sb[:], in_=QT), writes=["q"], dsem="q")
        S.op("sp", lambda e: e.dma_start(out=qi_sb[:], in_=QiT), writes=["qi"], dsem="qi")
        score = cx.sb("score", [128, 8192], F32)
        for i in range(4):
            S.op("sp", (lambda e, i=i: e.dma_start(out=score[:, i * 2048:(i + 1) * 2048], in_=KiT2[:, i * 2048:(i + 1) * 2048])),
                 writes=[("score", 4 * i + k_) for k_ in range(4)], dsem=("ki", i))
            S.op("pool", (lambda e, i=i: e.tensor_copy(ki_sb[:, i * 2048:(i + 1) * 2048], score[:, i * 2048:(i + 1) * 2048])),
                 reads=[("score", 4 * i + k_) for k_ in range(4)], writes=[("ki", i)])
        at = Attn(cx)
        ks = TileStream(cx, "kst", [128, 4, 128], BF16, 6)
        vs = TileStream(cx, "vst", [128, 4, 129], BF16, 6)
        maskqk = [cx.sb("maskqk%d" % i, [128, 8192], BF16) for i in range(2)]
        rbuf = [cx.sb("rbuf%d" % i, [128, 512], F32) for i in range(2)]
        pen = cx.sb("pen", [128, 1024], F32)
        c01 = cx.sb("c01", [128, 1024], BF16)
        m8 = cx.sb("m8", [128, 8], F32)
        ps_m = [cx.ps("ps_m%d" % i, [128, 128], BF16) for i in range(2)]
        cnt = [0]
        tcnt = [0]

        def indexer(j):
            N = 1024 * (j + 1)
            mq = maskqk[j % 2]
            mkey = ("maskqk", j % 2)
            def do_head(ch, hh):
                    csl = slice(ch * 512, (ch + 1) * 512)
                    c8, half = hh // 2, hh % 2
                    rows = slice(half * 64, half * 64 + 64)
                    b = cnt[0] % 2
                    cnt[0] += 1
                    ps = at.ps_s[b]
                    S.op("pe", (lambda e, ps=ps, c8=c8, rows=rows: e.matmul(ps[:], qi_sb[rows, j, c8, :], ki_sb[rows, csl], start=True, stop=True)),
                         reads=["qi", ("ki", ch // 4)], writes=[("ps_s", b)])
                    S.op("act", (lambda e, ps=ps, b=b: e.activation(rbuf[b][:], ps[:], AF.Relu)), reads=[("ps_s", b)], writes=[("rbuf", b)])
                    if hh == 0:
                        S.op("dve", (lambda e, b=b: e.tensor_scalar(score[:, csl], rbuf[b][:], C["wi"][:, j, 0:1], None, op0=ALU.mult)),
                             reads=[("rbuf", b), "wi"], writes=[("score", ch)])
                    else:
                        S.op("dve", (lambda e, b=b, hh=hh: e.scalar_tensor_tensor(out=score[:, csl], in0=rbuf[b][:], scalar=C["wi"][:, j, hh:hh + 1], in1=score[:, csl],
                                                                                  op0=ALU.mult, op1=ALU.add)),
                             reads=[("rbuf", b), "wi"], writes=[("score", ch)])

            for ch in range(N // 512):
                for hh in range(16):
                    do_head(ch, hh)
            allsc = [("score", ch) for ch in range(N // 512)]
            lsl = slice(1024 * j, 1024 * (j + 1))
            S.op("dve", (lambda e: e.tensor_scalar(pen[:], C["iota_row"][:], C["qrel_col"][:, j:j + 1], NEG, op0=ALU.is_gt, op1=ALU.mult)),
                 reads=["iota_row", "qrel_col"], writes=["pen"])
            S.op("dve", (lambda e: e.tensor_tensor(score[:, lsl], score[:, lsl], pen[:], op=ALU.add)), reads=["pen"], writes=[("score", 2 * j), ("score", 2 * j + 1)])
            for r in range(topk_rounds):
                S.op("dve", (lambda e: e.max(out=m8[:], in_=score[:, 0:N])), reads=allsc, writes=["m8"])
                S.op("dve", (lambda e: e.match_replace(out=score[:, 0:N], in_to_replace=m8[:], in_values=score[:, 0:N], imm_value=-3e38)),
                     reads=["m8"], writes=allsc)
            S.op("dve", (lambda e: e.tensor_scalar(mq[:, 0:N], score[:, 0:N], -2e38, None, op0=ALU.is_le)), reads=allsc, writes=[mkey])
            S.op("dve", (lambda e: e.tensor_scalar(c01[:], C["iota_row"][:], C["qrel_col"][:, j:j + 1], None, op0=ALU.is_le)),
                 reads=["iota_row", "qrel_col"], writes=["c01"])
            S.op("dve", (lambda e: e.tensor_tensor(mq[:, lsl], mq[:, lsl], c01[:], op=ALU.mult)), reads=["c01"], writes=[mkey])

        def mk(j, kt):
            def load():
                return (ks.load(KT[kt]), vs.load(V[kt]))

            def compute(hd):
                (kb, kkey), (vb, vkey) = hd
                mq = maskqk[j % 2]
                mkey = ("maskqk", j % 2)
                if kt == 0:
                    at.begin_acc()
                mms = [(h * 128, 128, kb[:, h, :], q_sb[:, j, h, :], [kkey, "q"]) for h in range(4)]
                b = at.scores(mms)
                k = at.exp(b)
                tb = tcnt[0] % 2
                tcnt[0] += 1
                S.op("pe", (lambda e: e.transpose(ps_m[tb][:], mq[:, kt * 128:(kt + 1) * 128], C["ident_bf"][:])),
                     reads=[mkey, "ident_bf"], writes=[("ps_m", tb)])
                at.mask_tt(k, ps_m[tb][:], [("ps_m", tb)])
                at.pv(k, True, [vb[:, h, :] for h in range(4)], [vkey])
                if kt == 8 * (j + 1) - 1:
                    at.finish_simple(oC[j * 128:(j + 1) * 128, :], ("oC", j))
            return (load, compute)

        for j in range(nslot):
            indexer(j)
            units = [mk(j, kt) for kt in range(8 * (j + 1))]
            pipeline(units, depth=4)
        finish_prog(cx, ("oC",))
    return nc


GELU_C = 0.7978845608028654


def build_p2d(nslot=NSLOT, debug=False):
    nc = new_nc()
    with ExitStack() as st:
        cx = Ctx(nc, st)
        S = cx.S
        QT = cx.din("QT", [128, NSLOT, 4, 128], BF16)
        KcT = cx.din("KcT", [128, 8192], BF16)
        VcT = cx.din("VcT", [128, 8192], BF16)
        KsT = cx.din("KsT", [128, 8192], BF16)
        Vs = cx.din("Vs", [128, 64, 129], BF16)
        KwT = cx.din("KwT", [128, NSLOT, 640], BF16)
        Vw = cx.din("Vw", [128, NSLOT, 5, 129], BF16)
        w1 = cx.din("w1", [2, 4096, 256], F32)
        w2 = cx.din("w2", [2, 256, 128], F32)
        oD = cx.dout("oD", [T, 512], BF16)
        C = load_consts(cx, [("dwmask", [128, 5, 128], BF16), ("dg", [128, NSLOT, 12], F32), ("peT", [128, 2, 32], F32),
                             ("qpos_rep", [128, T], F32), ("qrel_rep", [128, T], F32), ("qpos_col", [128, NSLOT], F32),
                             ("kcol_rel", [128, 8], F32), ("cend_col", [128, 4], F32), ("jstart_row", [128, 128], F32),
                             ("j0row", [128, 128], F32), ("G", [128, 8192], BF16), ("ovl", [128, 4, 128], BF16),
                             ("ident_bf", [128, 128], BF16)])
        q_sb = cx.sb("q_sb", [128, NSLOT, 4, 128], BF16)
        xc = [cx.sb("xc%d" % i, [128, 8192], BF16) for i in range(2)]
        ks_sb = cx.sb("ks_sb", [128, 8192], BF16)
        vs_sb = cx.sb("vs_sb", [128, 64, 129], BF16)
        kw_sb = cx.sb("kw_sb", [128, NSLOT, 640], BF16)
        vw_sb = cx.sb("vw_sb", [128, NSLOT, 5, 129], BF16)
        S.op("sp", lambda e: e.dma_start(out=q_sb[:], in_=QT), writes=["q"], dsem="q")
        S.op("sp", lambda e: e.dma_start(out=xc[0][:], in_=KcT), writes=[("xc", 0)], dsem=("xc", 0))
        S.op("sp", lambda e: e.dma_start(out=xc[1][:], in_=VcT), writes=[("xc", 1)], dsem=("xc", 1))
        S.op("sp", lambda e: e.dma_start(out=ks_sb[:], in_=KsT), writes=["ks"], dsem="ks")
        for t8 in range(8):
            S.op("sp", (lambda e, t8=t8: e.dma_start(out=vs_sb[:, t8 * 8:(t8 + 1) * 8], in_=Vs[:, t8 * 8:(t8 + 1) * 8])), writes=[("vs", t8)], dsem=("vs", t8))
        S.op("sp", lambda e: e.dma_start(out=kw_sb[:], in_=KwT), writes=["kw"], dsem="kw")
        S.op("sp", lambda e: e.dma_start(out=vw_sb[:], in_=Vw), writes=["vw"], dsem="vw")
        at = Attn(cx)
        ps_imp = cx.ps("ps_imp", [128, 512], F32)
        ps_m = cx.ps("ps_m", [128, 128], BF16)
        ps_m2 = [cx.ps("ps_m2%d" % i, [128, 128], F32) for i in range(2)]

        w1s = cx.sb("w1s", [128, 32, 256], F32)
        w1b = cx.sb("w1b", [128, 32, 256], BF16)
        w2s = cx.sb("w2s", [128, 2, 128], F32)
        w2b = cx.sb("w2b", [128, 2, 128], BF16)
        peb = cx.sb("peb", [128, 2, 32], BF16)
        bias_sb = cx.sb("bias_sb", [128, 2], F32)
        u_sb = cx.sb("u_sb", [128, 512], F32)
        z_sb = cx.sb("z_sb", [128, 512], F32)
        gT = [cx.sb("gT%d" % i, [128, 512], BF16) for i in range(2)]
        kcmp = cx.sb("kcmp", [128, 512], BF16)
        vcmp = cx.sb("vcmp", [128, 4, 129], BF16)
        S.op("dve", lambda e: e.tensor_copy(peb[:], C["peT"][:]), reads=["peT"], writes=["peb"])
        S.op("dve", lambda e: e.memset(kcmp[:], 0.0), writes=["kcmp"])
        S.op("dve", lambda e: e.memset(vcmp[:], 0.0), writes=["vcmp"])
        S.op("dve", lambda e: e.memset(vcmp[:, :, 128:129], 1.0), writes=["vcmp"])
        for i in range(2):
            S.op("dve", (lambda e, i=i: e.memset(gT[i][:], 0.0)), writes=[("gT", i)])

        def compress(which):
            src1 = w1[which].rearrange("(l d) c -> d l c", d=128)
            for l8 in range(4):
                S.op("sp", (lambda e, l8=l8: e.dma_start(out=w1s[:, l8 * 8:(l8 + 1) * 8, :], in_=src1[:, l8 * 8:(l8 + 1) * 8, :])),
                     writes=[("w1s", l8)], dsem=("w1s", l8))
                S.op("pool", (lambda e, l8=l8: e.tensor_copy(w1b[:, l8 * 8:(l8 + 1) * 8, :], w1s[:, l8 * 8:(l8 + 1) * 8, :])),
                     reads=[("w1s", l8)], writes=[("w1b", l8)])
            S.op("sp", lambda e: e.dma_start(out=w2s[:], in_=w2[which].rearrange("(cc c) d -> c cc d", c=128)), writes=["w2s"], dsem="w2s")
            S.op("pool", lambda e: e.tensor_copy(w2b[:], w2s[:]), reads=["w2s"], writes=["w2b"])
            xv = xc[which][:].rearrange("p (n s) -> p n s", s=16)
            ph0 = at.ps_s[0]
            pb = at.ps_s[1]
            for cc in range(2):
                for l in range(32):
                    S.op("pe", (lambda e, l=l, cc=cc: e.matmul(ph0[:, 0:511], w1b[:, l, cc * 128:(cc + 1) * 128], xv[:, l // 16:l // 16 + 511, l % 16],
                                                              start=(l == 0), stop=(l == 31))),
                         reads=[("w1b", l // 8), ("xc", which)], writes=[("ps_s", 0)])
                for l in range(32):
                    S.op("pe", (lambda e, l=l, cc=cc: e.matmul(pb[:, 0:1], w1b[:, l, cc * 128:(cc + 1) * 128], peb[:, which, l:l + 1],
                                                              start=(l == 0), stop=(l == 31))),
                         reads=[("w1b", l // 8), "peb"], writes=[("ps_s", 1)])
                S.op("dve", (lambda e, cc=cc: e.tensor_copy(bias_sb[:, cc:cc + 1], pb[:, 0:1])), reads=[("ps_s", 1)], writes=["bias_sb"])
                S.op("act", (lambda e, cc=cc: e.activation(u_sb[:, 0:511], ph0[:, 0:511], AF.Identity, bias=bias_sb[:, cc:cc + 1], scale=1.0)),
                     reads=[("ps_s", 0), "bias_sb"], writes=["u_sb"])
                S.op("dve", lambda e: e.tensor_tensor(z_sb[:, 0:511], u_sb[:, 0:511], u_sb[:, 0:511], op=ALU.mult), reads=["u_sb"], writes=["z_sb"])
                S.op("dve", lambda e: e.tensor_scalar(z_sb[:, 0:511], z_sb[:, 0:511], 0.044715, 1.0, op0=ALU.mult, op1=ALU.add), reads=["z_sb"], writes=["z_sb"])
                S.op("dve", lambda e: e.tensor_tensor(z_sb[:, 0:511], z_sb[:, 0:511], u_sb[:, 0:511], op=ALU.mult), reads=["z_sb", "u_sb"], writes=["z_sb"])
                S.op("act", lambda e: e.activation(z_sb[:, 0:511], z_sb[:, 0:511], AF.Sigmoid, scale=2.0 * GELU_C), reads=["z_sb"], writes=["z_sb"])
                S.op("dve", (lambda e, cc=cc: e.tensor_tensor(gT[cc][:, 0:511], u_sb[:, 0:511], z_sb[:, 0:511], op=ALU.mult)),
                     reads=["z_sb", "u_sb"], writes=[("gT", cc)])
            if which == 0:
                for cc in range(2):
                    S.op("pe", (lambda e, cc=cc: e.matmul(ph0[:, 0:511], w2b[:, cc, :], gT[cc][:, 0:511], start=(cc == 0), stop=(cc == 1))),
                         reads=["w2b", ("gT", cc)], writes=[("ps_s", 0)])
                S.op("act", lambda e: e.copy(kcmp[:, 0:511], ph0[:, 0:511]), reads=[("ps_s", 0)], writes=["kcmp"])
            else:
                for nt in range(4):
                    M = 128 if nt < 3 else 127
                    ph = at.ps_s[nt % 2]
                    for cc in range(2):
                        S.op("pe", (lambda e, cc=cc, nt=nt, M=M, ph=ph: e.matmul(ph[0:M, 0:128], gT[cc][:, nt * 128:nt * 128 + M], w2b[:, cc, :], start=(cc == 0), stop=(cc == 1))),
                             reads=["w2b", ("gT", cc)], writes=[("ps_s", nt % 2)])
                    S.op("act", (lambda e, nt=nt, M=M, ph=ph: e.copy(vcmp[0:M, nt, 0:128], ph[0:M, 0:128])), reads=[("ps_s", nt % 2)], writes=["vcmp"])

        compress(0)
        compress(1)
        if debug:
            dk = cx.dout("dbg_k", [128, 512], BF16)
            dv = cx.dout("dbg_v", [128, 4, 129], BF16)
            S.op("sp", lambda e: e.dma_start(out=dk, in_=kcmp[:]), reads=["kcmp"], writes=[("oD", "dk")], dsem="dbgk")
            S.op("sp", lambda e: e.dma_start(out=dv, in_=vcmp[:]), reads=["vcmp"], writes=[("oD", "dv")], dsem="dbgv")
            dxc = cx.dout("dbg_xc", [128, 64], BF16)
            S.op("sp", lambda e: e.dma_start(out=dxc, in_=xc[1][:, 0:64]), reads=[("xc", 1)], writes=[("oD", "dxc")], dsem="dbgxc")
            du = cx.dout("dbg_u", [128, 511], F32)
            S.op("sp", lambda e: e.dma_start(out=du, in_=u_sb[:, 0:511]), reads=["u_sb"], writes=[("oD", "du")], dsem="dbgu")
            dgt = cx.dout("dbg_g", [2, 128, 512], BF16)
            for i in range(2):
                S.op("sp", (lambda e, i=i: e.dma_start(out=dgt[i], in_=gT[i][:])), reads=[("gT", i)], writes=[("oD", "dg", i)], dsem=("dbgg", i))

        gsig = cx.sb("gsig", [128, 12], F32)
        coef = cx.sb("coef", [128, 4], F32)
        accO = cx.sb("accO", [128, 512], F32)
        imp = cx.sb("imp", [128, 128], F32)
        nD = cx.sb("nD", [128, 128], F32)
        adm = cx.sb("adm", [128, 128], F32)
        frc = cx.sb("frc", [128, 128], F32)
        val = cx.sb("val", [128, 128], F32)
        wrk = cx.sb("wrk", [128, 128], F32)
        m8 = cx.sb("m8", [128, 8], F32)
        thr = cx.sb("thr", [128, 1], F32)
        sel = cx.sb("sel", [128, 128], BF16)
        selT = cx.sb("selT", [128, 128], BF16)
        mcnt = [0]

        def coef_for(branch):
            at.recip()
            g3 = gsig[:].rearrange("p (h b) -> p h b", b=3)
            S.op("dve", (lambda e: e.tensor_tensor(coef[:], at.rs[:], g3[:, :, branch], op=ALU.mult)), reads=["rs_t", "gsig"], writes=["coef"])

        def do_slot(j):
            jsl = slice(j * 128, (j + 1) * 128)
            q512 = q_sb[:, j].rearrange("p h q -> p (h q)")
            S.op("act", (lambda e: e.activation(gsig[:], C["dg"][:, j, :], AF.Sigmoid)), reads=["dg"], writes=["gsig"])
            at.begin_acc()
            imp_started = [False]
            for nt in range(4):
                b = at.scores([(0, 512, kcmp[:, nt * 128:(nt + 1) * 128], q512, ["kcmp", "q"])])
                k = at.exp(b)
                at.mask_stt(k, C["qpos_rep"][:, jsl], C["cend_col"][:, nt:nt + 1], ALU.is_ge, ["qpos_rep", "cend_col"])
                at.pv(k, True, [vcmp[:, nt, :]] * 4, ["vcmp"], extra=(ps_imp, "ps_imp", C["ovl"][:, nt, :], ["ovl"], imp_started))
            coef_for(0)
            for h in range(4):
                a, c0 = h // 2, (h % 2) * 129
                S.op("dve", (lambda e, h=h, a=a, c0=c0: e.tensor_scalar(accO[:, h * 128:(h + 1) * 128], at.acc[a][:, c0:c0 + 128], coef[:, h:h + 1], None, op0=ALU.mult)),
                     reads=[("ps_acc", a), "coef"], writes=["accO"])
            S.op("dve", (lambda e: e.tensor_scalar(imp[:], ps_imp[:, 0:128], at.rs[:, 0:1], None, op0=ALU.mult)), reads=["ps_imp", "rs_t"], writes=["imp"])
            for h in range(1, 4):
                S.op("dve", (lambda e, h=h: e.scalar_tensor_tensor(out=imp[:], in0=ps_imp[:, h * 128:(h + 1) * 128], scalar=at.rs[:, h:h + 1], in1=imp[:], op0=ALU.mult, op1=ALU.add)),
                     reads=["ps_imp", "rs_t"], writes=["imp"])
            S.op("dve", (lambda e: e.tensor_scalar(nD[:], C["jstart_row"][:], C["qpos_col"][:, j:j + 1], None, op0=ALU.subtract)),
                 reads=["jstart_row", "qpos_col"], writes=["nD"])
            S.op("dve", (lambda e: e.tensor_scalar(adm[:], nD[:], 0.0, None, op0=ALU.is_le)), reads=["nD"], writes=["adm"])
            S.op("dve", (lambda e: e.scalar_tensor_tensor(out=frc[:], in0=nD[:], scalar=-128.0, in1=adm[:], op0=ALU.is_gt, op1=ALU.mult)),
                 reads=["nD", "adm"], writes=["frc"])
            S.op("dve", (lambda e: e.tensor_tensor(frc[:], frc[:], C["j0row"][:], op=ALU.max)), reads=["j0row"], writes=["frc"])
            S.op("dve", (lambda e: e.tensor_tensor(val[:], imp[:], adm[:], op=ALU.mult)), reads=["imp", "adm"], writes=["val"])
            S.op("dve", (lambda e: e.tensor_scalar(adm[:], adm[:], -1.0, 1e30, op0=ALU.add, op1=ALU.mult)), reads=["adm"], writes=["adm"])
            S.op("dve", (lambda e: e.tensor_tensor(val[:], val[:], adm[:], op=ALU.add)), reads=["adm"], writes=["val"])
            S.op("dve", (lambda e: e.scalar_tensor_tensor(out=val[:], in0=frc[:], scalar=1e9, in1=val[:], op0=ALU.mult, op1=ALU.add)),
                 reads=["frc"], writes=["val"])
            S.op("dve", (lambda e: e.max(out=m8[:], in_=val[:])), reads=["val"], writes=["m8"])
            S.op("dve", (lambda e: e.match_replace(out=wrk[:], in_to_replace=m8[:], in_values=val[:], imm_value=-3e38)), reads=["m8", "val"], writes=["wrk"])
            S.op("dve", (lambda e: e.max(out=m8[:], in_=wrk[:])), reads=["wrk"], writes=["m8"])
            S.op("dve", (lambda e: e.tensor_scalar(thr[:], m8[:, 7:8], -1e29, None, op0=ALU.max)), reads=["m8"], writes=["thr"])
            S.op("dve", (lambda e: e.tensor_scalar(sel[:], val[:], thr[:], None, op0=ALU.is_ge)), reads=["val", "thr"], writes=["sel"])
            S.op("pe", (lambda e: e.transpose(ps_m[:], sel[:], C["ident_bf"][:])), reads=["sel", "ident_bf"], writes=["ps_m"])
            S.op("act", (lambda e: e.copy(selT[:], ps_m[:])), reads=["ps_m"], writes=["selT"])
            at.begin_acc()
            for kt in range(8 * (j + 1)):
                b = at.scores([(0, 512, ks_sb[:, kt * 128:(kt + 1) * 128], q512, ["ks", "q"])])
                k = at.exp(b)
                mb = mcnt[0] % 2
                mcnt[0] += 1
                S.op("pe", (lambda e, kt=kt, mb=mb: e.matmul(ps_m2[mb][:], C["G"][:, kt * 128:(kt + 1) * 128], selT[:], start=True, stop=True)),
                     reads=["G", "selT"], writes=[("ps_m2", mb)])
                at.mask_tt(k, ps_m2[mb][:], [("ps_m2", mb)])
                if kt >= 8 * j:
                    at.mask_stt(k, C["qrel_rep"][:, jsl], C["kcol_rel"][:, kt - 8 * j:kt - 8 * j + 1], ALU.is_ge, ["qrel_rep", "kcol_rel"], inplace_pt=True)
                at.pv(k, True, [vs_sb[:, kt, :]] * 4, [("vs", kt // 8)])
            coef_for(1)
            for h in range(4):
                a, c0 = h // 2, (h % 2) * 129
                S.op("dve", (lambda e, h=h, a=a, c0=c0: e.scalar_tensor_tensor(out=accO[:, h * 128:(h + 1) * 128], in0=at.acc[a][:, c0:c0 + 128], scalar=coef[:, h:h + 1],
                                                                               in1=accO[:, h * 128:(h + 1) * 128], op0=ALU.mult, op1=ALU.add)),
                     reads=[("ps_acc", a), "coef"], writes=["accO"])
            at.begin_acc()
            for w in range(5):
                b = at.scores([(0, 512, kw_sb[:, j, w * 128:(w + 1) * 128], q512, ["kw", "q"])])
                k = at.exp(b)
                at.mask_tt(k, C["dwmask"][:, w, :], ["dwmask"])
                at.pv(k, True, [vw_sb[:, j, w, :]] * 4, ["vw"])
            coef_for(2)
            o = at.no % 2
            at.no += 1
            ob = at.osb[o]
            for h in range(4):
                a, c0 = h // 2, (h % 2) * 129
                S.op("dve", (lambda e, h=h, a=a, c0=c0: e.scalar_tensor_tensor(out=ob[:, h * 128:(h + 1) * 128], in0=at.acc[a][:, c0:c0 + 128], scalar=coef[:, h:h + 1],
                                                                               in1=accO[:, h * 128:(h + 1) * 128], op0=ALU.mult, op1=ALU.add)),
                     reads=[("ps_acc", a), "coef", "accO"], writes=[("osb", o)])
            S.op("sp", (lambda e: e.dma_start(out=oD[jsl, :], in_=ob[:])), reads=[("osb", o)], writes=[("oD", j)], dsem=("oo", o))

        for j in range(nslot):
            do_slot(j)
        finish_prog(cx, ("oD",))
    return nc

BF = ml_dtypes.bfloat16
NSLOT = 8


def slot_tile(c, j):
    return 16 * (j // 2) + (c if j % 2 == 0 else 15 - c)


def core_tokens(c):
    return np.concatenate([np.arange(slot_tile(c, j) * 128, slot_tile(c, j) * 128 + 128) for j in range(NSLOT)])


def common_consts(c):
    toks = core_tokens(c).astype(np.float32)
    qpos_rep = np.broadcast_to(toks[None, :], (128, 1024)).copy()
    qrel = toks - np.repeat(np.arange(NSLOT) * 1024.0, 128).astype(np.float32)
    qrel_rep = np.broadcast_to(qrel[None, :], (128, 1024)).copy()
    qpos_col = toks.reshape(NSLOT, 128).T.copy()
    qrel_col = qrel.reshape(NSLOT, 128).T.copy()
    kcol_rel = (np.arange(8)[None, :] * 128 + np.arange(128)[:, None]).astype(np.float32)
    return dict(qpos_rep=qpos_rep, qrel_rep=qrel_rep, qpos_col=qpos_col, qrel_col=qrel_col, kcol_rel=kcol_rel)


def q_layout(qT, c_unused=None, H=4):
    a = np.asarray(qT).reshape(H, 128, NSLOT, 128)
    return np.ascontiguousarray(a.transpose(1, 2, 0, 3))


def kT_layout(kT, H=4):
    a = np.asarray(kT).reshape(H, 128, 8192)
    return np.ascontiguousarray(a.transpose(1, 0, 2))


def v_layout(vT, H=4):
    a = np.asarray(vT).reshape(H, 128, 64, 128)
    out = np.ones((128, 64, H, 129), dtype=a.dtype)
    out[:, :, :, :128] = a.transpose(3, 2, 0, 1)
    return out


def b_consts(c, flogT, b_f):
    cc = common_consts(c)
    flog = np.ascontiguousarray(np.asarray(flogT, np.float32).reshape(4, 64, 128).transpose(2, 0, 1))
    oh = np.zeros((128, NSLOT, 64), np.float32)
    for j in range(NSLOT):
        oh[:, j, slot_tile(c, j)] = 1.0
    tri = (np.arange(128)[:, None] <= np.arange(128)[None, :]).astype(np.float32)
    return dict(flog=flog, bf_rep=np.broadcast_to(np.asarray(b_f, np.float32)[None, :], (128, 4)).copy(), oh=oh,
                qrel_rep=cc["qrel_rep"], kcol_rel=cc["kcol_rel"], tri32=tri)


def a_halo(c, kT, vT):
    kT = np.asarray(kT).reshape(12, 128, 64, 128)
    vT = np.asarray(vT).reshape(12, 128, 64, 128)
    W = [2, 5, 17]
    KT = np.zeros((NSLOT, 24, 128, 4, 128), kT.dtype)
    V = np.zeros((NSLOT, 24, 128, 4, 129), vT.dtype)
    for j in range(NSLOT):
        t = slot_tile(c, j)
        ti = 0
        for g in range(3):
            for w in range(W[g]):
                tk = t - (W[g] - 1) + w
                if tk >= 0:
                    KT[j, ti] = kT[g * 4:(g + 1) * 4, :, tk, :].transpose(1, 0, 2)
                    V[j, ti, :, :, :128] = vT[g * 4:(g + 1) * 4, :, tk, :].transpose(2, 0, 1)
                    V[j, ti, :, :, 128] = 1
                ti += 1
    return KT, V


def a_mask():
    pats = ((128, 1), (512, 4), (2048, 16))
    W = [2, 5, 17]
    m = np.zeros((128, 24, 128), np.float32)
    kp = np.arange(128)[:, None]
    qp = np.arange(128)[None, :]
    ti = 0
    for g in range(3):
        Wd, r = pats[g]
        for w in range(W[g]):
            Dm = qp - kp + 128 * (W[g] - 1 - w)
            m[:, ti, :] = ((Dm >= 0) & (Dm <= Wd) & (Dm % r == 0)).astype(np.float32)
            ti += 1
    return m.astype(BF)


def kv_tiles(kT, vT, H=4):
    k = np.asarray(kT).reshape(H, 128, 64, 128)
    v = np.asarray(vT).reshape(H, 128, 64, 128)
    KT = np.ascontiguousarray(k.transpose(2, 1, 0, 3))
    V = np.ones((64, 128, H, 129), v.dtype)
    V[:, :, :, :128] = v.transpose(2, 3, 0, 1)
    return KT, V


def c_consts(c, wiT_loc):
    cc = common_consts(c)
    wi = np.ascontiguousarray(np.asarray(wiT_loc, np.float32).reshape(16, NSLOT, 128).transpose(2, 1, 0))
    iota = np.broadcast_to(np.arange(1024, dtype=np.float32)[None, :], (128, 1024)).copy()
    return dict(wi=wi, qrel_col=cc["qrel_col"], iota_row=iota, ident_bf=np.eye(128, dtype=np.float32).astype(BF))


def qi_layout(qiT_loc):
    a = np.asarray(qiT_loc).reshape(8, 128, NSLOT, 128)
    return np.ascontiguousarray(a.transpose(1, 2, 0, 3))


def d_inputs(c, qT_loc, kcT, vcT, ksT, vsT, kwT, vwT, dgT_loc, cmp_pe, cmp_w1, cmp_w2):
    cc = common_consts(c)
    vs = np.ones((128, 64, 129), np.asarray(vsT).dtype)
    vs[:, :, :128] = np.asarray(vsT).reshape(128, 64, 128).transpose(2, 1, 0)
    kw = np.zeros((128, NSLOT, 640), np.asarray(kwT).dtype)
    vw = np.zeros((128, NSLOT, 5, 129), np.asarray(vwT).dtype)
    kwt = np.asarray(kwT).reshape(128, 64, 128)
    vwt = np.asarray(vwT).reshape(128, 64, 128)
    for j in range(NSLOT):
        t = slot_tile(c, j)
        for w in range(5):
            tk = t - 4 + w
            if tk >= 0:
                kw[:, j, w * 128:(w + 1) * 128] = kwt[:, tk, :]
                vw[:, j, w, :128] = vwt[:, tk, :].T
                vw[:, j, w, 128] = 1
    kp = np.arange(128)[:, None]
    qp = np.arange(128)[None, :]
    dwmask = np.zeros((128, 5, 128), np.float32)
    for w in range(5):
        Dm = qp - kp + 128 * (4 - w)
        dwmask[:, w, :] = ((Dm >= 0) & (Dm < 512)).astype(np.float32)
    dg = np.ascontiguousarray(np.asarray(dgT_loc, np.float32).reshape(12, NSLOT, 128).transpose(2, 1, 0))
    peT = np.ascontiguousarray(np.asarray(cmp_pe, np.float32).transpose(2, 0, 1))
    n = np.arange(512)
    cend = (16 * n + 31).astype(np.float32).reshape(4, 128).T.copy()
    jstart = np.broadcast_to((64.0 * np.arange(128, dtype=np.float32))[None, :], (128, 128)).copy()
    j0row = np.zeros((128, 128), np.float32)
    j0row[:, 0] = 1
    G = (np.arange(128)[:, None] == (np.arange(8192)[None, :] // 64)).astype(np.float32).astype(BF)
    ci = np.arange(512)[:, None]
    sj = np.arange(128)[None, :]
    ovl = ((ci * 16 < (sj + 1) * 64) & (ci * 16 + 32 > sj * 64) & (ci < 511)).astype(np.float32)
    ovl = np.ascontiguousarray(ovl.reshape(4, 128, 128).transpose(1, 0, 2)).astype(BF)
    return dict(QT=q_layout(qT_loc), KcT=np.ascontiguousarray(kcT), VcT=np.ascontiguousarray(vcT), KsT=np.ascontiguousarray(ksT), Vs=vs,
                KwT=kw, Vw=vw, w1=np.asarray(cmp_w1, np.float32), w2=np.asarray(cmp_w2, np.float32),
                dwmask=dwmask.astype(BF), dg=dg, peT=peT, qpos_rep=cc["qpos_rep"], qrel_rep=cc["qrel_rep"], qpos_col=cc["qpos_col"],
                kcol_rel=cc["kcol_rel"], cend_col=cend, jstart_row=jstart, j0row=j0row, G=G, ovl=ovl,
                ident_bf=np.eye(128, dtype=np.float32).astype(BF))


NCORES = 8
_PROGS = {}


def _prog(name, builder):
    if name not in _PROGS:
        _PROGS[name] = builder()
    return _PROGS[name]


def _run(name, builder, in_maps):
    nc = _prog(name, builder)
    res = run_bass_kernel_spmd(nc, in_maps, core_ids=list(range(NCORES)))
    return res.results


def rope_tables(pos):
    T_ = len(pos)
    inv128 = (10000.0 ** (-np.arange(64, dtype=np.float32) / 64)).astype(np.float32)
    ang128 = pos.astype(np.float32)[None, :] * inv128[np.arange(128) % 64][:, None]
    inv64 = (10000.0 ** (-np.arange(32, dtype=np.float32) / 32)).astype(np.float32)
    ang64 = pos.astype(np.float32)[None, :] * inv64[(np.arange(128) % 64) % 32][:, None]
    cs = np.zeros((6, 128, T_), np.float32)
    cs[0] = np.cos(ang128)
    cs[1] = np.sin(ang128)
    cs[2] = np.cos(ang64)
    cs[3] = np.sin(ang64)
    cs[4] = cs[2]
    cs[5] = cs[3]
    cs[4, 64:] = 1.0
    cs[5, 64:] = 0.0
    return cs


def rot_mats():
    R = np.zeros((3, 128, 128), np.float32)
    for dp in range(128):
        if dp < 64:
            R[0, dp + 64, dp] = -1.0
        else:
            R[0, dp - 64, dp] = 1.0
    for blk in range(2):
        for r in range(64):
            dp = blk * 64 + r
            if r < 32:
                R[1, dp + 32, dp] = -1.0
            else:
                R[1, dp - 32, dp] = 1.0
    return R


def gcol(g):
    return np.ascontiguousarray(np.asarray(g, np.float32).reshape(16, 128).T)


def kernel(x, norm_mix, w_in, w_gate, b_gate, b_f, cmp_pe, cmp_w1, cmp_w2, w_branch, w_out,
           norm_ffn, w_ffn_in, w_ffn_out, norm_final):
    x = np.asarray(x, np.float32)
    S_ = x.shape[1]
    toks = [core_tokens(c) for c in range(NCORES)]
    xT = [np.ascontiguousarray(x[0, toks[c]].T) for c in range(NCORES)]
    cs = [rope_tables(toks[c]) for c in range(NCORES)]
    rm = rot_mats()
    perm = in_col_perm()
    offs, _ = fm_row_offsets()
    amask = a_mask()
    depth = np.asarray(w_in).shape[0]
    onT = None
    for l in range(depth):
        wr = np.ascontiguousarray(np.asarray(w_in[l], np.float32)[:, perm])
        g1 = gcol(norm_mix[l])
        r1 = _run("p1", build_p1, [dict(xT=xT[c], gcol=g1, w=wr, cs=cs[c], rmat=rm) for c in range(NCORES)])
        del wr

        def glob(name, src="fm"):
            r0, n = offs[name]
            out = np.zeros((n, S_), r1[0]["fm"].dtype)
            for c in range(NCORES):
                out[:, toks[c]] = r1[c]["fm"][r0:r0 + n]
            return out

        def loc(name, c):
            r0, n = offs[name]
            return r1[c]["fm"][r0:r0 + n]

        def glob_sm(r0, n):
            out = np.zeros((n, S_), np.float32)
            for c in range(NCORES):
                out[:, toks[c]] = r1[c]["sm"][r0:r0 + n]
            return out

        ak, av = glob("ak"), glob("av")
        ins = []
        for c in range(NCORES):
            KT, V = a_halo(c, ak, av)
            ins.append(dict(QT=q_layout(loc("aq", c), H=12), KT=KT, V=V, amask=amask))
        rA = _run("p2a", build_p2a, ins)
        del ak, av, ins
        bk, bv = kT_layout(glob("bk")), v_layout(glob("bv"))
        bfl = glob_sm(64, 4)
        ins = []
        for c in range(NCORES):
            d = dict(QT=q_layout(loc("bq", c)), KT=bk, V=bv)
            d.update(b_consts(c, bfl, np.asarray(b_f[l], np.float32)))
            ins.append(d)
        rB = _run("p2b", build_p2b, ins)
        del bk, bv, ins
        cKT, cV = kv_tiles(glob("ck"), glob("cv"))
        ki = glob_sm(0, 64)
        ki2 = np.ascontiguousarray(np.concatenate([ki, ki], 0))
        ins = []
        for c in range(NCORES):
            d = dict(QT=q_layout(loc("cq", c)), KT=cKT, V=cV, QiT=qi_layout(loc("cqi", c)), KiT2=ki2)
            d.update(c_consts(c, r1[c]["sm"][68:84]))
            ins.append(d)
        rC = _run("p2c", build_p2c, ins)
        del cKT, cV, ins
        kc, vc, ks_, vs_, kw, vw = glob("dkc"), glob("dvc"), glob("dks"), glob("dvs"), glob("dkw"), glob("dvw")
        ins = [d_inputs(c, loc("dq", c), kc, vc, ks_, vs_, kw, vw, r1[c]["sm"][84:96], cmp_pe[l], cmp_w1[l], cmp_w2[l]) for c in range(NCORES)]
        rD = _run("p2d", build_p2d, ins)
        del ins
        wg = np.ascontiguousarray(np.asarray(w_gate[l], np.float32).reshape(2048, 8192))
        bg = np.ascontiguousarray(np.asarray(b_gate[l], np.float32).reshape(4, 16, 128).transpose(2, 0, 1).reshape(128, 64))
        wb = np.ascontiguousarray(np.asarray(w_branch[l], np.float32).reshape(2048, 2048))
        ins = []
        for c in range(NCORES):
            brT = np.ascontiguousarray(np.concatenate([rA[c]["oA"].T, rB[c]["oB"].T, rC[c]["oC"].T, rD[c]["oD"].T], 0))
            ins.append(dict(xT=xT[c], hT=r1[c]["hT"], brT=brT, wg=wg, bg=bg, wb=wb, wo=np.asarray(w_out[l], np.float32), gffn=gcol(norm_ffn[l]),
                            wfi=np.asarray(w_ffn_in[l], np.float32), wfo=np.asarray(w_ffn_out[l], np.float32), gfin=gcol(norm_final)))
        r3 = _run("p3", build_p3, ins)
        del ins
        xT = [np.ascontiguousarray(r3[c]["x2T"]) for c in range(NCORES)]
        onT = [r3[c]["onT"] for c in range(NCORES)]
    out = np.zeros((1, S_, 2048), np.float32)
    for c in range(NCORES):
        out[0, toks[c]] = np.asarray(onT[c], np.float32).T
    return out
```

```python
from contextlib import ExitStack
import numpy as np
import ml_dtypes
import concourse.bass as bass
import concourse.mybir as mybir
from concourse.bass_utils import run_bass_kernel_spmd


F32 = mybir.dt.float32
BF16 = mybir.dt.bfloat16
AF = mybir.ActivationFunctionType
ALU = mybir.AluOpType
AX = mybir.AxisListType

SEM_MAXV = 16000
SAME_ENGINE_SYNC = True


class Sched:
    ENGS = ("pe", "act", "dve", "pool", "sp")

    def __init__(self, nc, stack):
        self.nc = nc
        self.stack = stack
        self.ops = {e: [] for e in self.ENGS}
        self.semh = {}
        self.cnt = {}
        self.epoch = {}
        self.waited = {e: {} for e in self.ENGS}
        self.res = {}
        self.nsem = 0
        self.free = {}

    def _semkey(self, base, inc):
        ep = self.epoch.get(base, 0)
        key = (base, ep)
        if self.cnt.get(key, 0) + inc > (32000 if base[0] != "e" else SEM_MAXV):
            ep += 1
            self.epoch[base] = ep
            key = (base, ep)
        if key not in self.semh:
            lim = 32000 if base[0] != "e" else SEM_MAXV
            got = None
            if base[0] != "e":
                fl = self.free.setdefault(base[0], [])
                for i, (h, c) in enumerate(fl):
                    if c + inc <= lim:
                        got = fl.pop(i)
                        break
            if got is not None:
                self.semh[key], self.cnt[key] = got
            else:
                self.semh[key] = self.stack.enter_context(self.nc.semaphore("s%d" % self.nsem))
                self.nsem += 1
                self.cnt[key] = 0
        return key

    def phase_end(self):
        self.barrier()
        self.emit()
        for key in list(self.semh.keys()):
            base = key[0]
            if base[0] in ("d", "dsw"):
                self.free.setdefault(base[0], []).append((self.semh.pop(key), self.cnt.pop(key)))
                self.epoch.pop(base, None)
                for e in self.ENGS:
                    self.waited[e].pop(key, None)
        self.res = {}

    def op(self, eng, fn, reads=(), writes=(), dsem=None, dinc=16, _grp=None):
        deps = []
        def _isps(k):
            n = k if isinstance(k, str) else k[0]
            return isinstance(n, str) and n.startswith("ps")
        writes = list(writes) + [r for r in reads if _isps(r) and r not in writes]
        reads = [r for r in reads if not _isps(r)]
        for r in reads:
            st = self.res.get(r)
            if st is not None and st[0] is not None:
                deps.append(st[0])
        for w in writes:
            st = self.res.get(w)
            if st is not None:
                if st[0] is not None:
                    deps.append(st[0])
                deps.extend(st[1])
        is_dma = dsem is not None
        inc = dinc if is_dma else 1
        if _grp is not None:
            key, evval = _grp
            self.cnt[key] += inc
            ev = (key, evval, None)
        else:
            cls = "d"
            if is_dma and dinc != 16:
                cls = "dcc"
            elif is_dma and eng == "pool":
                cls = "dsw"
            key = self._semkey((cls, dsem) if is_dma else ("e", eng), inc)
            self.cnt[key] += inc
            ev = (key, self.cnt[key], eng if not is_dma else None)
        waits = []
        wd = self.waited[eng]
        best = {}
        for (k, v, src) in deps:
            if _grp is not None and k == _grp[0] and v == _grp[1]:
                continue
            if src == eng and (eng == "pe" or not SAME_ENGINE_SYNC):
                continue
            if wd.get(k, 0) >= v:
                continue
            if best.get(k, 0) < v:
                best[k] = v
        for k, v in best.items():
            wd[k] = v
            waits.append((k, v))
        self.ops[eng].append((waits, fn, key, inc))
        for r in reads:
            st = self.res.setdefault(r, [None, []])
            st[1].append(ev)
        for w in writes:
            self.res[w] = [ev, []]
        return ev

    def dma_many(self, eng, fns, writes_list, dsem, reads_list=None):
        n = len(fns)
        base = ("dsw" if eng == "pool" else "d", dsem)
        key = self._semkey(base, 16 * n)
        final = self.cnt[key] + 16 * n
        evs = []
        for i, fn in enumerate(fns):
            evs.append(self.op(eng, fn, reads=(reads_list[i] if reads_list else ()), writes=writes_list[i], dsem=dsem, _grp=(key, final)))
        return evs

    def wait_all(self, eng, evs):
        waits = []
        wd = self.waited[eng]
        best = {}
        for (k, v, src) in evs:
            if wd.get(k, 0) < v and best.get(k, 0) < v:
                best[k] = v
        for k, v in best.items():
            wd[k] = v
            waits.append((k, v))
        self.ops[eng].append((waits, None, None, 0))

    def emit(self):
        nc = self.nc
        eng_obj = {"pe": "tensor", "act": "scalar", "dve": "vector", "pool": "gpsimd", "sp": "sync"}
        with nc.Block() as block:
            for e in self.ENGS:
                if not self.ops[e]:
                    continue
                ops = self.ops[e]
                semh = self.semh

                def body(engine, ops=ops):
                    for (waits, fn, key, inc) in ops:
                        for (k, v) in waits:
                            engine.wait_ge(semh[k], v)
                        if fn is not None:
                            ins = fn(engine)
                            ins.then_inc(semh[key], inc)

                getattr(block, eng_obj[e])(body)
        self.ops = {e: [] for e in self.ENGS}

    def barrier(self):
        evs = [(k, v, None) for k, v in self.cnt.items() if v > 0]
        for e in self.ENGS:
            self.wait_all(e, evs)

    def n_ops(self):
        return {e: len(self.ops[e]) for e in self.ENGS}


T = 1024
TG = 512
NTG = T // TG
D = 2048
KC = D // 128
EPS = 1e-6


def new_nc():
    return bass.Bass("TRN2", target_bir_lowering=False)


U32 = mybir.dt.uint32


class Ctx:
    _uid = 0

    def __init__(self, nc, st, S=None, ov=None):
        self.nc = nc
        self.st = st
        self.S = S if S is not None else Sched(nc, st)
        self.ov = ov if ov is not None else {}
        Ctx._uid += 1
        self.uid = Ctx._uid

    def sb(self, name, shape, dt):
        return self.st.enter_context(self.nc.sbuf_tensor("%s_u%d" % (name, self.uid), shape, dt))

    def ps(self, name, shape, dt):
        return self.st.enter_context(self.nc.psum_tensor("%s_u%d" % (name, self.uid), shape, dt))

    def din(self, name, shape, dt):
        if name in self.ov:
            ap = self.ov[name]
            assert list(ap.shape) == list(shape), (name, ap.shape, shape)
            return ap
        return self.nc.dram_tensor(name, shape, dt, kind="ExternalInput").ap()

    def dout(self, name, shape, dt):
        if name in self.ov:
            ap = self.ov[name]
            assert list(ap.shape) == list(shape), (name, ap.shape, shape)
            return ap
        return self.nc.dram_tensor(name, shape, dt, kind="ExternalOutput").ap()


class WStream:
    def __init__(self, cx, nslots=4, kcmax=16):
        self.cx = cx
        self.n = nslots
        self.i = 0
        self.stg = [cx.sb("wstg%d" % i, [128, kcmax, 128], F32) for i in range(nslots)]
        self.bf = [cx.sb("wbf%d" % i, [128, kcmax, 128], BF16) for i in range(nslots)]

    def load(self, w_ap, k0, kc, c0, M, cast_eng="pool"):
        S = self.cx.S
        s = self.i % self.n
        self.i += 1
        src = w_ap[k0:k0 + kc * 128, c0:c0 + M].rearrange("(kc p) m -> p kc m", p=128)
        stg = self.stg[s]
        bf = self.bf[s]
        for k4 in range(0, kc, 4):
            k5 = min(kc, k4 + 4)
            S.op("sp", (lambda e, k4=k4, k5=k5: e.dma_start(out=stg[:, k4:k5, 0:M], in_=src[:, k4:k5, :])), writes=[("wstg", s)], dsem=("wstg", s))
        S.op(cast_eng, lambda e: e.tensor_copy(bf[:, 0:kc, 0:M], stg[:, 0:kc, 0:M]), reads=[("wstg", s)], writes=[("wbf", s)])
        return bf, ("wbf", s)


def pipeline(units, depth=2):
    handles = {}
    n = len(units)
    for i in range(n + depth):
        if i < n:
            handles[i] = units[i][0]()
        j = i - depth
        if j >= 0:
            units[j][1](handles.pop(j))


def rmsnorm_fm(cx, x_sb, xkey, gcol_sb, gkey, out_sb, okey, ones32, ps_ss, sq, rstd, out_scaled_by_g=True):
    S = cx.S

    def do_tg(tg):
        tsl = slice(tg * TG, (tg + 1) * TG)
        for kc in range(KC):
            b = kc % 2
            S.op("act", (lambda e, kc=kc, b=b: e.activation(sq[b][:], x_sb[:, kc, tsl], AF.Square)),
                 reads=[(xkey, kc)], writes=[("sq", b)])
            S.op("pe", (lambda e, kc=kc, b=b: e.matmul(ps_ss[:], ones32[:], sq[b][:], start=(kc == 0), stop=(kc == KC - 1))),
                 reads=[("sq", b), "ones32"], writes=["ps_ss"])
        S.op("dve", lambda e: e.tensor_scalar(rstd[:], ps_ss[:], 1.0 / D, EPS, op0=ALU.mult, op1=ALU.add),
             reads=["ps_ss"], writes=["rstd"])
        S.op("act", lambda e: e.activation(rstd[:], rstd[:], AF.Sqrt),
             reads=["rstd"], writes=["rstd"])
        S.op("dve", lambda e: e.reciprocal(rstd[:], rstd[:]),
             reads=["rstd"], writes=["rstd"])
        for kc in range(KC):
            S.op("dve", (lambda e, kc=kc: e.scalar_tensor_tensor(out=out_sb[:, kc, tsl], in0=x_sb[:, kc, tsl], scalar=gcol_sb[:, kc:kc + 1],
                                                               in1=rstd[:], op0=ALU.mult, op1=ALU.mult)),
                 reads=[(xkey, kc), gkey, "rstd"], writes=[(okey, kc)])

    for tg in range(NTG):
        do_tg(tg)


IN_GROUPS = [
    ("aq", 0, 1536, "r128"), ("ak", 1536, 1536, "r128"), ("av", 3072, 1536, None),
    ("bq", 4608, 512, None), ("bk", 5120, 512, None), ("bv", 5632, 512, None),
    ("cq", 6148, 512, "r128"), ("ck", 6660, 512, "r128"), ("cv", 7172, 512, None),
    ("cqi", 7684, 1024, "r64"),
    ("dq", 8788, 512, "r128"), ("dkc", 9300, 128, "r128"), ("dvc", 9428, 128, None),
    ("dks", 9556, 128, "r128"), ("dvs", 9684, 128, None), ("dkw", 9812, 128, "r128"), ("dvw", 9940, 128, None),
]
LAST_COLS = list(range(8708, 8772)) + list(range(6144, 6148)) + list(range(8772, 8788)) + list(range(10068, 10080))


def in_col_perm():
    cols = []
    for (_, c0, n, _) in IN_GROUPS:
        cols += list(range(c0, c0 + n))
    cols += LAST_COLS
    assert len(cols) == 10080 and len(set(cols)) == 10080
    return np.array(cols)


def fm_row_offsets():
    off = {}
    r = 0
    for (name, c0, n, _) in IN_GROUPS:
        off[name] = (r, n)
        r += n
    return off, r


def p1_chunks():
    ch = []
    c = 0
    for (name, c0, n, rk) in IN_GROUPS:
        for i in range(n // 128):
            ch.append((c, 128, rk, "fm"))
            c += 128
    ch.append((c, 96, "rL", "sm"))
    return ch


def build_p1(nchunks=None, stage=9):
    nc = new_nc()
    with ExitStack() as st:
        cx = Ctx(nc, st)
        S = cx.S
        xT = cx.din("xT", [D, T], F32)
        gcol = cx.din("gcol", [128, KC], F32)
        w = cx.din("w", [D, 10080], F32)
        cs = cx.din("cs", [6, 128, T], F32)
        rmat = cx.din("rmat", [3, 128, 128], F32)
        fm = cx.dout("fm", [9984, T], BF16)
        sm = cx.dout("sm", [96, T], F32)
        hT = cx.dout("hT", [D, T], BF16)

        x_sb = cx.sb("x_sb", [128, KC, T], F32)
        h_sb = cx.sb("h_sb", [128, KC, T], BF16)
        g_sb = cx.sb("g_sb", [128, KC], F32)
        cs_sb = cx.sb("cs_sb", [128, 6, T], F32)
        r32 = cx.sb("r32", [128, 3, 128], F32)
        rbf = cx.sb("rbf", [128, 3, 128], BF16)
        ones32 = cx.sb("ones32", [128, 128], F32)
        sq = [cx.sb("sq%d" % i, [128, TG], F32) for i in range(2)]
        rstd = cx.sb("rstd", [128, TG], F32)
        ysb = [cx.sb("ysb%d" % i, [128, TG], BF16) for i in range(2)]
        t1 = [cx.sb("t1_%d" % i, [128, TG], F32) for i in range(2)]
        t2 = [cx.sb("t2_%d" % i, [128, TG], F32) for i in range(2)]
        obf = [cx.sb("obf%d" % i, [128, TG], BF16) for i in range(4)]
        o32 = [cx.sb("o32_%d" % i, [128, TG], F32) for i in range(2)]
        ps_y = [cx.ps("ps_y%d" % i, [128, TG], F32) for i in range(2)]
        ps_r = [cx.ps("ps_r%d" % i, [128, TG], F32) for i in range(2)]
        ps_ss = cx.ps("ps_ss", [128, TG], F32)
        ws = WStream(cx, nslots=4)

        for kc in range(KC):
            S.op("sp", (lambda e, kc=kc: e.dma_start(out=x_sb[:, kc, :], in_=xT[kc * 128:(kc + 1) * 128, :])),
                 writes=[("x", kc)], dsem=("x", kc))
        S.op("sp", lambda e: e.dma_start(out=g_sb[:], in_=gcol), writes=["g"], dsem="g")
        S.op("sp", lambda e: e.dma_start(out=cs_sb[:], in_=cs.rearrange("s p t -> p s t")), writes=["cs"], dsem="cs")
        S.op("sp", lambda e: e.dma_start(out=r32[:], in_=rmat.rearrange("s p t -> p s t")), writes=["r32"], dsem="r32")
        S.op("dve", lambda e: e.tensor_copy(rbf[:], r32[:]), reads=["r32"], writes=["rbf"])
        S.op("dve", lambda e: e.memset(ones32[:], 1.0), writes=["ones32"])

        if stage >= 2:
            rmsnorm_fm(cx, x_sb, "x", g_sb, "g", h_sb, "h", ones32, ps_ss, sq, rstd)
        else:
            for kc in range(KC):
                S.op("dve", (lambda e, kc=kc: e.tensor_copy(h_sb[:, kc, :], x_sb[:, kc, :])), reads=[("x", kc)], writes=[("h", kc)])
        hreads = [("h", kc) for kc in range(KC)]
        for kc in range(KC):
            S.op("sp", (lambda e, kc=kc: e.dma_start(out=hT[kc * 128:(kc + 1) * 128, :], in_=h_sb[:, kc, :])),
                 reads=[("h", kc)], writes=[("hT", kc)], dsem=("hTo", kc % 2))

        chunks = p1_chunks()
        if nchunks is not None:
            chunks = chunks[:nchunks] + chunks[-1:]
        if stage < 3:
            chunks = []
        if stage == 3:
            chunks = chunks[:-1]
        cnt = [0]

        def mk_unit(ci, c0, M, rk, okind):
            def load():
                return ws.load(w, 0, KC, c0, M)

            def compute(hd):
                bf, bkey = hd
                for tg in range(NTG):
                    do_tg(bf, bkey, tg)

            def do_tg(bf, bkey, tg):
                if True:
                    tsl = slice(tg * TG, (tg + 1) * TG)
                    k = cnt[0]
                    cnt[0] += 1
                    b = k % 2
                    py = ps_y[b]
                    for kc in range(KC):
                        S.op("pe", (lambda e, kc=kc, py=py: e.matmul(py[0:M, :], bf[:, kc, 0:M], h_sb[:, kc, tsl], start=(kc == 0), stop=(kc == KC - 1))),
                             reads=[bkey, ("h", kc)], writes=[("ps_y", b)])
                    if okind == "fm":
                        ob = obf[k % 4]
                        okey = ("obf", k % 4)
                    else:
                        ob = o32[b]
                        okey = ("o32", b)
                    if rk is None:
                        S.op("act", (lambda e, py=py, ob=ob: e.copy(ob[0:M, :], py[0:M, :])), reads=[("ps_y", b)], writes=[okey])
                    else:
                        ri = {"r128": 0, "r64": 1, "rL": 1}[rk]
                        ci_ = {"r128": 0, "r64": 2, "rL": 4}[rk]
                        pr = ps_r[b]
                        S.op("act", (lambda e, py=py: e.copy(ysb[b][0:M, :], py[0:M, :])), reads=[("ps_y", b)], writes=[("ysb", b)])
                        S.op("pe", (lambda e, pr=pr: e.matmul(pr[0:M, :], rbf[0:M, ri, 0:M], ysb[b][0:M, :], start=True, stop=True)),
                             reads=[("ysb", b), "rbf"], writes=[("ps_r", b)])
                        S.op("dve", (lambda e, py=py: e.tensor_tensor(t1[b][0:M, :], py[0:M, :], cs_sb[0:M, ci_, tsl], op=ALU.mult)),
                             reads=[("ps_y", b), "cs"], writes=[("t1", b)])
                        S.op("dve", (lambda e, pr=pr: e.tensor_tensor(t2[b][0:M, :], pr[0:M, :], cs_sb[0:M, ci_ + 1, tsl], op=ALU.mult)),
                             reads=[("ps_r", b), "cs"], writes=[("t2", b)])
                        S.op("pool", (lambda e, ob=ob: e.tensor_tensor(ob[0:M, :], t1[b][0:M, :], t2[b][0:M, :], op=ALU.add)),
                             reads=[("t1", b), ("t2", b)], writes=[okey])
                    if okind == "fm":
                        S.op("sp", (lambda e, ob=ob: e.dma_start(out=fm[c0:c0 + M, tsl], in_=ob[0:M, :])),
                             reads=[okey], writes=[("fm", c0, tg)], dsem=("fmo", k % 4))
                    else:
                        S.op("sp", (lambda e, ob=ob: e.dma_start(out=sm[0:M, tsl], in_=ob[0:M, :])),
                             reads=[okey], writes=[("sm", tg)], dsem=("smo", tg))
            return (load, compute)

        units = [mk_unit(i, *ch) for i, ch in enumerate(chunks)]
        pipeline(units, depth=2)
        evs = [v[0] for k, v in S.res.items() if isinstance(k, tuple) and k[0] in ("fm", "sm", "hT") and v[0] is not None]
        S.wait_all("sp", evs)
        S.emit()
    return nc


FF = 5632
FC = FF // 128
FCH = FC // 2


def rmsnorm_gen(cx, xv, xkey, gcol_sb, gkey, ones32, ps_ss, sq, sqkey, rstd, write_out):
    S = cx.S

    def do_tg(tg):
        tsl = slice(tg * TG, (tg + 1) * TG)
        for kc in range(KC):
            b = kc % 2
            S.op("act", (lambda e, kc=kc, b=b: e.activation(sq[b][:], xv(kc, tsl), AF.Square)),
                 reads=[(xkey, kc)], writes=[(sqkey, b)])
            S.op("pe", (lambda e, kc=kc, b=b: e.matmul(ps_ss[:], ones32[:], sq[b][:], start=(kc == 0), stop=(kc == KC - 1))),
                 reads=[(sqkey, b), "ones32"], writes=["ps_ss"])
        S.op("dve", lambda e: e.tensor_scalar(rstd[:], ps_ss[:], 1.0 / D, EPS, op0=ALU.mult, op1=ALU.add),
             reads=["ps_ss"], writes=["rstd"])
        S.op("act", lambda e: e.activation(rstd[:], rstd[:], AF.Sqrt), reads=["rstd"], writes=["rstd"])
        S.op("dve", lambda e: e.reciprocal(rstd[:], rstd[:]), reads=["rstd"], writes=["rstd"])
        for kc in range(KC):
            def emit(out_ap, okey, kc=kc):
                S.op("dve", (lambda e: e.scalar_tensor_tensor(out=out_ap, in0=xv(kc, tsl), scalar=gcol_sb[:, kc:kc + 1],
                                                              in1=rstd[:], op0=ALU.mult, op1=ALU.mult)),
                     reads=[(xkey, kc), gkey, "rstd"], writes=[okey])
            write_out(kc, tg, tsl, emit)

    for tg in range(NTG):
        do_tg(tg)


def build_p3():
    nc = new_nc()
    with ExitStack() as st:
        cx = Ctx(nc, st)
        S = cx.S
        xT = cx.din("xT", [D, T], F32)
        hT = cx.din("hT", [D, T], BF16)
        brT = cx.din("brT", [2048, T], BF16)
        wg = cx.din("wg", [D, 8192], F32)
        bg = cx.din("bg", [128, 64], F32)
        wb = cx.din("wb", [2048, D], F32)
        wo = cx.din("wo", [D, D], F32)
        gffn = cx.din("gffn", [128, KC], F32)
        wfi = cx.din("wfi", [D, 2 * FF], F32)
        wfo = cx.din("wfo", [FF, D], F32)
        gfin = cx.din("gfin", [128, KC], F32)
        x2T = cx.dout("x2T", [D, T], F32)
        onT = cx.dout("onT", [D, T], F32)

        R = cx.sb("R", [128, 24576], F32)
        Rb = R.bitcast(BF16)
        h1 = Rb[:, 0:16384].rearrange("p (k t) -> p k t", k=KC)
        br = Rb[:, 16384:32768].rearrange("p (k t) -> p k t", k=KC)
        mg = Rb[:, 32768:49152].rearrange("p (k t) -> p k t", k=KC)
        xs = R[:, 0:16384].rearrange("p (k t) -> p k t", k=KC)
        h2 = mg
        act = cx.sb("act", [128, FCH, T], BF16)
        tA = [[cx.sb("tA%d%d" % (i, j), [128, TG], F32) for j in range(2)] for i in range(2)]
        acc = [cx.sb("acc%d" % i, [128, TG], F32) for i in range(2)]
        tmp = [cx.sb("tmp%d" % i, [128, TG], F32) for i in range(2)]
        rstd = cx.sb("rstd", [128, TG], F32)
        ones32 = cx.sb("ones32", [128, 128], F32)
        bg_sb = cx.sb("bg_sb", [128, 64], F32)
        gf_sb = cx.sb("gf_sb", [128, KC], F32)
        gl_sb = cx.sb("gl_sb", [128, KC], F32)
        ps_a = [cx.ps("ps_a%d" % i, [128, TG], F32) for i in range(2)]
        ps_b = [cx.ps("ps_b%d" % i, [128, TG], F32) for i in range(2)]
        ps_ss = cx.ps("ps_ss", [128, TG], F32)
        ws = WStream(cx, nslots=3)

        S.op("dve", lambda e: e.memset(ones32[:], 1.0), writes=["ones32"])
        S.op("sp", lambda e: e.dma_start(out=bg_sb[:], in_=bg), writes=["bg"], dsem="bg")
        S.op("sp", lambda e: e.dma_start(out=gf_sb[:], in_=gffn), writes=["gf"], dsem="gf")
        S.op("sp", lambda e: e.dma_start(out=gl_sb[:], in_=gfin), writes=["gl"], dsem="gl")
        for kc in range(KC):
            S.op("sp", (lambda e, kc=kc: e.dma_start(out=h1[:, kc, :], in_=hT[kc * 128:(kc + 1) * 128, :])),
                 writes=[("h", kc)], dsem=("h", kc))
        for kc in range(KC):
            S.op("sp", (lambda e, kc=kc: e.dma_start(out=br[:, kc, :], in_=brT[kc * 128:(kc + 1) * 128, :])),
                 writes=[("br", kc)], dsem=("br", kc))

        def tsl_of(tg):
            return slice(tg * TG, (tg + 1) * TG)

        def mm_group(ps, pkey, bf, bkey, nk, M, rhs_fn, rkey_fn, first=True, last=True):
            for kc in range(nk):
                S.op("pe", (lambda e, kc=kc: e.matmul(ps[0:M, :], bf[:, kc, 0:M], rhs_fn(kc), start=(first and kc == 0), stop=(last and kc == nk - 1))),
                     reads=[bkey, rkey_fn(kc)], writes=[pkey])

        units = []

        def u_gate(dc, n):
            def load():
                return ws.load(wg, 0, KC, n * 2048 + dc * 128, 128)

            def compute(hd):
                bf, bkey = hd
                for tg in range(NTG):
                    tsl = tsl_of(tg)
                    mm_group(ps_a[tg], ("ps_a", tg), bf, bkey, KC, 128, (lambda kc, tsl=tsl: h1[:, kc, tsl]), (lambda kc: ("h", kc)))
                    gs = tA[tg][n % 2]
                    S.op("act", (lambda e, tg=tg, gs=gs: e.activation(gs[:], ps_a[tg][:], AF.Sigmoid, bias=bg_sb[:, n * 16 + dc:n * 16 + dc + 1], scale=1.0)),
                         reads=[("ps_a", tg), "bg"], writes=[("tA", tg, n % 2)])
            return (load, compute)

        def u_branch(dc, n):
            def load():
                return ws.load(wb, n * 512, 4, dc * 128, 128)

            def compute(hd):
                bf, bkey = hd
                for tg in range(NTG):
                    tsl = tsl_of(tg)
                    mm_group(ps_b[tg], ("ps_b", tg), bf, bkey, 4, 128, (lambda kc, tsl=tsl: br[:, n * 4 + kc, tsl]), (lambda kc: ("br", n * 4 + kc)))
                    gs = tA[tg][n % 2]
                    if n == 0:
                        S.op("dve", (lambda e, tg=tg, gs=gs: e.tensor_tensor(acc[tg][:], gs[:], ps_b[tg][:], op=ALU.mult)),
                             reads=[("tA", tg, n % 2), ("ps_b", tg)], writes=[("acc", tg)])
                    else:
                        S.op("dve", (lambda e, tg=tg, gs=gs: e.tensor_tensor(tmp[tg][:], gs[:], ps_b[tg][:], op=ALU.mult)),
                             reads=[("tA", tg, n % 2), ("ps_b", tg)], writes=[("tmp", tg)])
                        if n < 3:
                            S.op("pool", (lambda e, tg=tg: e.tensor_tensor(acc[tg][:], acc[tg][:], tmp[tg][:], op=ALU.add)),
                                 reads=[("tmp", tg)], writes=[("acc", tg)])
                        else:
                            S.op("pool", (lambda e, tg=tg, tsl=tsl: e.tensor_tensor(mg[:, dc, tsl], acc[tg][:], tmp[tg][:], op=ALU.add)),
                                 reads=[("tmp", tg), ("acc", tg)], writes=[("mg", dc)])
            return (load, compute)

        for dc in range(KC):
            for n in range(4):
                units.append(u_gate(dc, n))
                units.append(u_branch(dc, n))
        pipeline(units, depth=2)
        S.barrier()
        for kc in range(KC):
            S.op("sp", (lambda e, kc=kc: e.dma_start(out=xs[:, kc, :], in_=xT[kc * 128:(kc + 1) * 128, :])),
                 writes=[("x", kc)], dsem=("x", kc))

        def u_out(dc):
            def load():
                return ws.load(wo, 0, KC, dc * 128, 128)

            def compute(hd):
                bf, bkey = hd
                for tg in range(NTG):
                    tsl = tsl_of(tg)
                    mm_group(ps_a[tg], ("ps_a", tg), bf, bkey, KC, 128, (lambda kc, tsl=tsl: mg[:, kc, tsl]), (lambda kc: ("mg", kc)))
                    S.op("dve", (lambda e, tg=tg, tsl=tsl: e.tensor_tensor(xs[:, dc, tsl], xs[:, dc, tsl], ps_a[tg][:], op=ALU.add)),
                         reads=[("ps_a", tg)], writes=[("x", dc)])
            return (load, compute)

        pipeline([u_out(dc) for dc in range(KC)], depth=2)
        S.barrier()
        def wr_h2(kc, tg, tsl, emit):
            emit(h2[:, kc, tsl], ("h2", kc))

        rmsnorm_gen(cx, (lambda kc, tsl: xs[:, kc, tsl]), "x", gf_sb, "gf", ones32, ps_ss, tmp, "tmp", rstd, wr_h2)

        for hh in range(2):
            units = []

            def u_ffn_in(fcl, which, hh=hh):
                col0 = which * FF + (hh * FCH + fcl) * 128

                def load():
                    return ws.load(wfi, 0, KC, col0, 128)

                def compute(hd):
                    bf, bkey = hd
                    for tg in range(NTG):
                        tsl = tsl_of(tg)
                        if which == 0:
                            mm_group(ps_a[tg], ("ps_a", tg), bf, bkey, KC, 128, (lambda kc, tsl=tsl: h2[:, kc, tsl]), (lambda kc: ("h2", kc)))
                            sg = tA[tg][fcl % 2]
                            S.op("act", (lambda e, tg=tg, sg=sg: e.activation(sg[:], ps_a[tg][:], AF.Silu)),
                                 reads=[("ps_a", tg)], writes=[("tA", tg, fcl % 2)])
                        else:
                            mm_group(ps_b[tg], ("ps_b", tg), bf, bkey, KC, 128, (lambda kc, tsl=tsl: h2[:, kc, tsl]), (lambda kc: ("h2", kc)))
                            sg = tA[tg][fcl % 2]
                            S.op("dve", (lambda e, tg=tg, sg=sg, tsl=tsl: e.tensor_tensor(act[:, fcl, tsl], sg[:], ps_b[tg][:], op=ALU.mult)),
                                 reads=[("tA", tg, fcl % 2), ("ps_b", tg)], writes=[("act", fcl)])
                return (load, compute)

            def u_ffn_out(dc, part, hh=hh):
                k0 = 0 if part == 0 else 16
                nk = 16 if part == 0 else FCH - 16

                def load():
                    return ws.load(wfo, (hh * FCH + k0) * 128, nk, dc * 128, 128)

                def compute(hd):
                    bf, bkey = hd
                    for tg in range(NTG):
                        tsl = tsl_of(tg)
                        mm_group(ps_a[tg], ("ps_a", tg), bf, bkey, nk, 128, (lambda kc, tsl=tsl: act[:, k0 + kc, tsl]), (lambda kc: ("act", k0 + kc)),
                                 first=(part == 0), last=(part == 1))
                        if part == 1:
                            S.op("dve", (lambda e, tg=tg, tsl=tsl: e.tensor_tensor(xs[:, dc, tsl], xs[:, dc, tsl], ps_a[tg][:], op=ALU.add)),
                                 reads=[("ps_a", tg)], writes=[("x", dc)])
                return (load, compute)

            for fcl in range(FCH):
                units.append(u_ffn_in(fcl, 0))
                units.append(u_ffn_in(fcl, 1))
            for dc in range(KC):
                units.append(u_ffn_out(dc, 0))
                units.append(u_ffn_out(dc, 1))
            pipeline(units, depth=2)

        for kc in range(KC):
            S.op("sp", (lambda e, kc=kc: e.dma_start(out=x2T[kc * 128:(kc + 1) * 128, :], in_=xs[:, kc, :])),
                 reads=[("x", kc)], writes=[("x2T", kc)], dsem=("x2o", kc % 2))
        ocnt = [0]

        def wr_fin(kc, tg, tsl, emit):
            k = ocnt[0]
            ocnt[0] += 1
            ob = tA[k % 2][(k // 2) % 2]
            okey = ("tA", k % 2, (k // 2) % 2)
            emit(ob[:], okey)
            S.op("sp", (lambda e: e.dma_start(out=onT[kc * 128:(kc + 1) * 128, tsl], in_=ob[:])),
                 reads=[okey], writes=[("onT", kc, tg)], dsem=("ono", k % 4))

        rmsnorm_gen(cx, (lambda kc, tsl: xs[:, kc, tsl]), "x", gl_sb, "gl", ones32, ps_ss, tmp, "tmp", rstd, wr_fin)
        evs = [v[0] for k, v in S.res.items() if isinstance(k, tuple) and k[0] in ("x2T", "onT") and v[0] is not None]
        S.wait_all("sp", evs)
        S.emit()
    return nc


NSLOT = 8
ATTN_SCALE = 128 ** -0.5
NEG = -1e30


def slot_tile(c, j):
    return 16 * (j // 2) + (c if j % 2 == 0 else 15 - c)


class Attn:
    def __init__(self, cx):
        self.cx = cx
        S = cx.S
        self.ps_s = [cx.ps("ps_s%d" % i, [128, 512], F32) for i in range(2)]
        self.acc = [cx.ps("ps_acc%d" % i, [128, 512], F32) for i in range(2)]
        self.pe_buf = [cx.sb("pe_buf%d" % i, [128, 512], BF16) for i in range(3)]
        self.pt_buf = [cx.sb("pt_buf%d" % i, [128, 512], BF16) for i in range(3)]
        self.rs = cx.sb("rs_t", [128, 4], F32)
        self.osb = [cx.sb("osb%d" % i, [128, 512], BF16) for i in range(2)]
        self.n = 0
        self.no = 0
        self.acc_started = [False, False]

    def begin_acc(self):
        self.acc_started = [False, False]

    def scores(self, mms):
        S = self.cx.S
        b = self.n % 2
        ps = self.ps_s[b]
        for i, (c0, ncol, lhsT, rhs, reads) in enumerate(mms):
            S.op("pe", (lambda e, i=i, c0=c0, ncol=ncol, lhsT=lhsT, rhs=rhs: e.matmul(ps[:, c0:c0 + ncol], lhsT, rhs, start=(i == 0), stop=(i == len(mms) - 1), skip_group_check=True)),
                 reads=reads, writes=[("ps_s", b)])
        return b

    def exp(self, b, biases=None, breads=()):
        S = self.cx.S
        k = self.n % 3
        ps = self.ps_s[b]
        buf = self.pe_buf[k]
        if biases is None:
            S.op("act", (lambda e: e.activation(buf[:], ps[:], AF.Exp, scale=ATTN_SCALE)), reads=[("ps_s", b)], writes=[("pe_buf", k)])
        else:
            for h in range(4):
                S.op("act", (lambda e, h=h: e.activation(buf[:, h * 128:(h + 1) * 128], ps[:, h * 128:(h + 1) * 128], AF.Exp, bias=biases[h], scale=ATTN_SCALE)),
                     reads=[("ps_s", b)] + list(breads), writes=[("pe_buf", k)])
        return k

    def mask_stt(self, k, in0, scalar, op0, reads, inplace_pt=False):
        S = self.cx.S
        src = self.pt_buf[k] if inplace_pt else self.pe_buf[k]
        skey = ("pt_buf", k) if inplace_pt else ("pe_buf", k)
        dst = self.pt_buf[k]
        S.op("dve", (lambda e: e.scalar_tensor_tensor(out=dst[:].rearrange("p (h q) -> p h q", h=4), in0=in0.unsqueeze(1).to_broadcast([128, 4, 128]),
                                                      scalar=scalar, in1=src[:].rearrange("p (h q) -> p h q", h=4), op0=op0, op1=ALU.mult)),
             reads=[skey] + list(reads), writes=[("pt_buf", k)])

    def mask_tt(self, k, mask_ap, reads, eng="dve"):
        S = self.cx.S
        src = self.pe_buf[k]
        dst = self.pt_buf[k]
        S.op(eng, (lambda e: e.tensor_tensor(dst[:].rearrange("p (h q) -> p h q", h=4), src[:].rearrange("p (h q) -> p h q", h=4),
                                             mask_ap.unsqueeze(1).to_broadcast([128, 4, 128]), op=ALU.mult)),
             reads=[("pe_buf", k)] + list(reads), writes=[("pt_buf", k)])

    def pv(self, k, masked, v_aps, vreads, extra=None):
        S = self.cx.S
        buf = self.pt_buf[k] if masked else self.pe_buf[k]
        bkey = ("pt_buf", k) if masked else ("pe_buf", k)
        for h in range(4):
            a = h // 2
            acc = self.acc[a]
            st_ = not self.acc_started[a]
            self.acc_started[a] = True
            c0 = (h % 2) * 129
            S.op("pe", (lambda e, h=h, acc=acc, st_=st_, c0=c0: e.matmul(acc[:, c0:c0 + 129], buf[:, h * 128:(h + 1) * 128], v_aps[h], start=st_, stop=False, skip_group_check=True)),
                 reads=[bkey] + list(vreads), writes=[("ps_acc", a)])
            if extra is not None:
                pst, pkey, rhs, rreads, started = extra
                S.op("pe", (lambda e, h=h, fs=(not started[0]): e.matmul(pst[:, h * 128:(h + 1) * 128], buf[:, h * 128:(h + 1) * 128], rhs, start=fs, stop=False, skip_group_check=True)),
                     reads=[bkey] + list(rreads), writes=[pkey])
                started[0] = True
        self.n += 1

    def recip(self):
        S = self.cx.S
        for h in range(4):
            a = h // 2
            c0 = (h % 2) * 129 + 128
            S.op("dve", (lambda e, h=h, a=a, c0=c0: e.tensor_scalar(self.rs[:, h:h + 1], self.acc[a][:, c0:c0 + 1], 1e-30, None, op0=ALU.max)),
                 reads=[("ps_acc", a)], writes=["rs_t"])
        S.op("dve", (lambda e: e.reciprocal(self.rs[:], self.rs[:])), reads=["rs_t"], writes=["rs_t"])

    def finish_simple(self, out_ap, okey):
        S = self.cx.S
        self.recip()
        o = self.no % 2
        self.no += 1
        ob = self.osb[o]
        for h in range(4):
            a = h // 2
            c0 = (h % 2) * 129
            S.op("dve", (lambda e, h=h, a=a, c0=c0: e.tensor_scalar(ob[:, h * 128:(h + 1) * 128], self.acc[a][:, c0:c0 + 128], self.rs[:, h:h + 1], None, op0=ALU.mult)),
                 reads=[("ps_acc", a), "rs_t"], writes=[("osb", o)])
        S.op("sp", (lambda e: e.dma_start(out=out_ap, in_=ob[:])), reads=[("osb", o)], writes=[okey], dsem=("oo", o))


def load_consts(cx, names):
    S = cx.S
    out = {}
    for (name, shape, dt) in names:
        d = cx.din(name, shape, dt)
        t = cx.sb(name + "_sb", shape, dt)
        S.op("sp", (lambda e, d=d, t=t: e.dma_start(out=t[:], in_=d)), writes=[name], dsem=name)
        out[name] = t
    return out


def finish_prog(cx, prefixes):
    S = cx.S
    evs = [v[0] for k, v in S.res.items() if isinstance(k, tuple) and k[0] in prefixes and v[0] is not None]
    S.wait_all("sp", evs)
    S.emit()


def build_p2b(nslot=NSLOT):
    nc = new_nc()
    with ExitStack() as st:
        cx = Ctx(nc, st)
        S = cx.S
        QT = cx.din("QT", [128, NSLOT, 4, 128], BF16)
        KT = cx.din("KT", [128, 4, 8192], BF16)
        V = cx.din("V", [128, 64, 4, 129], BF16)
        oB = cx.dout("oB", [T, 512], BF16)
        C = load_consts(cx, [("flog", [128, 4, 64], F32), ("bf_rep", [128, 4], F32), ("oh", [128, NSLOT, 64], F32),
                             ("qrel_rep", [128, T], F32), ("kcol_rel", [128, 8], F32), ("tri32", [128, 128], F32)])
        q_sb = cx.sb("q_sb", [128, NSLOT, 4, 128], BF16)
        k_sb = cx.sb("k_sb", [128, 4, 8192], BF16)
        v_sb = cx.sb("v_sb", [128, 64, 4, 129], BF16)
        S.op("sp", lambda e: e.dma_start(out=q_sb[:], in_=QT), writes=["q"], dsem="q")
        for h in range(4):
            S.op("sp", (lambda e, h=h: e.dma_start(out=k_sb[:, h, :], in_=KT[:, h, :])), writes=[("k", h)], dsem=("k", h))
        for t8 in range(8):
            S.op("sp", (lambda e, t8=t8: e.dma_start(out=v_sb[:, t8 * 8:(t8 + 1) * 8], in_=V[:, t8 * 8:(t8 + 1) * 8])), writes=[("v", t8)], dsem=("v", t8))
        at = Attn(cx)
        ones32 = cx.sb("ones32", [128, 128], F32)
        S.op("dve", lambda e: e.memset(ones32[:], 1.0), writes=["ones32"])
        xl = cx.sb("xl", [128, 4, 64], F32)
        tot = cx.sb("tot", [128, 4, 64], F32)
        inc = cx.sb("inc", [128, 4, 64], F32)
        off = cx.sb("off", [128, 4, 64], F32)
        Lk = cx.sb("Lk", [128, 4, 64], F32)
        zer = cx.sb("zer", [128, 64], F32)
        t64 = cx.sb("t64", [128, 64], F32)
        lref = cx.sb("lref", [128, NSLOT * 4], F32)
        bb = cx.sb("bb", [128, NSLOT, 4, 64], F32)
        S.op("dve", lambda e: e.memset(zer[:], 0.0), writes=["zer"])
        for h in range(4):
            S.op("dve", (lambda e, h=h: e.tensor_scalar(xl[:, h, :], C["flog"][:, h, :], C["bf_rep"][:, h:h + 1], None, op0=ALU.add)),
                 reads=["flog", "bf_rep"], writes=["xl"])
        S.op("act", lambda e: e.activation(xl[:], xl[:], AF.Exp, scale=-1.0), reads=["xl"], writes=["xl"])
        S.op("act", lambda e: e.activation(xl[:], xl[:], AF.Ln, bias=1.0, scale=1.0), reads=["xl"], writes=["xl"])
        psw = at.ps_s[0]
        pst = at.ps_s[1]
        xl2 = xl[:].rearrange("p h t -> p (h t)")
        S.op("pe", lambda e: e.matmul(psw[:, 0:256], C["tri32"][:], xl2, start=True, stop=True), reads=["xl", "tri32"], writes=[("ps_s", 0)])
        S.op("pe", lambda e: e.matmul(pst[:, 0:256], ones32[:], xl2, start=True, stop=True), reads=["xl", "ones32"], writes=[("ps_s", 1)])
        S.op("dve", lambda e: e.tensor_copy(tot[:].rearrange("p h t -> p (h t)"), pst[:, 0:256]), reads=[("ps_s", 1)], writes=["tot"])
        for h in range(4):
            S.op("dve", (lambda e, h=h: e.tensor_tensor_scan(inc[:, h, :], tot[:, h, :], zer[:], 0.0, op0=ALU.add, op1=ALU.add)),
                 reads=["tot", "zer"], writes=["inc"])
        S.op("dve", lambda e: e.tensor_tensor(off[:], inc[:], tot[:], op=ALU.subtract), reads=["inc", "tot"], writes=["off"])
        S.op("dve", lambda e: e.tensor_tensor(Lk[:].rearrange("p h t -> p (h t)"), psw[:, 0:256], off[:].rearrange("p h t -> p (h t)"), op=ALU.add),
             reads=[("ps_s", 0), "off"], writes=["Lk"])
        for j in range(NSLOT):
            for h in range(4):
                S.op("dve", (lambda e, j=j, h=h: e.tensor_tensor(t64[:], C["oh"][:, j, :], off[:, h, :], op=ALU.mult)), reads=["oh", "off"], writes=["t64"])
                S.op("dve", (lambda e, j=j, h=h: e.reduce_sum(lref[:, j * 4 + h:j * 4 + h + 1], t64[:], axis=AX.X)), reads=["t64"], writes=["lref"])
        for j in range(NSLOT):
            for h in range(4):
                S.op("dve", (lambda e, j=j, h=h: e.tensor_scalar(bb[:, j, h, :], Lk[:, h, :], lref[:, j * 4 + h:j * 4 + h + 1], 30.0, op0=ALU.subtract, op1=ALU.min)),
                     reads=["Lk", "lref"], writes=["bb"])
        for j in range(nslot):
            at.begin_acc()
            nkt = 8 * (j + 1)
            for kt in range(nkt):
                mms = [(h * 128, 128, k_sb[:, h, kt * 128:(kt + 1) * 128], q_sb[:, j, h, :], [("k", h), "q"]) for h in range(4)]
                b = at.scores(mms)
                k = at.exp(b, biases=[bb[:, j, h, kt:kt + 1] for h in range(4)], breads=["bb"])
                masked = kt >= 8 * j
                if masked:
                    at.mask_stt(k, C["qrel_rep"][:, j * 128:(j + 1) * 128], C["kcol_rel"][:, kt - 8 * j:kt - 8 * j + 1], ALU.is_ge, ["qrel_rep", "kcol_rel"])
                at.pv(k, masked, [v_sb[:, kt, h, :] for h in range(4)], [("v", kt // 8)])
            at.finish_simple(oB[j * 128:(j + 1) * 128, :], ("oB", j))
        finish_prog(cx, ("oB",))
    return nc


class TileStream:
    def __init__(self, cx, name, shape, dt, nslots):
        self.cx = cx
        self.name = name
        self.n = nslots
        self.i = 0
        self.bufs = [cx.sb("%s%d" % (name, i), shape, dt) for i in range(nslots)]

    def load(self, src_ap):
        S = self.cx.S
        s = self.i % self.n
        self.i += 1
        buf = self.bufs[s]
        key = (self.name, s)
        S.op("sp", (lambda e: e.dma_start(out=buf[:], in_=src_ap)), writes=[key], dsem=key)
        return buf, key


A_TILES = [(0, w) for w in range(2)] + [(1, w) for w in range(5)] + [(2, w) for w in range(17)]


def build_p2a(nslot=NSLOT):
    nc = new_nc()
    with ExitStack() as st:
        cx = Ctx(nc, st)
        S = cx.S
        QT = cx.din("QT", [128, NSLOT, 12, 128], BF16)
        KT = cx.din("KT", [NSLOT, 24, 128, 4, 128], BF16)
        V = cx.din("V", [NSLOT, 24, 128, 4, 129], BF16)
        oA = cx.dout("oA", [T, 512], BF16)
        C = load_consts(cx, [("amask", [128, 24, 128], BF16)])
        q_sb = cx.sb("q_sb", [128, NSLOT, 12, 128], BF16)
        S.op("sp", lambda e: e.dma_start(out=q_sb[:], in_=QT), writes=["q"], dsem="q")
        at = Attn(cx)
        ks = TileStream(cx, "kst", [128, 4, 128], BF16, 6)
        vs = TileStream(cx, "vst", [128, 4, 129], BF16, 6)
        units = []

        def mk(j, ti):
            g, w = A_TILES[ti]

            def load():
                return (ks.load(KT[j, ti]), vs.load(V[j, ti]))

            def compute(hd):
                (kb, kkey), (vb, vkey) = hd
                if ti == 0:
                    at.begin_acc()
                mms = [(h * 128, 128, kb[:, h, :], q_sb[:, j, g * 4 + h, :], [kkey, "q"]) for h in range(4)]
                b = at.scores(mms)
                k = at.exp(b)
                at.mask_tt(k, C["amask"][:, ti, :], ["amask"])
                at.pv(k, True, [vb[:, h, :] for h in range(4)], [vkey])
                if ti == 23:
                    at.finish_simple(oA[j * 128:(j + 1) * 128, :], ("oA", j))
            return (load, compute)

        for j in range(nslot):
            for ti in range(24):
                units.append(mk(j, ti))
        pipeline(units, depth=4)
        finish_prog(cx, ("oA",))
    return nc


DSA_TOPK = 256


def build_p2c(nslot=NSLOT, topk_rounds=DSA_TOPK // 8):
    nc = new_nc()
    with ExitStack() as st:
        cx = Ctx(nc, st)
        S = cx.S
        QT = cx.din("QT", [128, NSLOT, 4, 128], BF16)
        KT = cx.din("KT", [64, 128, 4, 128], BF16)
        V = cx.din("V", [64, 128, 4, 129], BF16)
        QiT = cx.din("QiT", [128, NSLOT, 8, 128], BF16)
        KiT2 = cx.din("KiT2", [128, 8192], F32)
        oC = cx.dout("oC", [T, 512], BF16)
        C = load_consts(cx, [("wi", [128, NSLOT, 16], F32), ("qrel_col", [128, NSLOT], F32), ("iota_row", [128, 1024], F32),
                             ("ident_bf", [128, 128], BF16)])
        q_sb = cx.sb("q_sb", [128, NSLOT, 4, 128], BF16)
        qi_sb = cx.sb("qi_sb", [128, NSLOT, 8, 128], BF16)
        ki_sb = cx.sb("ki_sb", [128, 8192], BF16)
        S.op("sp", lambda e: e.dma_start(out=q_sb[:], in_=QT), writes=["q"], dsem="q")
        S.op("sp", lambda e: e.dma_start(out=qi_sb[:], in_=QiT), writes=["qi"], dsem="qi")
        score = cx.sb("score", [128, 8192], F32)
        for i in range(4):
            S.op("sp", (lambda e, i=i: e.dma_start(out=score[:, i * 2048:(i + 1) * 2048], in_=KiT2[:, i * 2048:(i + 1) * 2048])),
                 writes=[("score", 4 * i + k_) for k_ in range(4)], dsem=("ki", i))
            S.op("pool", (lambda e, i=i: e.tensor_copy(ki_sb[:, i * 2048:(i + 1) * 2048], score[:, i * 2048:(i + 1) * 2048])),
                 reads=[("score", 4 * i + k_) for k_ in range(4)], writes=[("ki", i)])
        at = Attn(cx)
        ks = TileStream(cx, "kst", [128, 4, 128], BF16, 6)
        vs = TileStream(cx, "vst", [128, 4, 129], BF16, 6)
        maskqk = [cx.sb("maskqk%d" % i, [128, 8192], BF16) for i in range(2)]
        rbuf = [cx.sb("rbuf%d" % i, [128, 512], F32) for i in range(2)]
        pen = cx.sb("pen", [128, 1024], F32)
        c01 = cx.sb("c01", [128, 1024], BF16)
        m8 = cx.sb("m8", [128, 8], F32)
        ps_m = [cx.ps("ps_m%d" % i, [128, 128], BF16) for i in range(2)]
        cnt = [0]
        tcnt = [0]

        def indexer(j):
            N = 1024 * (j + 1)
            mq = maskqk[j % 2]
            mkey = ("maskqk", j % 2)
            def do_head(ch, hh):
                    csl = slice(ch * 512, (ch + 1) * 512)
                    c8, half = hh // 2, hh % 2
                    rows = slice(half * 64, half * 64 + 64)
                    b = cnt[0] % 2
                    cnt[0] += 1
                    ps = at.ps_s[b]
                    S.op("pe", (lambda e, ps=ps, c8=c8, rows=rows: e.matmul(ps[:], qi_sb[rows, j, c8, :], ki_sb[rows, csl], start=True, stop=True)),
                         reads=["qi", ("ki", ch // 4)], writes=[("ps_s", b)])
                    S.op("act", (lambda e, ps=ps, b=b: e.activation(rbuf[b][:], ps[:], AF.Relu)), reads=[("ps_s", b)], writes=[("rbuf", b)])
                    if hh == 0:
                        S.op("dve", (lambda e, b=b: e.tensor_scalar(score[:, csl], rbuf[b][:], C["wi"][:, j, 0:1], None, op0=ALU.mult)),
                             reads=[("rbuf", b), "wi"], writes=[("score", ch)])
                    else:
                        S.op("dve", (lambda e, b=b, hh=hh: e.scalar_tensor_tensor(out=score[:, csl], in0=rbuf[b][:], scalar=C["wi"][:, j, hh:hh + 1], in1=score[:, csl],
                                                                                  op0=ALU.mult, op1=ALU.add)),
                             reads=[("rbuf", b), "wi"], writes=[("score", ch)])

            for ch in range(N // 512):
                for hh in range(16):
                    do_head(ch, hh)
            allsc = [("score", ch) for ch in range(N // 512)]
            lsl = slice(1024 * j, 1024 * (j + 1))
            S.op("dve", (lambda e: e.tensor_scalar(pen[:], C["iota_row"][:], C["qrel_col"][:, j:j + 1], NEG, op0=ALU.is_gt, op1=ALU.mult)),
                 reads=["iota_row", "qrel_col"], writes=["pen"])
            S.op("dve", (lambda e: e.tensor_tensor(score[:, lsl], score[:, lsl], pen[:], op=ALU.add)), reads=["pen"], writes=[("score", 2 * j), ("score", 2 * j + 1)])
            for r in range(topk_rounds):
                S.op("dve", (lambda e: e.max(out=m8[:], in_=score[:, 0:N])), reads=allsc, writes=["m8"])
                S.op("dve", (lambda e: e.match_replace(out=score[:, 0:N], in_to_replace=m8[:], in_values=score[:, 0:N], imm_value=-3e38)),
                     reads=["m8"], writes=allsc)
            S.op("dve", (lambda e: e.tensor_scalar(mq[:, 0:N], score[:, 0:N], -2e38, None, op0=ALU.is_le)), reads=allsc, writes=[mkey])
            S.op("dve", (lambda e: e.tensor_scalar(c01[:], C["iota_row"][:], C["qrel_col"][:, j:j + 1], None, op0=ALU.is_le)),
                 reads=["iota_row", "qrel_col"], writes=["c01"])
            S.op("dve", (lambda e: e.tensor_tensor(mq[:, lsl], mq[:, lsl], c01[:], op=ALU.mult)), reads=["c01"], writes=[mkey])

        def mk(j, kt):
            def load():
                return (ks.load(KT[kt]), vs.load(V[kt]))

            def compute(hd):
                (kb, kkey), (vb, vkey) = hd
                mq = maskqk[j % 2]
                mkey = ("maskqk", j % 2)
                if kt == 0:
                    at.begin_acc()
                mms = [(h * 128, 128, kb[:, h, :], q_sb[:, j, h, :], [kkey, "q"]) for h in range(4)]
                b = at.scores(mms)
                k = at.exp(b)
                tb = tcnt[0] % 2
                tcnt[0] += 1
                S.op("pe", (lambda e: e.transpose(ps_m[tb][:], mq[:, kt * 128:(kt + 1) * 128], C["ident_bf"][:])),
                     reads=[mkey, "ident_bf"], writes=[("ps_m", tb)])
                at.mask_tt(k, ps_m[tb][:], [("ps_m", tb)])
                at.pv(k, True, [vb[:, h, :] for h in range(4)], [vkey])
                if kt == 8 * (j + 1) - 1:
                    at.finish_simple(oC[j * 128:(j + 1) * 128, :], ("oC", j))
            return (load, compute)

        for j in range(nslot):
            indexer(j)
            units = [mk(j, kt) for kt in range(8 * (j + 1))]
            pipeline(units, depth=4)
        finish_prog(cx, ("oC",))
    return nc


GELU_C = 0.7978845608028654


def build_p2d(nslot=NSLOT, debug=False):
    nc = new_nc()
    with ExitStack() as st:
        cx = Ctx(nc, st)
        S = cx.S
        QT = cx.din("QT", [128, NSLOT, 4, 128], BF16)
        KcT = cx.din("KcT", [128, 8192], BF16)
        VcT = cx.din("VcT", [128, 8192], BF16)
        KsT = cx.din("KsT", [128, 8192], BF16)
        Vs = cx.din("Vs", [128, 64, 129], BF16)
        KwT = cx.din("KwT", [128, NSLOT, 640], BF16)
        Vw = cx.din("Vw", [128, NSLOT, 5, 129], BF16)
        w1 = cx.din("w1", [2, 4096, 256], F32)
        w2 = cx.din("w2", [2, 256, 128], F32)
        oD = cx.dout("oD", [T, 512], BF16)
        C = load_consts(cx, [("dwmask", [128, 5, 128], BF16), ("dg", [128, NSLOT, 12], F32), ("peT", [128, 2, 32], F32),
                             ("qpos_rep", [128, T], F32), ("qrel_rep", [128, T], F32), ("qpos_col", [128, NSLOT], F32),
                             ("kcol_rel", [128, 8], F32), ("cend_col", [128, 4], F32), ("jstart_row", [128, 128], F32),
                             ("j0row", [128, 128], F32), ("G", [128, 8192], BF16), ("ovl", [128, 4, 128], BF16),
                             ("ident_bf", [128, 128], BF16)])
        q_sb = cx.sb("q_sb", [128, NSLOT, 4, 128], BF16)
        xc = [cx.sb("xc%d" % i, [128, 8192], BF16) for i in range(2)]
        ks_sb = cx.sb("ks_sb", [128, 8192], BF16)
        vs_sb = cx.sb("vs_sb", [128, 64, 129], BF16)
        kw_sb = cx.sb("kw_sb", [128, NSLOT, 640], BF16)
        vw_sb = cx.sb("vw_sb", [128, NSLOT, 5, 129], BF16)
        S.op("sp", lambda e: e.dma_start(out=q_sb[:], in_=QT), writes=["q"], dsem="q")
        S.op("sp", lambda e: e.dma_start(out=xc[0][:], in_=KcT), writes=[("xc", 0)], dsem=("xc", 0))
        S.op("sp", lambda e: e.dma_start(out=xc[1][:], in_=VcT), writes=[("xc", 1)], dsem=("xc", 1))
        S.op("sp", lambda e: e.dma_start(out=ks_sb[:], in_=KsT), writes=["ks"], dsem="ks")
        for t8 in range(8):
            S.op("sp", (lambda e, t8=t8: e.dma_start(out=vs_sb[:, t8 * 8:(t8 + 1) * 8], in_=Vs[:, t8 * 8:(t8 + 1) * 8])), writes=[("vs", t8)], dsem=("vs", t8))
        S.op("sp", lambda e: e.dma_start(out=kw_sb[:], in_=KwT), writes=["kw"], dsem="kw")
        S.op("sp", lambda e: e.dma_start(out=vw_sb[:], in_=Vw), writes=["vw"], dsem="vw")
        at = Attn(cx)
        ps_imp = cx.ps("ps_imp", [128, 512], F32)
        ps_m = cx.ps("ps_m", [128, 128], BF16)
        ps_m2 = [cx.ps("ps_m2%d" % i, [128, 128], F32) for i in range(2)]

        w1s = cx.sb("w1s", [128, 32, 256], F32)
        w1b = cx.sb("w1b", [128, 32, 256], BF16)
        w2s = cx.sb("w2s", [128, 2, 128], F32)
        w2b = cx.sb("w2b", [128, 2, 128], BF16)
        peb = cx.sb("peb", [128, 2, 32], BF16)
        bias_sb = cx.sb("bias_sb", [128, 2], F32)
        u_sb = cx.sb("u_sb", [128, 512], F32)
        z_sb = cx.sb("z_sb", [128, 512], F32)
        gT = [cx.sb("gT%d" % i, [128, 512], BF16) for i in range(2)]
        kcmp = cx.sb("kcmp", [128, 512], BF16)
        vcmp = cx.sb("vcmp", [128, 4, 129], BF16)
        S.op("dve", lambda e: e.tensor_copy(peb[:], C["peT"][:]), reads=["peT"], writes=["peb"])
        S.op("dve", lambda e: e.memset(kcmp[:], 0.0), writes=["kcmp"])
        S.op("dve", lambda e: e.memset(vcmp[:], 0.0), writes=["vcmp"])
        S.op("dve", lambda e: e.memset(vcmp[:, :, 128:129], 1.0), writes=["vcmp"])
        for i in range(2):
            S.op("dve", (lambda e, i=i: e.memset(gT[i][:], 0.0)), writes=[("gT", i)])

        def compress(which):
            src1 = w1[which].rearrange("(l d) c -> d l c", d=128)
            for l8 in range(4):
                S.op("sp", (lambda e, l8=l8: e.dma_start(out=w1s[:, l8 * 8:(l8 + 1) * 8, :], in_=src1[:, l8 * 8:(l8 + 1) * 8, :])),
                     writes=[("w1s", l8)], dsem=("w1s", l8))
                S.op("pool", (lambda e, l8=l8: e.tensor_copy(w1b[:, l8 * 8:(l8 + 1) * 8, :], w1s[:, l8 * 8:(l8 + 1) * 8, :])),
                     reads=[("w1s", l8)], writes=[("w1b", l8)])
            S.op("sp", lambda e: e.dma_start(out=w2s[:], in_=w2[which].rearrange("(cc c) d -> c cc d", c=128)), writes=["w2s"], dsem="w2s")
            S.op("pool", lambda e: e.tensor_copy(w2b[:], w2s[:]), reads=["w2s"], writes=["w2b"])
            xv = xc[which][:].rearrange("p (n s) -> p n s", s=16)
            ph0 = at.ps_s[0]
            pb = at.ps_s[1]
            for cc in range(2):
                for l in range(32):
                    S.op("pe", (lambda e, l=l, cc=cc: e.matmul(ph0[:, 0:511], w1b[:, l, cc * 128:(cc + 1) * 128], xv[:, l // 16:l // 16 + 511, l % 16],
                                                              start=(l == 0), stop=(l == 31))),
                         reads=[("w1b", l // 8), ("xc", which)], writes=[("ps_s", 0)])
                for l in range(32):
                    S.op("pe", (lambda e, l=l, cc=cc: e.matmul(pb[:, 0:1], w1b[:, l, cc * 128:(cc + 1) * 128], peb[:, which, l:l + 1],
                                                              start=(l == 0), stop=(l == 31))),
                         reads=[("w1b", l // 8), "peb"], writes=[("ps_s", 1)])
                S.op("dve", (lambda e, cc=cc: e.tensor_copy(bias_sb[:, cc:cc + 1], pb[:, 0:1])), reads=[("ps_s", 1)], writes=["bias_sb"])
                S.op("act", (lambda e, cc=cc: e.activation(u_sb[:, 0:511], ph0[:, 0:511], AF.Identity, bias=bias_sb[:, cc:cc + 1], scale=1.0)),
                     reads=[("ps_s", 0), "bias_sb"], writes=["u_sb"])
                S.op("dve", lambda e: e.tensor_tensor(z_sb[:, 0:511], u_sb[:, 0:511], u_sb[:, 0:511], op=ALU.mult), reads=["u_sb"], writes=["z_sb"])
                S.op("dve", lambda e: e.tensor_scalar(z_sb[:, 0:511], z_sb[:, 0:511], 0.044715, 1.0, op0=ALU.mult, op1=ALU.add), reads=["z_sb"], writes=["z_sb"])
                S.op("dve", lambda e: e.tensor_tensor(z_sb[:, 0:511], z_sb[:, 0:511], u_sb[:, 0:511], op=ALU.mult), reads=["z_sb", "u_sb"], writes=["z_sb"])
                S.op("act", lambda e: e.activation(z_sb[:, 0:511], z_sb[:, 0:511], AF.Sigmoid, scale=2.0 * GELU_C), reads=["z_sb"], writes=["z_sb"])
                S.op("dve", (lambda e, cc=cc: e.tensor_tensor(gT[cc][:, 0:511], u_sb[:, 0:511], z_sb[:, 0:511], op=ALU.mult)),
                     reads=["z_sb", "u_sb"], writes=[("gT", cc)])
            if which == 0:
                for cc in range(2):
                    S.op("pe", (lambda e, cc=cc: e.matmul(ph0[:, 0:511], w2b[:, cc, :], gT[cc][:, 0:511], start=(cc == 0), stop=(cc == 1))),
                         reads=["w2b", ("gT", cc)], writes=[("ps_s", 0)])
                S.op("act", lambda e: e.copy(kcmp[:, 0:511], ph0[:, 0:511]), reads=[("ps_s", 0)], writes=["kcmp"])
            else:
                for nt in range(4):
                    M = 128 if nt < 3 else 127
                    ph = at.ps_s[nt % 2]
                    for cc in range(2):
                        S.op("pe", (lambda e, cc=cc, nt=nt, M=M, ph=ph: e.matmul(ph[0:M, 0:128], gT[cc][:, nt * 128:nt * 128 + M], w2b[:, cc, :], start=(cc == 0), stop=(cc == 1))),
                             reads=["w2b", ("gT", cc)], writes=[("ps_s", nt % 2)])
                    S.op("act", (lambda e, nt=nt, M=M, ph=ph: e.copy(vcmp[0:M, nt, 0:128], ph[0:M, 0:128])), reads=[("ps_s", nt % 2)], writes=["vcmp"])

        compress(0)
        compress(1)
        if debug:
            dk = cx.dout("dbg_k", [128, 512], BF16)
            dv = cx.dout("dbg_v", [128, 4, 129], BF16)
            S.op("sp", lambda e: e.dma_start(out=dk, in_=kcmp[:]), reads=["kcmp"], writes=[("oD", "dk")], dsem="dbgk")
            S.op("sp", lambda e: e.dma_start(out=dv, in_=vcmp[:]), reads=["vcmp"], writes=[("oD", "dv")], dsem="dbgv")
            dxc = cx.dout("dbg_xc", [128, 64], BF16)
            S.op("sp", lambda e: e.dma_start(out=dxc, in_=xc[1][:, 0:64]), reads=[("xc", 1)], writes=[("oD", "dxc")], dsem="dbgxc")
            du = cx.dout("dbg_u", [128, 511], F32)
            S.op("sp", lambda e: e.dma_start(out=du, in_=u_sb[:, 0:511]), reads=["u_sb"], writes=[("oD", "du")], dsem="dbgu")
            dgt = cx.dout("dbg_g", [2, 128, 512], BF16)
            for i in range(2):
                S.op("sp", (lambda e, i=i: e.dma_start(out=dgt[i], in_=gT[i][:])), reads=[("gT", i)], writes=[("oD", "dg", i)], dsem=("dbgg", i))

        gsig = cx.sb("gsig", [128, 12], F32)
        coef = cx.sb("coef", [128, 4], F32)
        accO = cx.sb("accO", [128, 512], F32)
        imp = cx.sb("imp", [128, 128], F32)
        nD = cx.sb("nD", [128, 128], F32)
        adm = cx.sb("adm", [128, 128], F32)
        frc = cx.sb("frc", [128, 128], F32)
        val = cx.sb("val", [128, 128], F32)
        wrk = cx.sb("wrk", [128, 128], F32)
        m8 = cx.sb("m8", [128, 8], F32)
        thr = cx.sb("thr", [128, 1], F32)
        sel = cx.sb("sel", [128, 128], BF16)
        selT = cx.sb("selT", [128, 128], BF16)
        mcnt = [0]

        def coef_for(branch):
            at.recip()
            g3 = gsig[:].rearrange("p (h b) -> p h b", b=3)
            S.op("dve", (lambda e: e.tensor_tensor(coef[:], at.rs[:], g3[:, :, branch], op=ALU.mult)), reads=["rs_t", "gsig"], writes=["coef"])

        def do_slot(j):
            jsl = slice(j * 128, (j + 1) * 128)
            q512 = q_sb[:, j].rearrange("p h q -> p (h q)")
            S.op("act", (lambda e: e.activation(gsig[:], C["dg"][:, j, :], AF.Sigmoid)), reads=["dg"], writes=["gsig"])
            at.begin_acc()
            imp_started = [False]
            for nt in range(4):
                b = at.scores([(0, 512, kcmp[:, nt * 128:(nt + 1) * 128], q512, ["kcmp", "q"])])
                k = at.exp(b)
                at.mask_stt(k, C["qpos_rep"][:, jsl], C["cend_col"][:, nt:nt + 1], ALU.is_ge, ["qpos_rep", "cend_col"])
                at.pv(k, True, [vcmp[:, nt, :]] * 4, ["vcmp"], extra=(ps_imp, "ps_imp", C["ovl"][:, nt, :], ["ovl"], imp_started))
            coef_for(0)
            for h in range(4):
                a, c0 = h // 2, (h % 2) * 129
                S.op("dve", (lambda e, h=h, a=a, c0=c0: e.tensor_scalar(accO[:, h * 128:(h + 1) * 128], at.acc[a][:, c0:c0 + 128], coef[:, h:h + 1], None, op0=ALU.mult)),
                     reads=[("ps_acc", a), "coef"], writes=["accO"])
            S.op("dve", (lambda e: e.tensor_scalar(imp[:], ps_imp[:, 0:128], at.rs[:, 0:1], None, op0=ALU.mult)), reads=["ps_imp", "rs_t"], writes=["imp"])
            for h in range(1, 4):
                S.op("dve", (lambda e, h=h: e.scalar_tensor_tensor(out=imp[:], in0=ps_imp[:, h * 128:(h + 1) * 128], scalar=at.rs[:, h:h + 1], in1=imp[:], op0=ALU.mult, op1=ALU.add)),
                     reads=["ps_imp", "rs_t"], writes=["imp"])
            S.op("dve", (lambda e: e.tensor_scalar(nD[:], C["jstart_row"][:], C["qpos_col"][:, j:j + 1], None, op0=ALU.subtract)),
                 reads=["jstart_row", "qpos_col"], writes=["nD"])
            S.op("dve", (lambda e: e.tensor_scalar(adm[:], nD[:], 0.0, None, op0=ALU.is_le)), reads=["nD"], writes=["adm"])
            S.op("dve", (lambda e: e.scalar_tensor_tensor(out=frc[:], in0=nD[:], scalar=-128.0, in1=adm[:], op0=ALU.is_gt, op1=ALU.mult)),
                 reads=["nD", "adm"], writes=["frc"])
            S.op("dve", (lambda e: e.tensor_tensor(frc[:], frc[:], C["j0row"][:], op=ALU.max)), reads=["j0row"], writes=["frc"])
            S.op("dve", (lambda e: e.tensor_tensor(val[:], imp[:], adm[:], op=ALU.mult)), reads=["imp", "adm"], writes=["val"])
            S.op("dve", (lambda e: e.tensor_scalar(adm[:], adm[:], -1.0, 1e30, op0=ALU.add, op1=ALU.mult)), reads=["adm"], writes=["adm"])
            S.op("dve", (lambda e: e.tensor_tensor(val[:], val[:], adm[:], op=ALU.add)), reads=["adm"], writes=["val"])
            S.op("dve", (lambda e: e.scalar_tensor_tensor(out=val[:], in0=frc[:], scalar=1e9, in1=val[:], op0=ALU.mult, op1=ALU.add)),
                 reads=["frc"], writes=["val"])
            S.op("dve", (lambda e: e.max(out=m8[:], in_=val[:])), reads=["val"], writes=["m8"])
            S.op("dve", (lambda e: e.match_replace(out=wrk[:], in_to_replace=m8[:], in_values=val[:], imm_value=-3e38)), reads=["m8", "val"], writes=["wrk"])
            S.op("dve", (lambda e: e.max(out=m8[:], in_=wrk[:])), reads=["wrk"], writes=["m8"])
            S.op("dve", (lambda e: e.tensor_scalar(thr[:], m8[:, 7:8], -1e29, None, op0=ALU.max)), reads=["m8"], writes=["thr"])
            S.op("dve", (lambda e: e.tensor_scalar(sel[:], val[:], thr[:], None, op0=ALU.is_ge)), reads=["val", "thr"], writes=["sel"])
            S.op("pe", (lambda e: e.transpose(ps_m[:], sel[:], C["ident_bf"][:])), reads=["sel", "ident_bf"], writes=["ps_m"])
            S.op("act", (lambda e: e.copy(selT[:], ps_m[:])), reads=["ps_m"], writes=["selT"])
            at.begin_acc()
            for kt in range(8 * (j + 1)):
                b = at.scores([(0, 512, ks_sb[:, kt * 128:(kt + 1) * 128], q512, ["ks", "q"])])
                k = at.exp(b)
                mb = mcnt[0] % 2
                mcnt[0] += 1
                S.op("pe", (lambda e, kt=kt, mb=mb: e.matmul(ps_m2[mb][:], C["G"][:, kt * 128:(kt + 1) * 128], selT[:], start=True, stop=True)),
                     reads=["G", "selT"], writes=[("ps_m2", mb)])
                at.mask_tt(k, ps_m2[mb][:], [("ps_m2", mb)])
                if kt >= 8 * j:
                    at.mask_stt(k, C["qrel_rep"][:, jsl], C["kcol_rel"][:, kt - 8 * j:kt - 8 * j + 1], ALU.is_ge, ["qrel_rep", "kcol_rel"], inplace_pt=True)
                at.pv(k, True, [vs_sb[:, kt, :]] * 4, [("vs", kt // 8)])
            coef_for(1)
            for h in range(4):
                a, c0 = h // 2, (h % 2) * 129
                S.op("dve", (lambda e, h=h, a=a, c0=c0: e.scalar_tensor_tensor(out=accO[:, h * 128:(h + 1) * 128], in0=at.acc[a][:, c0:c0 + 128], scalar=coef[:, h:h + 1],
                                                                               in1=accO[:, h * 128:(h + 1) * 128], op0=ALU.mult, op1=ALU.add)),
                     reads=[("ps_acc", a), "coef"], writes=["accO"])
            at.begin_acc()
            for w in range(5):
                b = at.scores([(0, 512, kw_sb[:, j, w * 128:(w + 1) * 128], q512, ["kw", "q"])])
                k = at.exp(b)
                at.mask_tt(k, C["dwmask"][:, w, :], ["dwmask"])
                at.pv(k, True, [vw_sb[:, j, w, :]] * 4, ["vw"])
            coef_for(2)
            o = at.no % 2
            at.no += 1
            ob = at.osb[o]
            for h in range(4):
                a, c0 = h // 2, (h % 2) * 129
                S.op("dve", (lambda e, h=h, a=a, c0=c0: e.scalar_tensor_tensor(out=ob[:, h * 128:(h + 1) * 128], in0=at.acc[a][:, c0:c0 + 128], scalar=coef[:, h:h + 1],
                                                                               in1=accO[:, h * 128:(h + 1) * 128], op0=ALU.mult, op1=ALU.add)),
                     reads=[("ps_acc", a), "coef", "accO"], writes=[("osb", o)])
            S.op("sp", (lambda e: e.dma_start(out=oD[jsl, :], in_=ob[:])), reads=[("osb", o)], writes=[("oD", j)], dsem=("oo", o))

        for j in range(nslot):
            do_slot(j)
        finish_prog(cx, ("oD",))
    return nc


NCORES = 8
SHARED_OUT = False
TILE_RS = {slot_tile(c, j): (c, j) for c in range(NCORES) for j in range(NSLOT)}
KX_ROWS = 1408
KX_OFF = {"bk": 0, "ck": 512, "dkc": 1024, "dvc": 1152, "dks": 1280}
FMQ_OFF = {"aq": 0, "bq": 1536, "cq": 2048, "cqi": 2560, "dq": 3584}
VX_COLS = 1152
VX_OFF = {"bv": 0, "cv": 512, "dvs": 1024}


def fused_chunks():
    ch = []
    c = 0
    for (name, c0, n, rk) in IN_GROUPS:
        for i in range(n // 128):
            if name in FMQ_OFF:
                dest = ("fmq", FMQ_OFF[name] + i * 128)
            elif name in KX_OFF:
                dest = ("kx", KX_OFF[name] + i * 128)
            elif name in VX_OFF:
                dest = ("tm", "vx", VX_OFF[name] + i * 128)
            elif name == "ak":
                dest = ("tm", "akx%d" % (i // 4), (i % 4) * 128)
            elif name == "av":
                dest = ("tm", "avx%d" % (i // 4), (i % 4) * 128)
            elif name == "dkw":
                dest = ("tm", "kwx", 0)
            elif name == "dvw":
                dest = ("tm", "vwx", 0)
            else:
                raise AssertionError(name)
            ch.append((c, 128, rk, dest))
            c += 128
    ch.append((c, 96, "rL", ("last",)))
    return ch


def phase_p1(cx, Dm):
    S = cx.S
    xT, gcol, w, cs, rmat = Dm["xT"], Dm["gcol"], Dm["w"], Dm["cs"], Dm["rmat"]
    x_sb = cx.sb("x_sb", [128, KC, T], F32)
    h_sb = cx.sb("h_sb", [128, KC, T], BF16)
    g_sb = cx.sb("g_sb", [128, KC], F32)
    cs_sb = cx.sb("cs_sb", [128, 6, T], F32)
    r32 = cx.sb("r32", [128, 3, 128], F32)
    rbf = cx.sb("rbf", [128, 3, 128], BF16)
    ones32 = cx.sb("ones32", [128, 128], F32)
    id32 = cx.sb("id32", [128, 128], F32)
    idbf = cx.sb("idbf", [128, 128], BF16)
    sq = [cx.sb("sq%d" % i, [128, TG], F32) for i in range(2)]
    rstd = cx.sb("rstd", [128, TG], F32)
    ysb = [cx.sb("ysb%d" % i, [128, TG], BF16) for i in range(2)]
    t1 = [cx.sb("t1_%d" % i, [128, TG], F32) for i in range(2)]
    t2 = [cx.sb("t2_%d" % i, [128, TG], F32) for i in range(2)]
    obf = [cx.sb("obf%d" % i, [128, TG], BF16) for i in range(4)]
    o32 = [cx.sb("o32_%d" % i, [128, TG], F32) for i in range(2)]
    tmb = [cx.sb("tmb%d" % i, [128, 4, 128], BF16) for i in range(2)]
    tm32 = [cx.sb("tm32_%d" % i, [128, 4, 96], F32) for i in range(2)]
    ps_y = [cx.ps("ps_y%d" % i, [128, TG], F32) for i in range(2)]
    ps_r = [cx.ps("ps_r%d" % i, [128, TG], F32) for i in range(2)]
    ps_ss = cx.ps("ps_ss", [128, TG], F32)
    ps_t = [cx.ps("ps_t%d" % i, [128, 1024], BF16) for i in range(2)]
    ws = WStream(cx, nslots=4)

    S.dma_many("sp", [(lambda e, kc=kc: e.dma_start(out=x_sb[:, kc, :], in_=xT[kc * 128:(kc + 1) * 128, :])) for kc in range(KC)],
               [[("x", kc)] for kc in range(KC)], "xin")
    S.op("sp", lambda e: e.dma_start(out=g_sb[:], in_=gcol), writes=["g"], dsem="g")
    S.op("sp", lambda e: e.dma_start(out=cs_sb[:], in_=cs.rearrange("s p t -> p s t")), writes=["cs"], dsem="cs")
    S.op("sp", lambda e: e.dma_start(out=r32[:], in_=rmat.rearrange("s p t -> p s t")), writes=["r32"], dsem="r32")
    S.op("sp", lambda e: e.dma_start(out=id32[:], in_=Dm["ident32"]), writes=["id32"], dsem="id32")
    S.op("dve", lambda e: e.tensor_copy(rbf[:], r32[:]), reads=["r32"], writes=["rbf"])
    S.op("dve", lambda e: e.tensor_copy(idbf[:], id32[:]), reads=["id32"], writes=["idbf"])
    S.op("dve", lambda e: e.memset(ones32[:], 1.0), writes=["ones32"])
    rmsnorm_fm(cx, x_sb, "x", g_sb, "g", h_sb, "h", ones32, ps_ss, sq, rstd)
    S.dma_many("sp", [(lambda e, kc=kc: e.dma_start(out=Dm["hT"][kc * 128:(kc + 1) * 128, :], in_=h_sb[:, kc, :])) for kc in range(KC)],
               [[("hT_d", kc)] for kc in range(KC)], "hTo", reads_list=[[("h", kc)] for kc in range(KC)])

    cnt = [0]

    def mk_unit(c0, M, rk, dest):
        def load():
            return ws.load(w, 0, KC, c0, M)

        def compute(hd):
            bf, bkey = hd
            for tg in range(NTG):
                do_tg(bf, bkey, tg)

        def do_tg(bf, bkey, tg):
            tsl = slice(tg * TG, (tg + 1) * TG)
            k = cnt[0]
            cnt[0] += 1
            b = k % 2
            py = ps_y[b]
            for kc in range(KC):
                S.op("pe", (lambda e, kc=kc: e.matmul(py[0:M, :], bf[:, kc, 0:M], h_sb[:, kc, tsl], start=(kc == 0), stop=(kc == KC - 1))),
                     reads=[bkey, ("h", kc)], writes=[("ps_y", b)])
            last = dest[0] == "last"
            if not last:
                ob = obf[k % 4]
                okey = ("obf", k % 4)
            else:
                ob = o32[b]
                okey = ("o32", b)
            if rk is None:
                S.op("act", (lambda e: e.copy(ob[0:M, :], py[0:M, :])), reads=[("ps_y", b)], writes=[okey])
            else:
                ri = {"r128": 0, "r64": 1, "rL": 1}[rk]
                ci_ = {"r128": 0, "r64": 2, "rL": 4}[rk]
                pr = ps_r[b]
                S.op("act", (lambda e: e.copy(ysb[b][0:M, :], py[0:M, :])), reads=[("ps_y", b)], writes=[("ysb", b)])
                S.op("pe", (lambda e: e.matmul(pr[0:M, :], rbf[0:M, ri, 0:M], ysb[b][0:M, :], start=True, stop=True)),
                     reads=[("ysb", b), "rbf"], writes=[("ps_r", b)])
                S.op("dve", (lambda e: e.tensor_tensor(t1[b][0:M, :], py[0:M, :], cs_sb[0:M, ci_, tsl], op=ALU.mult)),
                     reads=[("ps_y", b), "cs"], writes=[("t1", b)])
                S.op("dve", (lambda e: e.tensor_tensor(t2[b][0:M, :], pr[0:M, :], cs_sb[0:M, ci_ + 1, tsl], op=ALU.mult)),
                     reads=[("ps_r", b), "cs"], writes=[("t2", b)])
                S.op("pool", (lambda e: e.tensor_tensor(ob[0:M, :], t1[b][0:M, :], t2[b][0:M, :], op=ALU.add)),
                     reads=[("t1", b), ("t2", b)], writes=[okey])
            if dest[0] in ("fmq", "kx"):
                dst = Dm[dest[0]]
                r0 = dest[1]
                S.op("sp", (lambda e: e.dma_start(out=dst[r0:r0 + M, tsl], in_=ob[0:M, :])),
                     reads=[okey], writes=[(dest[0], r0, tg)], dsem=("fmo", k % 4))
            elif dest[0] == "tm":
                dst = Dm[dest[1]]
                col0 = dest[2]
                pt = ps_t[b]
                for i in range(4):
                    S.op("pe", (lambda e, i=i: e.transpose(pt[:, i * 128:(i + 1) * 128], ob[:, i * 128:(i + 1) * 128], idbf[:])),
                         reads=[okey, "idbf"], writes=[("ps_t", b)])
                tb = tmb[b]
                S.op("act", (lambda e: e.copy(tb[:].rearrange("p i d -> p (i d)"), pt[:, 0:512])), reads=[("ps_t", b)], writes=[("tmb", b)])
                S.op("sp", (lambda e: e.dma_start(out=dst[tg * 512:(tg + 1) * 512, col0:col0 + 128].rearrange("(i p) d -> p i d", p=128), in_=tb[:])),
                     reads=[("tmb", b)], writes=[(dest[1], col0, tg)], dsem=("tmo", b))
            else:
                S.op("sp", (lambda e: e.dma_start(out=Dm["sx"][0:64, tsl], in_=ob[0:64, :])), reads=[okey], writes=[("sx", tg)], dsem=("smo", tg))
                pt32 = ps_r[b]
                for i in range(4):
                    S.op("pe", (lambda e, i=i: e.transpose(pt32[:, i * 96:(i + 1) * 96], ob[0:96, i * 128:(i + 1) * 128], id32[0:96, 0:96])),
                         reads=[okey, "id32"], writes=[("ps_r", b)])
                tb = tm32[b]
                S.op("act", (lambda e: e.copy(tb[:].rearrange("p i d -> p (i d)"), pt32[:, 0:384])), reads=[("ps_r", b)], writes=[("tm32", b)])
                S.op("sp", (lambda e: e.dma_start(out=Dm["smt"][tg * 512:(tg + 1) * 512, :].rearrange("(i p) d -> p i d", p=128), in_=tb[:])),
                     reads=[("tm32", b)], writes=[("smt", tg)], dsem=("smto", tg))
        return (load, compute)

    units = [mk_unit(*ch) for ch in fused_chunks()]
    pipeline(units, depth=2)


def attn_out_T(at, cx, ob, obkey, dest, dkey):
    S = cx.S
    pso, pkey = at.ps_o, at.ps_o_key
    for h in range(4):
        S.op("pe", (lambda e, h=h: e.transpose(pso[:, h * 128:(h + 1) * 128], ob[:, h * 128:(h + 1) * 128], at.ident[:])),
             reads=[obkey, at.ident_key], writes=[pkey])
    o = at.nt % 2
    at.nt += 1
    tb = at.obT[o]
    S.op("act", (lambda e: e.copy(tb[:], pso[:, 0:512])), reads=[pkey], writes=[("obT", o)])
    S.op("sp", (lambda e: e.dma_start(out=dest, in_=tb[:].rearrange("p (h q) -> p h q", h=4))), reads=[("obT", o)], writes=[dkey], dsem=("obTo", o))


def attn_setup_out(at, cx, ps_o, ps_o_key, ident, ident_key):
    at.ps_o, at.ps_o_key, at.ident, at.ident_key = ps_o, ps_o_key, ident, ident_key
    at.obT = [cx.sb("obT%d" % i, [128, 512], BF16) for i in range(2)]
    at.nt = 0


def finish_T(at, cx, dest, dkey):
    S = cx.S
    at.recip()
    o = at.no % 2
    at.no += 1
    ob = at.osb[o]
    for h in range(4):
        a = h // 2
        c0 = (h % 2) * 129
        S.op("dve", (lambda e, h=h, a=a, c0=c0: e.tensor_scalar(ob[:, h * 128:(h + 1) * 128], at.acc[a][:, c0:c0 + 128], at.rs[:, h:h + 1], None, op0=ALU.mult)),
             reads=[("ps_acc", a), "rs_t"], writes=[("osb", o)])
    attn_out_T(at, cx, ob, ("osb", o), dest, dkey)


def mask_stt2(at, cx, k, scalar, mask_ap, reads):
    S = cx.S
    src = at.pe_buf[k]
    dst = at.pt_buf[k]
    S.op("dve", (lambda e: e.scalar_tensor_tensor(out=dst[:].rearrange("p (h q) -> p h q", h=4), in0=src[:].rearrange("p (h q) -> p h q", h=4), scalar=scalar,
                                                  in1=mask_ap.unsqueeze(1).to_broadcast([128, 4, 128]), op0=ALU.mult, op1=ALU.mult)),
         reads=[("pe_buf", k)] + list(reads), writes=[("pt_buf", k)])


def br_dest(brT, n, j):
    return brT[n * 512:(n + 1) * 512, j * 128:(j + 1) * 128].rearrange("(h d) q -> d h q", h=4)


def load_q(cx, q_sb, fmq, r0, H, key):
    S = cx.S
    src = fmq[r0:r0 + H * 128, :].rearrange("(h d) (j q) -> d j h q", h=H, j=NSLOT)
    S.dma_many("sp", [(lambda e, j=j: e.dma_start(out=q_sb[:, j], in_=src[:, j])) for j in range(NSLOT)], [[key]] * NSLOT, key)


def phase_a(cx, Dm, nslot=NSLOT):
    S = cx.S
    C = load_consts(cx, [("amask", [128, 24, 128], BF16), ("idxA", [128, NSLOT * 24], U32), ("validA", [128, NSLOT * 24], F32),
                         ("identbf", [128, 128], BF16)])
    q_sb = cx.sb("q_sb", [128, NSLOT, 12, 128], BF16)
    load_q(cx, q_sb, Dm["fmq"], FMQ_OFF["aq"], 12, "q")
    at = Attn(cx)
    ps_o = cx.ps("ps_o", [128, 1024], BF16)
    ps_kt = [cx.ps("ps_kt%d" % i, [128, 1024], BF16) for i in range(2)]
    attn_setup_out(at, cx, ps_o, "ps_o", C["identbf"], "identbf")
    ktm = TileStream(cx, "ktm", [128, 512], BF16, 6)
    vtm = TileStream(cx, "vtm", [128, 512], BF16, 6)
    vst = TileStream(cx, "vst", [128, 4, 129], BF16, 6)
    kti = [cx.sb("kti%d" % i, [128, 4, 128], BF16) for i in range(3)]
    for i, b in enumerate(vst.bufs):
        S.op("dve", (lambda e, b=b: e.memset(b[:, :, 128:129], 1.0)), writes=[("vst", i)])
    kc = [0]
    units = []

    def mk(j, ti):
        g, w = A_TILES[ti]
        col = j * 24 + ti

        def gather(ts, src):
            s = ts.i % ts.n
            ts.i += 1
            buf = ts.bufs[s]
            key = (ts.name, s)
            S.op("pool", (lambda e: e.indirect_dma_start(out=buf[:], out_offset=None, in_=src,
                                                         in_offset=bass.IndirectOffsetOnAxis(ap=C["idxA"][:, col:col + 1], axis=0))),
                 reads=["idxA"], writes=[key], dsem=key)
            return buf, key

        def load():
            kk_ = gather(ktm, Dm["akxg%d" % g])
            vt, vtkey = gather(vtm, Dm["avxg%d" % g])
            s2 = vst.i % vst.n
            vst.i += 1
            vb = vst.bufs[s2]
            vkey = ("vst", s2)
            S.op("pool", (lambda e: e.tensor_copy(vb[:, :, 0:128], vt[:].rearrange("p (h d) -> p h d", h=4))), reads=[vtkey], writes=[vkey])
            return (kk_, (vb, vkey))

        def compute(hd):
            (kb, kkey), (vb, vkey) = hd
            if ti == 0:
                at.begin_acc()
            kk = kc[0]
            kc[0] += 1
            pk = ps_kt[kk % 2]
            for h in range(4):
                S.op("pe", (lambda e, h=h: e.transpose(pk[:, h * 128:(h + 1) * 128], kb[:, h * 128:(h + 1) * 128], C["identbf"][:])), reads=[kkey, "identbf"], writes=[("ps_kt", kk % 2)])
            kt = kti[kk % 3]
            S.op("act", (lambda e: e.copy(kt[:].rearrange("p h k -> p (h k)"), pk[:, 0:512])), reads=[("ps_kt", kk % 2)], writes=[("kti", kk % 3)])
            mms = [(h * 128, 128, kt[:, h, :], q_sb[:, j, g * 4 + h, :], [("kti", kk % 3), "q"]) for h in range(4)]
            b = at.scores(mms)
            k = at.exp(b)
            mask_stt2(at, cx, k, C["validA"][:, col:col + 1], C["amask"][:, ti, :], ["amask", "validA"])
            at.pv(k, True, [vb[:, h, :] for h in range(4)], [vkey])
            if ti == 23:
                finish_T(at, cx, br_dest(Dm["brT"], 0, j), ("brT", 0, j))
        return (load, compute)

    for j in range(nslot):
        for ti in range(24):
            units.append(mk(j, ti))
    pipeline(units, depth=4)


def ktile_src(Dm, name, t):
    r, j = TILE_RS[t]
    r0 = r * KX_ROWS + KX_OFF[name]
    return Dm["kxg"][r0:r0 + 512, j * 128:(j + 1) * 128].rearrange("(h d) k -> d h k", h=4)


def k1_src(Dm, name, t):
    r, j = TILE_RS[t]
    r0 = r * KX_ROWS + KX_OFF[name]
    return Dm["kxg"][r0:r0 + 128, j * 128:(j + 1) * 128]


def vtile_src(Dm, name, t, H=4):
    r, j = TILE_RS[t]
    c0 = VX_OFF[name]
    return Dm["vxg"][r * 1024 + j * 128:r * 1024 + (j + 1) * 128, c0:c0 + H * 128]


def vload(cx, ts, src, H=4):
    S = cx.S
    s = ts.i % ts.n
    ts.i += 1
    buf = ts.bufs[s]
    key = (ts.name, s)
    S.op("sp", (lambda e: e.dma_start(out=buf[:, :, 0:128], in_=src.rearrange("p (h d) -> p h d", h=H))), writes=[key], dsem=key)
    return buf, key


def phase_b(cx, Dm, nslot=NSLOT):
    S = cx.S
    C = load_consts(cx, [("bf_rep", [128, 4], F32), ("oh", [128, NSLOT, 64], F32),
                         ("qrel_rep", [128, T], F32), ("kcol_rel", [128, 8], F32), ("tri32", [128, 128], F32), ("identbf", [128, 128], BF16)])
    q_sb = cx.sb("q_sb", [128, NSLOT, 4, 128], BF16)
    load_q(cx, q_sb, Dm["fmq"], FMQ_OFF["bq"], 4, "q")
    flog = cx.sb("flog", [128, 64, 4], F32)
    fl_dmas = []
    for t in range(64):
        r, j = TILE_RS[t]
        fl_dmas.append((lambda e, t=t, r=r, j=j: e.dma_start(out=flog[:, t, :], in_=Dm["smtg"][r * 1024 + j * 128:r * 1024 + (j + 1) * 128, 64:68])))
    S.dma_many("sp", fl_dmas, [["flog"]] * 64, "flog")
    at = Attn(cx)
    ps_o = cx.ps("ps_o", [128, 1024], BF16)
    attn_setup_out(at, cx, ps_o, "ps_o", C["identbf"], "identbf")
    kst = TileStream(cx, "kst", [128, 4, 128], BF16, 6)
    vst = TileStream(cx, "vst", [128, 4, 129], BF16, 6)
    for i, b in enumerate(vst.bufs):
        S.op("dve", (lambda e, b=b: e.memset(b[:, :, 128:129], 1.0)), writes=[("vst", i)])
    ones32 = cx.sb("ones32", [128, 128], F32)
    S.op("dve", lambda e: e.memset(ones32[:], 1.0), writes=["ones32"])
    xl = cx.sb("xl", [128, 4, 64], F32)
    tot = cx.sb("tot", [128, 4, 64], F32)
    inc = cx.sb("inc", [128, 4, 64], F32)
    off = cx.sb("off", [128, 4, 64], F32)
    Lk = cx.sb("Lk", [128, 4, 64], F32)
    zer = cx.sb("zer", [128, 64], F32)
    t64 = cx.sb("t64", [128, 64], F32)
    lref = cx.sb("lref", [128, NSLOT * 4], F32)
    bb = cx.sb("bb", [128, NSLOT, 4, 64], F32)
    S.op("dve", lambda e: e.memset(zer[:], 0.0), writes=["zer"])
    for h in range(4):
        S.op("dve", (lambda e, h=h: e.tensor_scalar(xl[:, h, :], flog[:, :, h], C["bf_rep"][:, h:h + 1], None, op0=ALU.add)),
             reads=["flog", "bf_rep"], writes=["xl"])
    S.op("act", lambda e: e.activation(xl[:], xl[:], AF.Exp, scale=-1.0), reads=["xl"], writes=["xl"])
    S.op("act", lambda e: e.activation(xl[:], xl[:], AF.Ln, bias=1.0, scale=1.0), reads=["xl"], writes=["xl"])
    psw = at.ps_s[0]
    pst = at.ps_s[1]
    xl2 = xl[:].rearrange("p h t -> p (h t)")
    S.op("pe", lambda e: e.matmul(psw[:, 0:256], C["tri32"][:], xl2, start=True, stop=True), reads=["xl", "tri32"], writes=[("ps_s", 0)])
    S.op("pe", lambda e: e.matmul(pst[:, 0:256], ones32[:], xl2, start=True, stop=True), reads=["xl", "ones32"], writes=[("ps_s", 1)])
    S.op("dve", lambda e: e.tensor_copy(tot[:].rearrange("p h t -> p (h t)"), pst[:, 0:256]), reads=[("ps_s", 1)], writes=["tot"])
    for h in range(4):
        S.op("dve", (lambda e, h=h: e.tensor_tensor_scan(inc[:, h, :], tot[:, h, :], zer[:], 0.0, op0=ALU.add, op1=ALU.add)),
             reads=["tot", "zer"], writes=["inc"])
    S.op("dve", lambda e: e.tensor_tensor(off[:], inc[:], tot[:], op=ALU.subtract), reads=["inc", "tot"], writes=["off"])
    S.op("dve", lambda e: e.tensor_tensor(Lk[:].rearrange("p h t -> p (h t)"), psw[:, 0:256], off[:].rearrange("p h t -> p (h t)"), op=ALU.add),
         reads=[("ps_s", 0), "off"], writes=["Lk"])
    for j in range(NSLOT):
        for h in range(4):
            S.op("dve", (lambda e, j=j, h=h: e.tensor_tensor(t64[:], C["oh"][:, j, :], off[:, h, :], op=ALU.mult)), reads=["oh", "off"], writes=["t64"])
            S.op("dve", (lambda e, j=j, h=h: e.reduce_sum(lref[:, j * 4 + h:j * 4 + h + 1], t64[:], axis=AX.X)), reads=["t64"], writes=["lref"])
    for j in range(NSLOT):
        for h in range(4):
            S.op("dve", (lambda e, j=j, h=h: e.tensor_scalar(bb[:, j, h, :], Lk[:, h, :], lref[:, j * 4 + h:j * 4 + h + 1], 30.0, op0=ALU.subtract, op1=ALU.min)),
                 reads=["Lk", "lref"], writes=["bb"])
    units = []

    def mk(j, kt):
        def load():
            return (kst.load(ktile_src(Dm, "bk", kt)), vload(cx, vst, vtile_src(Dm, "bv", kt)))

        def compute(hd):
            (kb, kkey), (vb, vkey) = hd
            if kt == 0:
                at.begin_acc()
            mms = [(h * 128, 128, kb[:, h, :], q_sb[:, j, h, :], [kkey, "q"]) for h in range(4)]
            b = at.scores(mms)
            k = at.exp(b, biases=[bb[:, j, h, kt:kt + 1] for h in range(4)], breads=["bb"])
            masked = kt >= 8 * j
            if masked:
                at.mask_stt(k, C["qrel_rep"][:, j * 128:(j + 1) * 128], C["kcol_rel"][:, kt - 8 * j:kt - 8 * j + 1], ALU.is_ge, ["qrel_rep", "kcol_rel"])
            at.pv(k, masked, [vb[:, h, :] for h in range(4)], [vkey])
            if kt == 8 * (j + 1) - 1:
                finish_T(at, cx, br_dest(Dm["brT"], 1, j), ("brT", 1, j))
        return (load, compute)

    for j in range(nslot):
        for kt in range(8 * (j + 1)):
            units.append(mk(j, kt))
    pipeline(units, depth=4)


def phase_c(cx, Dm, nslot=NSLOT, topk_rounds=DSA_TOPK // 8):
    S = cx.S
    C = load_consts(cx, [("qrel_col", [128, NSLOT], F32), ("iota_row", [128, 1024], F32), ("identbf", [128, 128], BF16)])
    q_sb = cx.sb("q_sb", [128, NSLOT, 4, 128], BF16)
    qi_sb = cx.sb("qi_sb", [128, NSLOT, 8, 128], BF16)
    ki_sb = cx.sb("ki_sb", [128, 8192], BF16)
    wi_sb = cx.sb("wi_sb", [128, NSLOT, 16], F32)
    load_q(cx, q_sb, Dm["fmq"], FMQ_OFF["cq"], 4, "q")
    load_q(cx, qi_sb, Dm["fmq"], FMQ_OFF["cqi"], 8, "qi")
    S.op("sp", lambda e: e.dma_start(out=wi_sb[:], in_=Dm["smt"][:, 68:84].rearrange("(j p) c -> p j c", p=128)), writes=["wi"], dsem="wi")
    score = cx.sb("score", [128, 8192], F32)
    for i in range(4):
        dm = []
        for t in range(16 * i, 16 * (i + 1)):
            r, j = TILE_RS[t]
            for half in range(2):
                dm.append((lambda e, t=t, r=r, j=j, half=half: e.dma_start(out=score[half * 64:(half + 1) * 64, t * 128:(t + 1) * 128],
                                                                           in_=Dm["sxg"][r * 64:(r + 1) * 64, j * 128:(j + 1) * 128])))
        S.dma_many("sp", dm, [[("score", 4 * i + k_) for k_ in range(4)]] * len(dm), ("kiin", i))
        S.op("pool", (lambda e, i=i: e.tensor_copy(ki_sb[:, i * 2048:(i + 1) * 2048], score[:, i * 2048:(i + 1) * 2048])),
             reads=[("score", 4 * i + k_) for k_ in range(4)], writes=[("ki", i)])
    at = Attn(cx)
    ps_o = cx.ps("ps_o", [128, 1024], BF16)
    attn_setup_out(at, cx, ps_o, "ps_o", C["identbf"], "identbf")
    kst = TileStream(cx, "kst", [128, 4, 128], BF16, 6)
    vst = TileStream(cx, "vst", [128, 4, 129], BF16, 6)
    for i, b in enumerate(vst.bufs):
        S.op("dve", (lambda e, b=b: e.memset(b[:, :, 128:129], 1.0)), writes=[("vst", i)])
    maskqk = [cx.sb("maskqk%d" % i, [128, 8192], BF16) for i in range(2)]
    rbuf = [cx.sb("rbuf%d" % i, [128, 512], F32) for i in range(2)]
    pen = cx.sb("pen", [128, 1024], F32)
    c01 = cx.sb("c01", [128, 1024], BF16)
    m8 = cx.sb("m8", [128, 8], F32)
    ps_m = [cx.ps("ps_m%d" % i, [128, 1024], BF16) for i in range(2)]
    cnt = [0]
    tcnt = [0]

    def indexer(j):
        N = 1024 * (j + 1)
        mq = maskqk[j % 2]
        mkey = ("maskqk", j % 2)

        def do_head(ch, hh):
            csl = slice(ch * 512, (ch + 1) * 512)
            c8, half = hh // 2, hh % 2
            rows = slice(half * 64, half * 64 + 64)
            b = cnt[0] % 2
            cnt[0] += 1
            ps = at.ps_s[b]
            S.op("pe", (lambda e: e.matmul(ps[:], qi_sb[rows, j, c8, :], ki_sb[rows, csl], start=True, stop=True)),
                 reads=["qi", ("ki", ch // 4)], writes=[("ps_s", b)])
            S.op("act", (lambda e: e.activation(rbuf[b][:], ps[:], AF.Relu)), reads=[("ps_s", b)], writes=[("rbuf", b)])
            if hh == 0:
                S.op("dve", (lambda e: e.tensor_scalar(score[:, csl], rbuf[b][:], wi_sb[:, j, 0:1], None, op0=ALU.mult)),
                     reads=[("rbuf", b), "wi"], writes=[("score", ch)])
            else:
                S.op("dve", (lambda e: e.scalar_tensor_tensor(out=score[:, csl], in0=rbuf[b][:], scalar=wi_sb[:, j, hh:hh + 1], in1=score[:, csl],
                                                              op0=ALU.mult, op1=ALU.add)),
                     reads=[("rbuf", b), "wi"], writes=[("score", ch)])

        for ch in range(N // 512):
            for hh in range(16):
                do_head(ch, hh)
        allsc = [("score", ch) for ch in range(N // 512)]
        lsl = slice(1024 * j, 1024 * (j + 1))
        S.op("dve", (lambda e: e.tensor_scalar(pen[:], C["iota_row"][:], C["qrel_col"][:, j:j + 1], NEG, op0=ALU.is_gt, op1=ALU.mult)),
             reads=["iota_row", "qrel_col"], writes=["pen"])
        S.op("dve", (lambda e: e.tensor_tensor(score[:, lsl], score[:, lsl], pen[:], op=ALU.add)), reads=["pen"], writes=[("score", 2 * j), ("score", 2 * j + 1)])
        for r in range(topk_rounds):
            S.op("dve", (lambda e: e.max(out=m8[:], in_=score[:, 0:N])), reads=allsc, writes=["m8"])
            S.op("dve", (lambda e: e.match_replace(out=score[:, 0:N], in_to_replace=m8[:], in_values=score[:, 0:N], imm_value=-3e38)),
                 reads=["m8"], writes=allsc)
        S.op("dve", (lambda e: e.tensor_scalar(mq[:, 0:N], score[:, 0:N], -2e38, None, op0=ALU.is_le)), reads=allsc, writes=[mkey])
        S.op("dve", (lambda e: e.tensor_scalar(c01[:], C["iota_row"][:], C["qrel_col"][:, j:j + 1], None, op0=ALU.is_le)),
             reads=["iota_row", "qrel_col"], writes=["c01"])
        S.op("dve", (lambda e: e.tensor_tensor(mq[:, lsl], mq[:, lsl], c01[:], op=ALU.mult)), reads=["c01"], writes=[mkey])

    def mk(j, kt):
        def load():
            return (kst.load(ktile_src(Dm, "ck", kt)), vload(cx, vst, vtile_src(Dm, "cv", kt)))

        def compute(hd):
            (kb, kkey), (vb, vkey) = hd
            mq = maskqk[j % 2]
            mkey = ("maskqk", j % 2)
            if kt == 0:
                at.begin_acc()
            mms = [(h * 128, 128, kb[:, h, :], q_sb[:, j, h, :], [kkey, "q"]) for h in range(4)]
            b = at.scores(mms)
            k = at.exp(b)
            tb = tcnt[0] % 2
            tcnt[0] += 1
            S.op("pe", (lambda e: e.transpose(ps_m[tb][:, 0:128], mq[:, kt * 128:(kt + 1) * 128], C["identbf"][:])),
                 reads=[mkey, "identbf"], writes=[("ps_m", tb)])
            at.mask_tt(k, ps_m[tb][:, 0:128], [("ps_m", tb)])
            at.pv(k, True, [vb[:, h, :] for h in range(4)], [vkey])
            if kt == 8 * (j + 1) - 1:
                finish_T(at, cx, br_dest(Dm["brT"], 2, j), ("brT", 2, j))
        return (load, compute)

    for j in range(nslot):
        indexer(j)
        units = [mk(j, kt) for kt in range(8 * (j + 1))]
        pipeline(units, depth=4)


def phase_d(cx, Dm, nslot=NSLOT):
    S = cx.S
    w1, w2 = Dm["w1"], Dm["w2"]
    C = load_consts(cx, [("dwmask", [128, 5, 128], BF16), ("peT", [128, 2, 32], F32),
                         ("qpos_rep", [128, T], F32), ("qrel_rep", [128, T], F32), ("qpos_col", [128, NSLOT], F32),
                         ("kcol_rel", [128, 8], F32), ("cend_col", [128, 4], F32), ("jstart_row", [128, 128], F32),
                         ("j0row", [128, 128], F32), ("G", [128, 8192], BF16), ("ovl", [128, 4, 128], BF16),
                         ("identbf", [128, 128], BF16), ("idxW", [128, NSLOT * 5], U32), ("validW", [128, NSLOT * 5], F32)])
    q_sb = cx.sb("q_sb", [128, NSLOT, 4, 128], BF16)
    load_q(cx, q_sb, Dm["fmq"], FMQ_OFF["dq"], 4, "q")
    dg_sb = cx.sb("dg_sb", [128, NSLOT, 12], F32)
    S.op("sp", lambda e: e.dma_start(out=dg_sb[:], in_=Dm["smt"][:, 84:96].rearrange("(j p) c -> p j c", p=128)), writes=["dg"], dsem="dg")
    xc = [cx.sb("xc%d" % i, [128, 8192], BF16) for i in range(2)]
    ks_sb = cx.sb("ks_sb", [128, 8192], BF16)
    vs_sb = cx.sb("vs_sb", [128, 64, 129], BF16)
    S.op("dve", lambda e: e.memset(vs_sb[:, :, 128:129], 1.0), writes=[("vs", t8) for t8 in range(8)])
    for (name, dst, key) in (("dkc", xc[0], ("xc", 0)), ("dvc", xc[1], ("xc", 1)), ("dks", ks_sb, "ks")):
        S.dma_many("sp", [(lambda e, t=t, dst=dst, name=name: e.dma_start(out=dst[:, t * 128:(t + 1) * 128], in_=k1_src(Dm, name, t))) for t in range(64)],
                   [[key]] * 64, ("ld", name))
    for t8 in range(8):
        S.dma_many("sp", [(lambda e, t=t: e.dma_start(out=vs_sb[:, t, 0:128], in_=vtile_src(Dm, "dvs", t, H=1))) for t in range(t8 * 8, t8 * 8 + 8)],
                   [[("vs", t8)]] * 8, ("ldvs", t8))
    at = Attn(cx)
    ps_imp = cx.ps("ps_imp", [128, 512], F32)
    ps_m = cx.ps("ps_m", [128, 1024], BF16)
    ps_m2 = [cx.ps("ps_m2%d" % i, [128, 512], F32) for i in range(2)]
    attn_setup_out(at, cx, ps_m, "ps_m", C["identbf"], "identbf")
    at.ps_o = ps_m
    w1s = cx.sb("w1s", [128, 32, 256], F32)
    w1b = cx.sb("w1b", [128, 32, 256], BF16)
    w2s = cx.sb("w2s", [128, 2, 128], F32)
    w2b = cx.sb("w2b", [128, 2, 128], BF16)
    peb = cx.sb("peb", [128, 2, 32], BF16)
    bias_sb = cx.sb("bias_sb", [128, 2], F32)
    u_sb = cx.sb("u_sb", [128, 512], F32)
    z_sb = cx.sb("z_sb", [128, 512], F32)
    gT = [cx.sb("gT%d" % i, [128, 512], BF16) for i in range(2)]
    kcmp = cx.sb("kcmp", [128, 512], BF16)
    vcmp = cx.sb("vcmp", [128, 4, 129], BF16)
    S.op("dve", lambda e: e.tensor_copy(peb[:], C["peT"][:]), reads=["peT"], writes=["peb"])
    S.op("dve", lambda e: e.memset(kcmp[:], 0.0), writes=["kcmp"])
    S.op("dve", lambda e: e.memset(vcmp[:], 0.0), writes=["vcmp"])
    S.op("dve", lambda e: e.memset(vcmp[:, :, 128:129], 1.0), writes=["vcmp"])
    for i in range(2):
        S.op("dve", (lambda e, i=i: e.memset(gT[i][:], 0.0)), writes=[("gT", i)])
    ph0 = at.ps_s[0]
    pb = at.ps_s[1]

    def compress(which):
        src1 = w1[which].rearrange("(l d) c -> d l c", d=128)
        for l8 in range(4):
            S.op("sp", (lambda e, l8=l8: e.dma_start(out=w1s[:, l8 * 8:(l8 + 1) * 8, :], in_=src1[:, l8 * 8:(l8 + 1) * 8, :])),
                 writes=[("w1s", l8)], dsem=("w1s", l8))
            S.op("pool", (lambda e, l8=l8: e.tensor_copy(w1b[:, l8 * 8:(l8 + 1) * 8, :], w1s[:, l8 * 8:(l8 + 1) * 8, :])),
                 reads=[("w1s", l8)], writes=[("w1b", l8)])
        S.op("sp", lambda e: e.dma_start(out=w2s[:], in_=w2[which].rearrange("(cc c) d -> c cc d", c=128)), writes=["w2s"], dsem="w2s")
        S.op("pool", lambda e: e.tensor_copy(w2b[:], w2s[:]), reads=["w2s"], writes=["w2b"])
        xv = xc[which][:].rearrange("p (n s) -> p n s", s=16)
        for cc in range(2):
            for l in range(32):
                S.op("pe", (lambda e, l=l, cc=cc: e.matmul(ph0[:, 0:511], w1b[:, l, cc * 128:(cc + 1) * 128], xv[:, l // 16:l // 16 + 511, l % 16],
                                                          start=(l == 0), stop=(l == 31))),
                     reads=[("w1b", l // 8), ("xc", which)], writes=[("ps_s", 0)])
            for l in range(32):
                S.op("pe", (lambda e, l=l, cc=cc: e.matmul(pb[:, 0:1], w1b[:, l, cc * 128:(cc + 1) * 128], peb[:, which, l:l + 1],
                                                          start=(l == 0), stop=(l == 31))),
                     reads=[("w1b", l // 8), "peb"], writes=[("ps_s", 1)])
            S.op("dve", (lambda e, cc=cc: e.tensor_copy(bias_sb[:, cc:cc + 1], pb[:, 0:1])), reads=[("ps_s", 1)], writes=["bias_sb"])
            S.op("act", (lambda e, cc=cc: e.activation(u_sb[:, 0:511], ph0[:, 0:511], AF.Identity, bias=bias_sb[:, cc:cc + 1], scale=1.0)),
                 reads=[("ps_s", 0), "bias_sb"], writes=["u_sb"])
            S.op("dve", lambda e: e.tensor_tensor(z_sb[:, 0:511], u_sb[:, 0:511], u_sb[:, 0:511], op=ALU.mult), reads=["u_sb"], writes=["z_sb"])
            S.op("dve", lambda e: e.tensor_scalar(z_sb[:, 0:511], z_sb[:, 0:511], 0.044715, 1.0, op0=ALU.mult, op1=ALU.add), reads=["z_sb"], writes=["z_sb"])
            S.op("dve", lambda e: e.tensor_tensor(z_sb[:, 0:511], z_sb[:, 0:511], u_sb[:, 0:511], op=ALU.mult), reads=["z_sb", "u_sb"], writes=["z_sb"])
            S.op("act", lambda e: e.activation(z_sb[:, 0:511], z_sb[:, 0:511], AF.Sigmoid, scale=2.0 * GELU_C), reads=["z_sb"], writes=["z_sb"])
            S.op("dve", (lambda e, cc=cc: e.tensor_tensor(gT[cc][:, 0:511], u_sb[:, 0:511], z_sb[:, 0:511], op=ALU.mult)),
                 reads=["z_sb", "u_sb"], writes=[("gT", cc)])
        if which == 0:
            for cc in range(2):
                S.op("pe", (lambda e, cc=cc: e.matmul(ph0[:, 0:511], w2b[:, cc, :], gT[cc][:, 0:511], start=(cc == 0), stop=(cc == 1))),
                     reads=["w2b", ("gT", cc)], writes=[("ps_s", 0)])
            S.op("act", lambda e: e.copy(kcmp[:, 0:511], ph0[:, 0:511]), reads=[("ps_s", 0)], writes=["kcmp"])
        else:
            for nt in range(4):
                M = 128 if nt < 3 else 127
                phn = at.ps_s[nt % 2]
                for cc in range(2):
                    S.op("pe", (lambda e, cc=cc, nt=nt, M=M, phn=phn: e.matmul(phn[0:M, 0:128], gT[cc][:, nt * 128:nt * 128 + M], w2b[:, cc, :], start=(cc == 0), stop=(cc == 1))),
                         reads=["w2b", ("gT", cc)], writes=[("ps_s", nt % 2)])
                S.op("act", (lambda e, nt=nt, M=M, phn=phn: e.copy(vcmp[0:M, nt, 0:128], phn[0:M, 0:128])), reads=[("ps_s", nt % 2)], writes=["vcmp"])

    compress(0)
    compress(1)
    gsig = cx.sb("gsig", [128, 12], F32)
    coef = cx.sb("coef", [128, 4], F32)
    accO = cx.sb("accO", [128, 512], F32)
    imp = cx.sb("imp", [128, 128], F32)
    nD = cx.sb("nD", [128, 128], F32)
    adm = cx.sb("adm", [128, 128], F32)
    frc = cx.sb("frc", [128, 128], F32)
    val = cx.sb("val", [128, 128], F32)
    wrk = cx.sb("wrk", [128, 128], F32)
    m8 = cx.sb("m8", [128, 8], F32)
    thr = cx.sb("thr", [128, 1], F32)
    sel = cx.sb("sel", [128, 128], BF16)
    selT = cx.sb("selT", [128, 128], BF16)
    kwtm = [cx.sb("kwtm%d" % i, [128, 128], BF16) for i in range(5)]
    kwT = cx.sb("kwT", [128, 5, 128], BF16)
    vwb = [cx.sb("vwb%d" % i, [128, 129], BF16) for i in range(5)]
    for i in range(5):
        S.op("dve", (lambda e, i=i: e.memset(vwb[i][:, 128:129], 1.0)), writes=[("vwb", i)])
    mcnt = [0]

    def coef_for(branch):
        at.recip()
        g3 = gsig[:].rearrange("p (h b) -> p h b", b=3)
        S.op("dve", (lambda e: e.tensor_tensor(coef[:], at.rs[:], g3[:, :, branch], op=ALU.mult)), reads=["rs_t", "gsig"], writes=["coef"])

    def do_slot(j):
        jsl = slice(j * 128, (j + 1) * 128)
        q512 = q_sb[:, j].rearrange("p h q -> p (h q)")
        S.op("act", (lambda e: e.activation(gsig[:], dg_sb[:, j, :], AF.Sigmoid)), reads=["dg"], writes=["gsig"])
        for w in range(5):
            col = j * 5 + w
            S.op("pool", (lambda e, w=w, col=col: e.indirect_dma_start(out=kwtm[w][:], out_offset=None, in_=Dm["kwxg"],
                                                                       in_offset=bass.IndirectOffsetOnAxis(ap=C["idxW"][:, col:col + 1], axis=0))),
                 reads=["idxW"], writes=[("kwtm", w)], dsem=("kwtm", w))
            S.op("pool", (lambda e, w=w, col=col: e.indirect_dma_start(out=vwb[w][:, 0:128], out_offset=None, in_=Dm["vwxg"],
                                                                       in_offset=bass.IndirectOffsetOnAxis(ap=C["idxW"][:, col:col + 1], axis=0))),
                 reads=["idxW"], writes=[("vwb", w)], dsem=("vwb", w))
        at.begin_acc()
        imp_started = [False]
        for nt in range(4):
            b = at.scores([(0, 512, kcmp[:, nt * 128:(nt + 1) * 128], q512, ["kcmp", "q"])])
            k = at.exp(b)
            at.mask_stt(k, C["qpos_rep"][:, jsl], C["cend_col"][:, nt:nt + 1], ALU.is_ge, ["qpos_rep", "cend_col"])
            at.pv(k, True, [vcmp[:, nt, :]] * 4, ["vcmp"], extra=(ps_imp, "ps_imp", C["ovl"][:, nt, :], ["ovl"], imp_started))
        coef_for(0)
        for h in range(4):
            a, c0 = h // 2, (h % 2) * 129
            S.op("dve", (lambda e, h=h, a=a, c0=c0: e.tensor_scalar(accO[:, h * 128:(h + 1) * 128], at.acc[a][:, c0:c0 + 128], coef[:, h:h + 1], None, op0=ALU.mult)),
                 reads=[("ps_acc", a), "coef"], writes=["accO"])
        S.op("dve", (lambda e: e.tensor_scalar(imp[:], ps_imp[:, 0:128], at.rs[:, 0:1], None, op0=ALU.mult)), reads=["ps_imp", "rs_t"], writes=["imp"])
        for h in range(1, 4):
            S.op("dve", (lambda e, h=h: e.scalar_tensor_tensor(out=imp[:], in0=ps_imp[:, h * 128:(h + 1) * 128], scalar=at.rs[:, h:h + 1], in1=imp[:], op0=ALU.mult, op1=ALU.add)),
                 reads=["ps_imp", "rs_t"], writes=["imp"])
        S.op("dve", (lambda e: e.tensor_scalar(nD[:], C["jstart_row"][:], C["qpos_col"][:, j:j + 1], None, op0=ALU.subtract)),
             reads=["jstart_row", "qpos_col"], writes=["nD"])
        S.op("dve", (lambda e: e.tensor_scalar(adm[:], nD[:], 0.0, None, op0=ALU.is_le)), reads=["nD"], writes=["adm"])
        S.op("dve", (lambda e: e.scalar_tensor_tensor(out=frc[:], in0=nD[:], scalar=-128.0, in1=adm[:], op0=ALU.is_gt, op1=ALU.mult)),
             reads=["nD", "adm"], writes=["frc"])
        S.op("dve", (lambda e: e.tensor_tensor(frc[:], frc[:], C["j0row"][:], op=ALU.max)), reads=["j0row"], writes=["frc"])
        S.op("dve", (lambda e: e.tensor_tensor(val[:], imp[:], adm[:], op=ALU.mult)), reads=["imp", "adm"], writes=["val"])
        S.op("dve", (lambda e: e.tensor_scalar(adm[:], adm[:], -1.0, 1e30, op0=ALU.add, op1=ALU.mult)), reads=["adm"], writes=["adm"])
        S.op("dve", (lambda e: e.tensor_tensor(val[:], val[:], adm[:], op=ALU.add)), reads=["adm"], writes=["val"])
        S.op("dve", (lambda e: e.scalar_tensor_tensor(out=val[:], in0=frc[:], scalar=1e9, in1=val[:], op0=ALU.mult, op1=ALU.add)),
             reads=["frc"], writes=["val"])
        S.op("dve", (lambda e: e.max(out=m8[:], in_=val[:])), reads=["val"], writes=["m8"])
        S.op("dve", (lambda e: e.match_replace(out=wrk[:], in_to_replace=m8[:], in_values=val[:], imm_value=-3e38)), reads=["m8", "val"], writes=["wrk"])
        S.op("dve", (lambda e: e.max(out=m8[:], in_=wrk[:])), reads=["wrk"], writes=["m8"])
        S.op("dve", (lambda e: e.tensor_scalar(thr[:], m8[:, 7:8], -1e29, None, op0=ALU.max)), reads=["m8"], writes=["thr"])
        S.op("dve", (lambda e: e.tensor_scalar(sel[:], val[:], thr[:], None, op0=ALU.is_ge)), reads=["val", "thr"], writes=["sel"])
        S.op("pe", (lambda e: e.transpose(ps_m[:, 0:128], sel[:], C["identbf"][:])), reads=["sel", "identbf"], writes=["ps_m"])
        S.op("act", (lambda e: e.copy(selT[:], ps_m[:, 0:128])), reads=["ps_m"], writes=["selT"])
        at.begin_acc()
        for kt in range(8 * (j + 1)):
            b = at.scores([(0, 512, ks_sb[:, kt * 128:(kt + 1) * 128], q512, ["ks", "q"])])
            k = at.exp(b)
            mb = mcnt[0] % 2
            mcnt[0] += 1
            S.op("pe", (lambda e, kt=kt, mb=mb: e.matmul(ps_m2[mb][:, 0:128], C["G"][:, kt * 128:(kt + 1) * 128], selT[:], start=True, stop=True)),
                 reads=["G", "selT"], writes=[("ps_m2", mb)])
            at.mask_tt(k, ps_m2[mb][:, 0:128], [("ps_m2", mb)])
            if kt >= 8 * j:
                at.mask_stt(k, C["qrel_rep"][:, jsl], C["kcol_rel"][:, kt - 8 * j:kt - 8 * j + 1], ALU.is_ge, ["qrel_rep", "kcol_rel"], inplace_pt=True)
            at.pv(k, True, [vs_sb[:, kt, :]] * 4, [("vs", kt // 8)])
        coef_for(1)
        for h in range(4):
            a, c0 = h // 2, (h % 2) * 129
            S.op("dve", (lambda e, h=h, a=a, c0=c0: e.scalar_tensor_tensor(out=accO[:, h * 128:(h + 1) * 128], in0=at.acc[a][:, c0:c0 + 128], scalar=coef[:, h:h + 1],
                                                                           in1=accO[:, h * 128:(h + 1) * 128], op0=ALU.mult, op1=ALU.add)),
                 reads=[("ps_acc", a), "coef"], writes=["accO"])
        for w in range(5):
            S.op("pe", (lambda e, w=w: e.transpose(ps_m[:, 128 + w * 128:256 + w * 128], kwtm[w][:], C["identbf"][:])), reads=[("kwtm", w), "identbf"], writes=["ps_m"])
        S.op("act", (lambda e: e.copy(kwT[:].rearrange("p w k -> p (w k)"), ps_m[:, 128:768])), reads=["ps_m"], writes=["kwT"])
        at.begin_acc()
        for w in range(5):
            b = at.scores([(0, 512, kwT[:, w, :], q512, ["kwT", "q"])])
            k = at.exp(b)
            mask_stt2(at, cx, k, C["validW"][:, j * 5 + w:j * 5 + w + 1], C["dwmask"][:, w, :], ["dwmask", "validW"])
            at.pv(k, True, [vwb[w][:]] * 4, [("vwb", w)])
        coef_for(2)
        o = at.no % 2
        at.no += 1
        ob = at.osb[o]
        for h in range(4):
            a, c0 = h // 2, (h % 2) * 129
            S.op("dve", (lambda e, h=h, a=a, c0=c0: e.scalar_tensor_tensor(out=ob[:, h * 128:(h + 1) * 128], in0=at.acc[a][:, c0:c0 + 128], scalar=coef[:, h:h + 1],
                                                                           in1=accO[:, h * 128:(h + 1) * 128], op0=ALU.mult, op1=ALU.add)),
                 reads=[("ps_acc", a), "coef", "accO"], writes=[("osb", o)])
        attn_out_T(at, cx, ob, ("osb", o), br_dest(Dm["brT"], 3, j), ("brT", 3, j))

    for j in range(nslot):
        do_slot(j)


def phase_p3(cx, last):
    if True:
        S = cx.S
        xT = cx.din("xT", [D, T], F32)
        hT = cx.din("hT", [D, T], BF16)
        brT = cx.din("brT", [2048, T], BF16)
        wg = cx.din("wg", [D, 8192], F32)
        bg = cx.din("bg", [128, 64], F32)
        wb = cx.din("wb", [2048, D], F32)
        wo = cx.din("wo", [D, D], F32)
        gffn = cx.din("gffn", [128, KC], F32)
        wfi = cx.din("wfi", [D, 2 * FF], F32)
        wfo = cx.din("wfo", [FF, D], F32)
        gfin = cx.din("gfin", [128, KC], F32)
        x2T = cx.dout("x2T", [D, T], F32)
        onT = cx.dout("onT", [D, T], F32)

        R = cx.sb("R", [128, 24576], F32)
        Rb = R.bitcast(BF16)
        h1 = Rb[:, 0:16384].rearrange("p (k t) -> p k t", k=KC)
        br = Rb[:, 16384:32768].rearrange("p (k t) -> p k t", k=KC)
        mg = Rb[:, 32768:49152].rearrange("p (k t) -> p k t", k=KC)
        xs = R[:, 0:16384].rearrange("p (k t) -> p k t", k=KC)
        h2 = mg
        act = cx.sb("act", [128, FCH, T], BF16)
        tA = [[cx.sb("tA%d%d" % (i, j), [128, TG], F32) for j in range(2)] for i in range(2)]
        acc = [cx.sb("acc%d" % i, [128, TG], F32) for i in range(2)]
        tmp = [cx.sb("tmp%d" % i, [128, TG], F32) for i in range(2)]
        rstd = cx.sb("rstd", [128, TG], F32)
        ones32 = cx.sb("ones32", [128, 128], F32)
        bg_sb = cx.sb("bg_sb", [128, 64], F32)
        gf_sb = cx.sb("gf_sb", [128, KC], F32)
        gl_sb = cx.sb("gl_sb", [128, KC], F32)
        ps_a = [cx.ps("ps_a%d" % i, [128, TG], F32) for i in range(2)]
        ps_b = [cx.ps("ps_b%d" % i, [128, TG], F32) for i in range(2)]
        ps_ss = cx.ps("ps_ss", [128, TG], F32)
        ws = WStream(cx, nslots=3)

        S.op("dve", lambda e: e.memset(ones32[:], 1.0), writes=["ones32"])
        S.op("sp", lambda e: e.dma_start(out=bg_sb[:], in_=bg), writes=["bg"], dsem="bg")
        S.op("sp", lambda e: e.dma_start(out=gf_sb[:], in_=gffn), writes=["gf"], dsem="gf")
        S.op("sp", lambda e: e.dma_start(out=gl_sb[:], in_=gfin), writes=["gl"], dsem="gl")
        S.dma_many("sp", [(lambda e, kc=kc: e.dma_start(out=h1[:, kc, :], in_=hT[kc * 128:(kc + 1) * 128, :])) for kc in range(KC)],
                   [[("h", kc)] for kc in range(KC)], "hin")
        S.dma_many("sp", [(lambda e, kc=kc: e.dma_start(out=br[:, kc, :], in_=brT[kc * 128:(kc + 1) * 128, :])) for kc in range(KC)],
                   [[("br", kc)] for kc in range(KC)], "brin")

        def tsl_of(tg):
            return slice(tg * TG, (tg + 1) * TG)

        def mm_group(ps, pkey, bf, bkey, nk, M, rhs_fn, rkey_fn, first=True, last=True):
            for kc in range(nk):
                S.op("pe", (lambda e, kc=kc: e.matmul(ps[0:M, :], bf[:, kc, 0:M], rhs_fn(kc), start=(first and kc == 0), stop=(last and kc == nk - 1))),
                     reads=[bkey, rkey_fn(kc)], writes=[pkey])

        units = []

        def u_gate(dc, n):
            def load():
                return ws.load(wg, 0, KC, n * 2048 + dc * 128, 128)

            def compute(hd):
                bf, bkey = hd
                for tg in range(NTG):
                    tsl = tsl_of(tg)
                    mm_group(ps_a[tg], ("ps_a", tg), bf, bkey, KC, 128, (lambda kc, tsl=tsl: h1[:, kc, tsl]), (lambda kc: ("h", kc)))
                    gs = tA[tg][n % 2]
                    S.op("act", (lambda e, tg=tg, gs=gs: e.activation(gs[:], ps_a[tg][:], AF.Sigmoid, bias=bg_sb[:, n * 16 + dc:n * 16 + dc + 1], scale=1.0)),
                         reads=[("ps_a", tg), "bg"], writes=[("tA", tg, n % 2)])
            return (load, compute)

        def u_branch(dc, n):
            def load():
                return ws.load(wb, n * 512, 4, dc * 128, 128)

            def compute(hd):
                bf, bkey = hd
                for tg in range(NTG):
                    tsl = tsl_of(tg)
                    mm_group(ps_b[tg], ("ps_b", tg), bf, bkey, 4, 128, (lambda kc, tsl=tsl: br[:, n * 4 + kc, tsl]), (lambda kc: ("br", n * 4 + kc)))
                    gs = tA[tg][n % 2]
                    if n == 0:
                        S.op("dve", (lambda e, tg=tg, gs=gs: e.tensor_tensor(acc[tg][:], gs[:], ps_b[tg][:], op=ALU.mult)),
                             reads=[("tA", tg, n % 2), ("ps_b", tg)], writes=[("acc", tg)])
                    else:
                        S.op("dve", (lambda e, tg=tg, gs=gs: e.tensor_tensor(tmp[tg][:], gs[:], ps_b[tg][:], op=ALU.mult)),
                             reads=[("tA", tg, n % 2), ("ps_b", tg)], writes=[("tmp", tg)])
                        if n < 3:
                            S.op("pool", (lambda e, tg=tg: e.tensor_tensor(acc[tg][:], acc[tg][:], tmp[tg][:], op=ALU.add)),
                                 reads=[("tmp", tg)], writes=[("acc", tg)])
                        else:
                            S.op("pool", (lambda e, tg=tg, tsl=tsl: e.tensor_tensor(mg[:, dc, tsl], acc[tg][:], tmp[tg][:], op=ALU.add)),
                                 reads=[("tmp", tg), ("acc", tg)], writes=[("mg", dc)])
            return (load, compute)

        for dc in range(KC):
            for n in range(4):
                units.append(u_gate(dc, n))
                units.append(u_branch(dc, n))
        pipeline(units, depth=2)
        S.barrier()
        S.dma_many("sp", [(lambda e, kc=kc: e.dma_start(out=xs[:, kc, :], in_=xT[kc * 128:(kc + 1) * 128, :])) for kc in range(KC)],
                   [[("x", kc)] for kc in range(KC)], "xin")

        def u_out(dc):
            def load():
                return ws.load(wo, 0, KC, dc * 128, 128)

            def compute(hd):
                bf, bkey = hd
                for tg in range(NTG):
                    tsl = tsl_of(tg)
                    mm_group(ps_a[tg], ("ps_a", tg), bf, bkey, KC, 128, (lambda kc, tsl=tsl: mg[:, kc, tsl]), (lambda kc: ("mg", kc)))
                    S.op("dve", (lambda e, tg=tg, tsl=tsl: e.tensor_tensor(xs[:, dc, tsl], xs[:, dc, tsl], ps_a[tg][:], op=ALU.add)),
                         reads=[("ps_a", tg)], writes=[("x", dc)])
            return (load, compute)

        pipeline([u_out(dc) for dc in range(KC)], depth=2)
        S.barrier()
        def wr_h2(kc, tg, tsl, emit):
            emit(h2[:, kc, tsl], ("h2", kc))

        rmsnorm_gen(cx, (lambda kc, tsl: xs[:, kc, tsl]), "x", gf_sb, "gf", ones32, ps_ss, tmp, "tmp", rstd, wr_h2)

        for hh in range(2):
            units = []

            def u_ffn_in(fcl, which, hh=hh):
                col0 = which * FF + (hh * FCH + fcl) * 128

                def load():
                    return ws.load(wfi, 0, KC, col0, 128)

                def compute(hd):
                    bf, bkey = hd
                    for tg in range(NTG):
                        tsl = tsl_of(tg)
                        if which == 0:
                            mm_group(ps_a[tg], ("ps_a", tg), bf, bkey, KC, 128, (lambda kc, tsl=tsl: h2[:, kc, tsl]), (lambda kc: ("h2", kc)))
                            sg = tA[tg][fcl % 2]
                            S.op("act", (lambda e, tg=tg, sg=sg: e.activation(sg[:], ps_a[tg][:], AF.Silu)),
                                 reads=[("ps_a", tg)], writes=[("tA", tg, fcl % 2)])
                        else:
                            mm_group(ps_b[tg], ("ps_b", tg), bf, bkey, KC, 128, (lambda kc, tsl=tsl: h2[:, kc, tsl]), (lambda kc: ("h2", kc)))
                            sg = tA[tg][fcl % 2]
                            S.op("dve", (lambda e, tg=tg, sg=sg, tsl=tsl: e.tensor_tensor(act[:, fcl, tsl], sg[:], ps_b[tg][:], op=ALU.mult)),
                                 reads=[("tA", tg, fcl % 2), ("ps_b", tg)], writes=[("act", fcl)])
                return (load, compute)

            def u_ffn_out(dc, part, hh=hh):
                k0 = 0 if part == 0 else 16
                nk = 16 if part == 0 else FCH - 16

                def load():
                    return ws.load(wfo, (hh * FCH + k0) * 128, nk, dc * 128, 128)

                def compute(hd):
                    bf, bkey = hd
                    for tg in range(NTG):
                        tsl = tsl_of(tg)
                        mm_group(ps_a[tg], ("ps_a", tg), bf, bkey, nk, 128, (lambda kc, tsl=tsl: act[:, k0 + kc, tsl]), (lambda kc: ("act", k0 + kc)),
                                 first=(part == 0), last=(part == 1))
                        if part == 1:
                            S.op("dve", (lambda e, tg=tg, tsl=tsl: e.tensor_tensor(xs[:, dc, tsl], xs[:, dc, tsl], ps_a[tg][:], op=ALU.add)),
                                 reads=[("ps_a", tg)], writes=[("x", dc)])
                return (load, compute)

            for fcl in range(FCH):
                units.append(u_ffn_in(fcl, 0))
                units.append(u_ffn_in(fcl, 1))
            for dc in range(KC):
                units.append(u_ffn_out(dc, 0))
                units.append(u_ffn_out(dc, 1))
            pipeline(units, depth=2)

        S.dma_many("sp", [(lambda e, kc=kc: e.dma_start(out=x2T[kc * 128:(kc + 1) * 128, :], in_=xs[:, kc, :])) for kc in range(KC)],
                   [[("x2T", kc)] for kc in range(KC)], "x2o", reads_list=[[("x", kc)] for kc in range(KC)])
        if last:
            ocnt = [0]

            def wr_fin(kc, tg, tsl, emit):
                k = ocnt[0]
                ocnt[0] += 1
                ob = tA[k % 2][(k // 2) % 2]
                okey = ("tA", k % 2, (k // 2) % 2)
                emit(ob[:], okey)
                S.op("sp", (lambda e: e.dma_start(out=onT[kc * 128:(kc + 1) * 128, tsl], in_=ob[:])),
                     reads=[okey], writes=[("onT", kc, tg)], dsem=("ono", k % 4))

            rmsnorm_gen(cx, (lambda kc, tsl: xs[:, kc, tsl]), "x", gl_sb, "gl", ones32, ps_ss, tmp, "tmp", rstd, wr_fin)


def build_fused(depth=2, nslot=NSLOT, phases="1xabcd3", debug=False):
    nc = bass.Bass("TRN2", target_bir_lowering=False, num_devices=NCORES)
    EXT = {}

    def ext(name, shape, dt):
        if name not in EXT:
            EXT[name] = nc.dram_tensor(name, shape, dt, kind="ExternalInput").ap()
        return EXT[name]

    def scr(name, shape, dt, shared=False):
        if shared:
            return nc.dram_tensor(name, shape, dt, addr_space="Shared").ap()
        return nc.dram_tensor(name, shape, dt).ap()

    with ExitStack() as st:
        S = Sched(nc, st)
        xT_in = ext("xT", [D, T], F32)
        gmix = ext("gmix", [depth, 128, KC], F32)
        w_in = ext("w_in", [depth, D, 10080], F32)
        cs = ext("cs", [6, 128, T], F32)
        rmat = ext("rmat", [3, 128, 128], F32)
        ident32 = ext("ident32", [128, 128], F32)
        onT = nc.dram_tensor("onT", [D, T], F32, kind="ExternalOutput").ap()
        consts = dict(
            amask=ext("amask", [128, 24, 128], BF16), idxA=ext("idxA", [128, NSLOT * 24], U32), validA=ext("validA", [128, NSLOT * 24], F32),
            identbf=ext("identbf", [128, 128], BF16), oh=ext("oh", [128, NSLOT, 64], F32), qrel_rep=ext("qrel_rep", [128, T], F32),
            kcol_rel=ext("kcol_rel", [128, 8], F32), tri32=ext("tri32", [128, 128], F32), qrel_col=ext("qrel_col", [128, NSLOT], F32),
            iota_row=ext("iota_row", [128, 1024], F32), dwmask=ext("dwmask", [128, 5, 128], BF16), qpos_rep=ext("qpos_rep", [128, T], F32),
            qpos_col=ext("qpos_col", [128, NSLOT], F32), cend_col=ext("cend_col", [128, 4], F32), jstart_row=ext("jstart_row", [128, 128], F32),
            j0row=ext("j0row", [128, 128], F32), G=ext("G", [128, 8192], BF16), ovl=ext("ovl", [128, 4, 128], BF16),
            idxW=ext("idxW", [128, NSLOT * 5], U32), validW=ext("validW", [128, NSLOT * 5], F32))
        bf_rep = ext("bf_rep", [depth, 128, 4], F32)
        peT = ext("peT", [depth, 128, 2, 32], F32)
        w1 = ext("w1", [depth, 2, 4096, 256], F32)
        w2 = ext("w2", [depth, 2, 256, 128], F32)
        wg = ext("wg", [depth, D, 8192], F32)
        bg = ext("bg", [depth, 128, 64], F32)
        wb = ext("wb", [depth, 2048, D], F32)
        wo = ext("wo", [depth, D, D], F32)
        gffn = ext("gffn", [depth, 128, KC], F32)
        wfi = ext("wfi", [depth, D, 2 * FF], F32)
        wfo = ext("wfo", [depth, FF, D], F32)
        gfin = ext("gfin", [128, KC], F32)
        fmq = scr("fmq", [4096, T], BF16)
        kx = scr("kx", [KX_ROWS, T], BF16)
        vx = scr("vx", [T, VX_COLS], BF16)
        sx = scr("sx", [64, T], F32)
        smt = scr("smt", [T, 96], F32)
        hT_d = scr("hT_d", [D, T], BF16)
        brT = scr("brT_d", [2048, T], BF16)
        x_d = [scr("x_d%d" % i, [D, T], F32) for i in range(2)]
        tm = {"vx": vx, "kwx": scr("kwx", [T, 128], BF16), "vwx": scr("vwx", [T, 128], BF16)}
        for g in range(3):
            tm["akx%d" % g] = scr("akx%d" % g, [T, 512], BF16)
            tm["avx%d" % g] = scr("avx%d" % g, [T, 512], BF16)
        gath = {"kxg": scr("kxg", [NCORES * KX_ROWS, T], BF16, SHARED_OUT), "vxg": scr("vxg", [NCORES * T, VX_COLS], BF16, SHARED_OUT),
                "sxg": scr("sxg", [NCORES * 64, T], F32, SHARED_OUT), "smtg": scr("smtg", [NCORES * T, 96], F32, SHARED_OUT),
                "kwxg": scr("kwxg", [NCORES * T, 128], BF16, SHARED_OUT), "vwxg": scr("vwxg", [NCORES * T, 128], BF16, SHARED_OUT)}
        for g in range(3):
            gath["akxg%d" % g] = scr("akxg%d" % g, [NCORES * T, 512], BF16, SHARED_OUT)
            gath["avxg%d" % g] = scr("avxg%d" % g, [NCORES * T, 512], BF16, SHARED_OUT)
        pairs = [("kx", kx, "kxg"), ("vx", vx, "vxg"), ("sx", sx, "sxg"), ("smt", smt, "smtg"), ("kwx", tm["kwx"], "kwxg"), ("vwx", tm["vwx"], "vwxg")]
        for g in range(3):
            pairs.append(("akx%d" % g, tm["akx%d" % g], "akxg%d" % g))
            pairs.append(("avx%d" % g, tm["avx%d" % g], "avxg%d" % g))

        def run_phase(fn, ov=None):
            with ExitStack() as ph:
                cx = Ctx(nc, ph, S=S, ov=ov)
                fn(cx)
                S.phase_end()

        for l in range(depth):
            x_src = xT_in if l == 0 else x_d[(l - 1) % 2]
            x_dst = x_d[l % 2]
            last = (l == depth - 1)
            Dm = dict(xT=x_src, gcol=gmix[l], w=w_in[l], cs=cs, rmat=rmat, ident32=ident32, fmq=fmq, kx=kx, sx=sx, smt=smt, hT=hT_d, brT=brT,
                      w1=w1[l], w2=w2[l])
            Dm.update(tm)
            Dm.update(gath)
            if "1" in phases:
                run_phase(lambda cx: phase_p1(cx, Dm))
            if "x" in phases:
                for (nm, src, dstn) in pairs:
                    S.op("pool", (lambda e, src=src, dstn=dstn: e.collective_compute("AllGather", ALU.bypass, replica_groups=[list(range(NCORES))],
                                                                                     ins=[src.opt()], outs=[gath[dstn].opt()])),
                         reads=[nm], writes=[dstn], dsem=("cc", nm, l), dinc=1)
                S.phase_end()
            ovc = dict(consts)
            ovc["bf_rep"] = bf_rep[l]
            ovc["peT"] = peT[l]
            if "a" in phases:
                run_phase(lambda cx: phase_a(cx, Dm, nslot), ovc)
            if "b" in phases:
                run_phase(lambda cx: phase_b(cx, Dm, nslot), ovc)
            if "c" in phases:
                run_phase(lambda cx: phase_c(cx, Dm, nslot), ovc)
            if "d" in phases:
                run_phase(lambda cx: phase_d(cx, Dm, nslot), ovc)
            if "3" in phases:
                ov3 = dict(xT=x_src, hT=hT_d, brT=brT, wg=wg[l], bg=bg[l], wb=wb[l], wo=wo[l], gffn=gffn[l], wfi=wfi[l], wfo=wfo[l], gfin=gfin,
                           x2T=x_dst, onT=onT)
                run_phase(lambda cx: phase_p3(cx, last), ov3)
        if debug:
            def dump(cx, name, src, dt):
                R_, C_ = src.shape
                out = nc.dram_tensor("dbg_" + name, [R_, C_], dt, kind="ExternalOutput").ap()
                nb = R_ // 128
                bufs = [cx.sb("dbgb_%s%d" % (name, i), [128, C_], dt) for i in range(2)]
                for k in range(nb):
                    b = bufs[k % 2]
                    S.op("sp", (lambda e, k=k, b=b: e.dma_start(out=b[:], in_=src[k * 128:(k + 1) * 128, :])), writes=[("dbgb", name, k % 2)], dsem=("dbgi", name, k % 2))
                    S.op("sp", (lambda e, k=k, b=b: e.dma_start(out=out[k * 128:(k + 1) * 128, :], in_=b[:])), reads=[("dbgb", name, k % 2)], writes=[("dbgo", name, k)], dsem=("dbgo", name, k % 2))

            def dbg_phase(cx):
                dump(cx, "hT", hT_d, BF16)
                dump(cx, "fmq", fmq, BF16)
                dump(cx, "kx", kx, BF16)
                dump(cx, "akx0", tm["akx0"], BF16)
                dump(cx, "vx", vx, BF16)
                dump(cx, "smt", smt, F32)
                dump(cx, "sx", sx[0:0 + 0, :] if False else smt, F32) if False else None
                dump(cx, "kxg", gath["kxg"], BF16)
                dump(cx, "akxg0", gath["akxg0"], BF16)
                dump(cx, "vxg", gath["vxg"], BF16)
                dump(cx, "smtg", gath["smtg"], F32)
                dump(cx, "brT", brT, BF16)
                dump(cx, "x0", x_d[0], F32)
            run_phase(dbg_phase)
    return nc

BF = ml_dtypes.bfloat16
NSLOT = 8


def slot_tile(c, j):
    return 16 * (j // 2) + (c if j % 2 == 0 else 15 - c)


def core_tokens(c):
    return np.concatenate([np.arange(slot_tile(c, j) * 128, slot_tile(c, j) * 128 + 128) for j in range(NSLOT)])


def common_consts(c):
    toks = core_tokens(c).astype(np.float32)
    qpos_rep = np.broadcast_to(toks[None, :], (128, 1024)).copy()
    qrel = toks - np.repeat(np.arange(NSLOT) * 1024.0, 128).astype(np.float32)
    qrel_rep = np.broadcast_to(qrel[None, :], (128, 1024)).copy()
    qpos_col = toks.reshape(NSLOT, 128).T.copy()
    qrel_col = qrel.reshape(NSLOT, 128).T.copy()
    kcol_rel = (np.arange(8)[None, :] * 128 + np.arange(128)[:, None]).astype(np.float32)
    return dict(qpos_rep=qpos_rep, qrel_rep=qrel_rep, qpos_col=qpos_col, qrel_col=qrel_col, kcol_rel=kcol_rel)


def q_layout(qT, c_unused=None, H=4):
    a = np.asarray(qT).reshape(H, 128, NSLOT, 128)
    return np.ascontiguousarray(a.transpose(1, 2, 0, 3))


def kT_layout(kT, H=4):
    a = np.asarray(kT).reshape(H, 128, 8192)
    return np.ascontiguousarray(a.transpose(1, 0, 2))


def v_layout(vT, H=4):
    a = np.asarray(vT).reshape(H, 128, 64, 128)
    out = np.ones((128, 64, H, 129), dtype=a.dtype)
    out[:, :, :, :128] = a.transpose(3, 2, 0, 1)
    return out


def b_consts(c, flogT, b_f):
    cc = common_consts(c)
    flog = np.ascontiguousarray(np.asarray(flogT, np.float32).reshape(4, 64, 128).transpose(2, 0, 1))
    oh = np.zeros((128, NSLOT, 64), np.float32)
    for j in range(NSLOT):
        oh[:, j, slot_tile(c, j)] = 1.0
    tri = (np.arange(128)[:, None] <= np.arange(128)[None, :]).astype(np.float32)
    return dict(flog=flog, bf_rep=np.broadcast_to(np.asarray(b_f, np.float32)[None, :], (128, 4)).copy(), oh=oh,
                qrel_rep=cc["qrel_rep"], kcol_rel=cc["kcol_rel"], tri32=tri)


def a_halo(c, kT, vT):
    kT = np.asarray(kT).reshape(12, 128, 64, 128)
    vT = np.asarray(vT).reshape(12, 128, 64, 128)
    W = [2, 5, 17]
    KT = np.zeros((NSLOT, 24, 128, 4, 128), kT.dtype)
    V = np.zeros((NSLOT, 24, 128, 4, 129), vT.dtype)
    for j in range(NSLOT):
        t = slot_tile(c, j)
        ti = 0
        for g in range(3):
            for w in range(W[g]):
                tk = t - (W[g] - 1) + w
                if tk >= 0:
                    KT[j, ti] = kT[g * 4:(g + 1) * 4, :, tk, :].transpose(1, 0, 2)
                    V[j, ti, :, :, :128] = vT[g * 4:(g + 1) * 4, :, tk, :].transpose(2, 0, 1)
                    V[j, ti, :, :, 128] = 1
                ti += 1
    return KT, V


def a_mask():
    pats = ((128, 1), (512, 4), (2048, 16))
    W = [2, 5, 17]
    m = np.zeros((128, 24, 128), np.float32)
    kp = np.arange(128)[:, None]
    qp = np.arange(128)[None, :]
    ti = 0
    for g in range(3):
        Wd, r = pats[g]
        for w in range(W[g]):
            Dm = qp - kp + 128 * (W[g] - 1 - w)
            m[:, ti, :] = ((Dm >= 0) & (Dm <= Wd) & (Dm % r == 0)).astype(np.float32)
            ti += 1
    return m.astype(BF)


def kv_tiles(kT, vT, H=4):
    k = np.asarray(kT).reshape(H, 128, 64, 128)
    v = np.asarray(vT).reshape(H, 128, 64, 128)
    KT = np.ascontiguousarray(k.transpose(2, 1, 0, 3))
    V = np.ones((64, 128, H, 129), v.dtype)
    V[:, :, :, :128] = v.transpose(2, 3, 0, 1)
    return KT, V


def c_consts(c, wiT_loc):
    cc = common_consts(c)
    wi = np.ascontiguousarray(np.asarray(wiT_loc, np.float32).reshape(16, NSLOT, 128).transpose(2, 1, 0))
    iota = np.broadcast_to(np.arange(1024, dtype=np.float32)[None, :], (128, 1024)).copy()
    return dict(wi=wi, qrel_col=cc["qrel_col"], iota_row=iota, ident_bf=np.eye(128, dtype=np.float32).astype(BF))


def qi_layout(qiT_loc):
    a = np.asarray(qiT_loc).reshape(8, 128, NSLOT, 128)
    return np.ascontiguousarray(a.transpose(1, 2, 0, 3))


def d_inputs(c, qT_loc, kcT, vcT, ksT, vsT, kwT, vwT, dgT_loc, cmp_pe, cmp_w1, cmp_w2):
    cc = common_consts(c)
    vs = np.ones((128, 64, 129), np.asarray(vsT).dtype)
    vs[:, :, :128] = np.asarray(vsT).reshape(128, 64, 128).transpose(2, 1, 0)
    kw = np.zeros((128, NSLOT, 640), np.asarray(kwT).dtype)
    vw = np.zeros((128, NSLOT, 5, 129), np.asarray(vwT).dtype)
    kwt = np.asarray(kwT).reshape(128, 64, 128)
    vwt = np.asarray(vwT).reshape(128, 64, 128)
    for j in range(NSLOT):
        t = slot_tile(c, j)
        for w in range(5):
            tk = t - 4 + w
            if tk >= 0:
                kw[:, j, w * 128:(w + 1) * 128] = kwt[:, tk, :]
                vw[:, j, w, :128] = vwt[:, tk, :].T
                vw[:, j, w, 128] = 1
    kp = np.arange(128)[:, None]
    qp = np.arange(128)[None, :]
    dwmask = np.zeros((128, 5, 128), np.float32)
    for w in range(5):
        Dm = qp - kp + 128 * (4 - w)
        dwmask[:, w, :] = ((Dm >= 0) & (Dm < 512)).astype(np.float32)
    dg = np.ascontiguousarray(np.asarray(dgT_loc, np.float32).reshape(12, NSLOT, 128).transpose(2, 1, 0))
    peT = np.ascontiguousarray(np.asarray(cmp_pe, np.float32).transpose(2, 0, 1))
    n = np.arange(512)
    cend = (16 * n + 31).astype(np.float32).reshape(4, 128).T.copy()
    jstart = np.broadcast_to((64.0 * np.arange(128, dtype=np.float32))[None, :], (128, 128)).copy()
    j0row = np.zeros((128, 128), np.float32)
    j0row[:, 0] = 1
    G = (np.arange(128)[:, None] == (np.arange(8192)[None, :] // 64)).astype(np.float32).astype(BF)
    ci = np.arange(512)[:, None]
    sj = np.arange(128)[None, :]
    ovl = ((ci * 16 < (sj + 1) * 64) & (ci * 16 + 32 > sj * 64) & (ci < 511)).astype(np.float32)
    ovl = np.ascontiguousarray(ovl.reshape(4, 128, 128).transpose(1, 0, 2)).astype(BF)
    return dict(QT=q_layout(qT_loc), KcT=np.ascontiguousarray(kcT), VcT=np.ascontiguousarray(vcT), KsT=np.ascontiguousarray(ksT), Vs=vs,
                KwT=kw, Vw=vw, w1=np.asarray(cmp_w1, np.float32), w2=np.asarray(cmp_w2, np.float32),
                dwmask=dwmask.astype(BF), dg=dg, peT=peT, qpos_rep=cc["qpos_rep"], qrel_rep=cc["qrel_rep"], qpos_col=cc["qpos_col"],
                kcol_rel=cc["kcol_rel"], cend_col=cend, jstart_row=jstart, j0row=j0row, G=G, ovl=ovl,
                ident_bf=np.eye(128, dtype=np.float32).astype(BF))


_FUSED = {}


def rope_tables(pos):
    T_ = len(pos)
    inv128 = (10000.0 ** (-np.arange(64, dtype=np.float32) / 64)).astype(np.float32)
    ang128 = pos.astype(np.float32)[None, :] * inv128[np.arange(128) % 64][:, None]
    inv64 = (10000.0 ** (-np.arange(32, dtype=np.float32) / 32)).astype(np.float32)
    ang64 = pos.astype(np.float32)[None, :] * inv64[(np.arange(128) % 64) % 32][:, None]
    cs = np.zeros((6, 128, T_), np.float32)
    cs[0] = np.cos(ang128)
    cs[1] = np.sin(ang128)
    cs[2] = np.cos(ang64)
    cs[3] = np.sin(ang64)
    cs[4] = cs[2]
    cs[5] = cs[3]
    cs[4, 64:] = 1.0
    cs[5, 64:] = 0.0
    return cs


def rot_mats():
    R = np.zeros((3, 128, 128), np.float32)
    for dp in range(128):
        if dp < 64:
            R[0, dp + 64, dp] = -1.0
        else:
            R[0, dp - 64, dp] = 1.0
    for blk in range(2):
        for r in range(64):
            dp = blk * 64 + r
            if r < 32:
                R[1, dp + 32, dp] = -1.0
            else:
                R[1, dp - 32, dp] = 1.0
    return R


def gcols(g):
    g = np.asarray(g, np.float32)
    return np.ascontiguousarray(g.reshape(g.shape[:-1] + (16, 128)).swapaxes(-1, -2))


def halo_tables(c, W_list):
    ncol = NSLOT * sum(W_list)
    idx = np.zeros((128, ncol), np.uint32)
    val = np.zeros((128, ncol), np.float32)
    p = np.arange(128)
    for j in range(NSLOT):
        t = slot_tile(c, j)
        col = j * sum(W_list)
        for Wg in W_list:
            for w in range(Wg):
                tk = t - (Wg - 1) + w
                if tk >= 0:
                    r, jj = TILE_RS[tk]
                    idx[:, col] = r * 1024 + jj * 128 + p
                    val[:, col] = 1.0
                else:
                    idx[:, col] = p
                col += 1
    return idx, val


def core_inputs(c, shared):
    toks = core_tokens(c)
    cc = common_consts(c)
    d = dict(shared)
    d["cs"] = rope_tables(toks)
    idxA, validA = halo_tables(c, [2, 5, 17])
    idxW, validW = halo_tables(c, [5])
    oh = np.zeros((128, NSLOT, 64), np.float32)
    for j in range(NSLOT):
        oh[:, j, slot_tile(c, j)] = 1.0
    d.update(idxA=idxA, validA=validA, idxW=idxW, validW=validW, oh=oh, qrel_rep=cc["qrel_rep"], kcol_rel=cc["kcol_rel"],
             qrel_col=cc["qrel_col"], qpos_rep=cc["qpos_rep"], qpos_col=cc["qpos_col"])
    return d


def kernel(x, norm_mix, w_in, w_gate, b_gate, b_f, cmp_pe, cmp_w1, cmp_w2, w_branch, w_out,
           norm_ffn, w_ffn_in, w_ffn_out, norm_final):
    x = np.asarray(x, np.float32)
    S_ = x.shape[1]
    depth = np.asarray(w_in).shape[0]
    in_maps = prep_inputs(x, norm_mix, w_in, w_gate, b_gate, b_f, cmp_pe, cmp_w1, cmp_w2, w_branch, w_out,
                          norm_ffn, w_ffn_in, w_ffn_out, norm_final)
    if depth not in _FUSED:
        _FUSED[depth] = build_fused(depth=depth)
    nc = _FUSED[depth]
    res = run_bass_kernel_spmd(nc, in_maps, core_ids=list(range(NCORES))).results
    out = np.zeros((1, S_, 2048), np.float32)
    for c in range(NCORES):
        out[0, core_tokens(c)] = np.asarray(res[c]["onT"], np.float32).T
    return out


def prep_inputs(x, norm_mix, w_in, w_gate, b_gate, b_f, cmp_pe, cmp_w1, cmp_w2, w_branch, w_out,
                norm_ffn, w_ffn_in, w_ffn_out, norm_final):
    x = np.asarray(x, np.float32)
    depth = np.asarray(w_in).shape[0]
    perm = in_col_perm()
    kp = np.arange(128)[:, None]
    qp = np.arange(128)[None, :]
    dwmask = np.zeros((128, 5, 128), np.float32)
    for w in range(5):
        Dm_ = qp - kp + 128 * (4 - w)
        dwmask[:, w, :] = ((Dm_ >= 0) & (Dm_ < 512)).astype(np.float32)
    n = np.arange(512)
    ci = np.arange(512)[:, None]
    sj = np.arange(128)[None, :]
    ovl = ((ci * 16 < (sj + 1) * 64) & (ci * 16 + 32 > sj * 64) & (ci < 511)).astype(np.float32)
    j0row = np.zeros((128, 128), np.float32)
    j0row[:, 0] = 1
    shared = dict(
        gmix=gcols(norm_mix), w_in=np.ascontiguousarray(np.asarray(w_in, np.float32)[:, :, perm]), rmat=rot_mats(),
        ident32=np.eye(128, dtype=np.float32), amask=a_mask(), identbf=np.eye(128, dtype=np.float32).astype(BF),
        kcol_rel=None, tri32=(np.arange(128)[:, None] <= np.arange(128)[None, :]).astype(np.float32),
        iota_row=np.broadcast_to(np.arange(1024, dtype=np.float32)[None, :], (128, 1024)).copy(),
        dwmask=dwmask.astype(BF), cend_col=(16 * n + 31).astype(np.float32).reshape(4, 128).T.copy(),
        jstart_row=np.broadcast_to((64.0 * np.arange(128, dtype=np.float32))[None, :], (128, 128)).copy(), j0row=j0row,
        G=(np.arange(128)[:, None] == (np.arange(8192)[None, :] // 64)).astype(np.float32).astype(BF),
        ovl=np.ascontiguousarray(ovl.reshape(4, 128, 128).transpose(1, 0, 2)).astype(BF),
        bf_rep=np.ascontiguousarray(np.broadcast_to(np.asarray(b_f, np.float32)[:, None, :], (depth, 128, 4))),
        peT=np.ascontiguousarray(np.asarray(cmp_pe, np.float32).transpose(0, 3, 1, 2)),
        w1=np.asarray(cmp_w1, np.float32), w2=np.asarray(cmp_w2, np.float32),
        wg=np.ascontiguousarray(np.asarray(w_gate, np.float32).reshape(depth, 2048, 8192)),
        bg=np.ascontiguousarray(np.asarray(b_gate, np.float32).reshape(depth, 4, 16, 128).transpose(0, 3, 1, 2).reshape(depth, 128, 64)),
        wb=np.ascontiguousarray(np.asarray(w_branch, np.float32).reshape(depth, 2048, 2048)), wo=np.asarray(w_out, np.float32),
        gffn=gcols(norm_ffn), wfi=np.asarray(w_ffn_in, np.float32), wfo=np.asarray(w_ffn_out, np.float32), gfin=gcols(norm_final))
    in_maps = []
    for c in range(NCORES):
        d = core_inputs(c, shared)
        d["xT"] = np.ascontiguousarray(x[0, core_tokens(c)].T)
        in_maps.append(d)
    return in_maps
```
